# Optimizing a Trainium2 kernel written in Bass

```python
import math
import jax
import jax.numpy as jnp
from jax import lax
import numpy as np


D_MODEL = 2048
BATCH = 4
SEQ = 4096
DEPTH = 2

CTX_LEN = 256
GRID_W = 64
N_EVEN = (DEPTH + 1) // 2
N_ODD = DEPTH // 2
DEEPNORM_ALPHA = (2 * DEPTH) ** 0.25
DEEPNORM_BETA = (8 * DEPTH) ** -0.25
NORM_EPS = 1e-6
N_MOD = 6

S5_WIDTH = D_MODEL // 2
S5_GROUP = 16
S5_GROUPS = S5_WIDTH // S5_GROUP
S5_STATE = 64
S5_DT_MIN = 1e-3
S5_DT_MAX = 1e-1

DN_WIDTH = D_MODEL // 2
DN_HEAD_DIM = 128
DN_HEADS = DN_WIDTH // DN_HEAD_DIM
DN_CHUNK = 64
DN_CONV = 3
DN_DT_MIN = 1e-3
DN_DT_MAX = 1e-1

EVEN_COLS = (3 * DN_WIDTH, DN_WIDTH, S5_WIDTH, S5_WIDTH, 2 * DN_HEADS, 2 * DN_HEADS)
EVEN_IN = sum(EVEN_COLS)
EVEN_MIX = S5_WIDTH + DN_WIDTH

GLA_HEADS = 4
GLA_KEY_WIDTH = D_MODEL // 2
GLA_VAL_WIDTH = D_MODEL
GLA_DK = GLA_KEY_WIDTH // GLA_HEADS
GLA_DV = GLA_VAL_WIDTH // GLA_HEADS
GLA_RANK = 16
GLA_TAU = 16.0
GLA_CHUNK = 16
ODD_COLS = (GLA_KEY_WIDTH, GLA_KEY_WIDTH, GLA_VAL_WIDTH, GLA_VAL_WIDTH, 2 * GLA_RANK)
ODD_IN = sum(ODD_COLS)

FFN_HIDDEN = 5632
N_EXPERTS = 8
TOP_K = 2
EXPERT_HIDDEN = 7168
MOE_BLOCK = 512

F32 = jnp.float32

kernel_name = 'hybrid_s5_deltanet_gla_moe_dit'


def _cuts(cols):
    return np.cumsum(cols)[:-1].tolist()


def layer_norm(x, g, b):
    xf = x.astype(F32)
    mu = jnp.mean(xf, axis=-1, keepdims=True)
    var = jnp.mean(jnp.square(xf - mu), axis=-1, keepdims=True)
    return ((xf - mu) * lax.rsqrt(var + NORM_EPS) * g.astype(F32) + b.astype(F32)).astype(x.dtype)


def rms_norm(x, w):
    xf = x.astype(F32)
    return xf * lax.rsqrt(jnp.mean(jnp.square(xf), axis=-1, keepdims=True) + NORM_EPS) * w.astype(F32)


def l2_normalize(x):
    return x * lax.rsqrt(jnp.sum(jnp.square(x), axis=-1, keepdims=True) + NORM_EPS)


def modulate(h, shift, scale):
    return h * (1.0 + scale) + shift


def adaln(cond, w, b):
    return jnp.split(jax.nn.silu(cond) @ w + b, N_MOD, axis=-1)


def swiglu(h, w1, w3, w2):
    return (jax.nn.silu(h @ w1) * (h @ w3)) @ w2


def to_chunks(t, size):
    b, n = t.shape[0], t.shape[1]
    t = t.reshape((b, n // size, size) + t.shape[2:])
    return jnp.moveaxis(t, 3, 1)


def from_chunks(o):
    n, b, h, c, d = o.shape
    return jnp.transpose(o, (1, 0, 3, 2, 4)).reshape(b, n * c, h, d)


def run_bidirectional(rule, shared_x, shared_c, dirs_x, dirs_c, s0, need_ctx):
    outs_x, outs_c = [], []
    for d in range(2):
        flip = (lambda t: jnp.flip(t, axis=1)) if d == 1 else (lambda t: t)
        o_c, s_c = rule(*[flip(t) for t in shared_c + dirs_c[d]], s0)
        o_x, _ = rule(*[flip(t) for t in shared_x + dirs_x[d]], s_c)
        outs_x.append(flip(o_x))
        outs_c.append(flip(o_c))
    y_c = outs_c[0] + outs_c[1] if need_ctx else None
    return outs_x[0] + outs_x[1], y_c


def s5_discretize(lam_re, lam_im, log_step, b_re, b_im):
    delta = jnp.exp(log_step)[:, None]
    mag = jnp.exp(lam_re * delta)
    a_re = mag * jnp.cos(lam_im * delta)
    a_im = mag * jnp.sin(lam_im * delta)
    den = jnp.square(lam_re) + jnp.square(lam_im)
    f_re = ((a_re - 1.0) * lam_re + a_im * lam_im) / den
    f_im = (a_im * lam_re - (a_re - 1.0) * lam_im) / den
    bb_re = f_re[..., None] * b_re - f_im[..., None] * b_im
    bb_im = f_re[..., None] * b_im + f_im[..., None] * b_re
    return a_re, a_im, bb_re, bb_im


def linear_recurrence_combine(e1, e2):
    a1r, a1i, h1r, h1i = e1
    a2r, a2i, h2r, h2i = e2
    return (a2r * a1r - a2i * a1i, a2r * a1i + a2i * a1r,
            a2r * h1r - a2i * h1i + h2r, a2r * h1i + a2i * h1r + h2i)


def s5_states(u, a_re, a_im, bb_re, bb_im, h0, reverse):
    bu_re = jnp.einsum('btgh,gph->tbgp', u, bb_re)
    bu_im = jnp.einsum('btgh,gph->tbgp', u, bb_im)
    shape = (u.shape[1], 1) + a_re.shape
    ac_re, ac_im, h_re, h_im = lax.associative_scan(
        linear_recurrence_combine,
        (jnp.broadcast_to(a_re, shape), jnp.broadcast_to(a_im, shape), bu_re, bu_im),
        reverse=reverse, axis=0)
    if h0 is not None:
        h0_re, h0_im = h0
        h_re = h_re + ac_re * h0_re - ac_im * h0_im
        h_im = h_im + ac_re * h0_im + ac_im * h0_re
    return h_re, h_im


def s5_readout(h_re, h_im, c_re, c_im):
    return jnp.einsum('tbgp,ghp->btgh', h_re, c_re) - jnp.einsum('tbgp,ghp->btgh', h_im, c_im)


def s5_mixer(u_x, u_c, gate_x, gate_c, lam_re, lam_im, log_step, b_re, b_im, c_re, c_im, d_skip, need_ctx):
    lam_re, lam_im, log_step, b_re, b_im, c_re, c_im = [
        t.astype(F32) for t in (lam_re, lam_im, log_step, b_re, b_im, c_re, c_im)]

    def groups(u):
        return u.astype(F32).reshape(u.shape[0], u.shape[1], S5_GROUPS, S5_GROUP)

    ux, uc = groups(u_x), groups(u_c)
    ys_x, ys_c = [], []
    for d in range(2):
        rev = d == 1
        a_re, a_im, bb_re, bb_im = s5_discretize(lam_re[d], lam_im[d], log_step[d], b_re[d], b_im[d])
        hc_re, hc_im = s5_states(uc, a_re, a_im, bb_re, bb_im, None, rev)
        last = 0 if rev else -1
        hx_re, hx_im = s5_states(ux, a_re, a_im, bb_re, bb_im, (hc_re[last], hc_im[last]), rev)
        ys_x.append(s5_readout(hx_re, hx_im, c_re[d], c_im[d]))
        if need_ctx:
            ys_c.append(s5_readout(hc_re, hc_im, c_re[d], c_im[d]))

    def glu_out(y, u, gate):
        y = (y + d_skip.astype(F32).reshape(S5_GROUPS, S5_GROUP) * u).reshape(u.shape[0], u.shape[1], S5_WIDTH)
        return (jax.nn.gelu(y) * jax.nn.sigmoid(gate.astype(F32))).astype(gate.dtype)

    y_x = glu_out(ys_x[0] + ys_x[1], ux, gate_x)
    y_c = glu_out(ys_c[0] + ys_c[1], uc, gate_c) if need_ctx else None
    return y_x, y_c


def depthwise_conv2d(x, w, rows):
    b, n, ch = x.shape
    y = lax.conv_general_dilated(x.reshape(b, rows, GRID_W, ch), w[:, :, None, :], window_strides=(1, 1),
                                 padding='SAME', dimension_numbers=('NHWC', 'HWIO', 'NHWC'),
                                 feature_group_count=ch)
    return y.reshape(b, n, ch)


def depthwise_conv1d(x, w):
    ch = x.shape[-1]
    return lax.conv_general_dilated(x, w[:, None, :], window_strides=(1,), padding='SAME',
                                    dimension_numbers=('NWC', 'WIO', 'NWC'), feature_group_count=ch)


def gated_delta_rule(q, k, v, beta, g, s0):
    dv = v.shape[-1]
    q, k, v, beta, g = (to_chunks(t, DN_CHUNK) for t in (q, k, v, beta, g))
    gc = jnp.cumsum(g, axis=-1)
    idx = jnp.arange(DN_CHUNK)
    incl = idx[:, None] >= idx[None, :]
    strict = idx[:, None] > idx[None, :]
    decay = jnp.exp(jnp.where(incl, gc[..., :, None] - gc[..., None, :], -jnp.inf))
    kb = k * beta[..., None]
    m = jnp.eye(DN_CHUNK, dtype=F32) + jnp.where(strict, jnp.einsum('bhnid,bhnjd->bhnij', kb, k) * decay, 0.0)
    rhs = jnp.concatenate([v * beta[..., None], kb * jnp.exp(gc)[..., None]], axis=-1)
    sol = lax.linalg.triangular_solve(m, rhs, left_side=True, lower=True, unit_diagonal=True)
    u, w = sol[..., :dv], sol[..., dv:]
    attn = jnp.einsum('bhnid,bhnjd->bhnij', q, k) * decay
    qg = q * jnp.exp(gc)[..., None]
    kg = k * jnp.exp(gc[..., -1:] - gc)[..., None]
    g_last = jnp.exp(gc[..., -1])

    def step(s, xs):
        u_n, w_n, a_n, q_n, k_n, gl_n = xs
        v_new = u_n - jnp.einsum('bhck,bhkv->bhcv', w_n, s)
        o_n = jnp.einsum('bhck,bhkv->bhcv', q_n, s) + jnp.einsum('bhij,bhjv->bhiv', a_n, v_new)
        s = s * gl_n[..., None, None] + jnp.einsum('bhck,bhcv->bhkv', k_n, v_new)
        return s, o_n

    xs = tuple(jnp.moveaxis(t, 2, 0) for t in (u, w, attn, qg, kg, g_last))
    s_t, o = lax.scan(step, s0, xs)
    return from_chunks(o), s_t


def deltanet_mixer(qkv_x, qkv_c, z_x, z_c, beta_x, beta_c, a_x, a_c, conv_w, a_log, dt_bias, norm_w, need_ctx):
    rows = qkv_x.shape[1] // GRID_W
    qkv_x = jax.nn.silu(depthwise_conv2d(qkv_x, conv_w, rows))
    qkv_c = jax.nn.silu(depthwise_conv1d(qkv_c, conv_w[DN_CONV // 2]))
    a_log = a_log.astype(F32)
    dt_bias = dt_bias.astype(F32)

    def prep(qkv, beta, a):
        b, n = qkv.shape[0], qkv.shape[1]
        q, k, v = [t.reshape(b, n, DN_HEADS, DN_HEAD_DIM) for t in jnp.split(qkv.astype(F32), 3, axis=-1)]
        q = l2_normalize(q) * DN_HEAD_DIM ** -0.5
        k = l2_normalize(k)
        beta = jax.nn.sigmoid(beta.astype(F32))
        a = a.astype(F32)
        dirs = []
        for d in range(2):
            hs = slice(d * DN_HEADS, (d + 1) * DN_HEADS)
            g = -jnp.exp(a_log[d]) * jax.nn.softplus(a[..., hs] + dt_bias[d])
            dirs.append((beta[..., hs], g))
        return (q, k, v), dirs

    shared_x, dirs_x = prep(qkv_x, beta_x, a_x)
    shared_c, dirs_c = prep(qkv_c, beta_c, a_c)
    s0 = jnp.zeros((qkv_x.shape[0], DN_HEADS, DN_HEAD_DIM, DN_HEAD_DIM), F32)
    o_x, o_c = run_bidirectional(gated_delta_rule, shared_x, shared_c, dirs_x, dirs_c, s0, need_ctx)

    def gate_out(o, z):
        b, n = z.shape[0], z.shape[1]
        y = rms_norm(o, norm_w) * jax.nn.silu(z.astype(F32).reshape(b, n, DN_HEADS, DN_HEAD_DIM))
        return y.reshape(b, n, DN_WIDTH).astype(z.dtype)

    return gate_out(o_x, z_x), (gate_out(o_c, z_c) if need_ctx else None)


def even_mixer(hx, hc, w_in, lam_re, lam_im, log_step, b_re, b_im, c_re, c_im, d_skip,
               conv_w, a_log, dt_bias, norm_w, w_out, need_ctx):
    cuts = _cuts(EVEN_COLS)
    qkv_x, z_x, u_x, g_x, beta_x, a_x = jnp.split(hx @ w_in, cuts, axis=-1)
    qkv_c, z_c, u_c, g_c, beta_c, a_c = jnp.split(hc @ w_in, cuts, axis=-1)
    s5_x, s5_c = s5_mixer(u_x, u_c, g_x, g_c, lam_re, lam_im, log_step, b_re, b_im, c_re, c_im, d_skip, need_ctx)
    dn_x, dn_c = deltanet_mixer(qkv_x, qkv_c, z_x, z_c, beta_x, beta_c, a_x, a_c,
                                conv_w, a_log, dt_bias, norm_w, need_ctx)
    y_x = jnp.concatenate([s5_x, dn_x], axis=-1) @ w_out
    y_c = jnp.concatenate([s5_c, dn_c], axis=-1) @ w_out if need_ctx else None
    return y_x, y_c


def gla_rule(q, k, v, glog, s0):
    q, k, v, glog = (to_chunks(t, GLA_CHUNK) for t in (q, k, v, glog))
    cum = jnp.cumsum(glog, axis=3)
    q_in = q * jnp.exp(cum)
    k_in = k * jnp.exp(-cum)
    idx = jnp.arange(GLA_CHUNK)
    incl = idx[:, None] >= idx[None, :]
    attn = jnp.where(incl, jnp.einsum('bhnik,bhnjk->bhnij', q_in, k_in), 0.0)
    k_dec = k * jnp.exp(cum[..., -1:, :] - cum)
    g_last = jnp.exp(cum[..., -1, :])

    def step(s, xs):
        q_n, k_n, v_n, a_n, g_n = xs
        o_n = jnp.einsum('bhck,bhkv->bhcv', q_n, s) + jnp.einsum('bhij,bhjv->bhiv', a_n, v_n)
        s = s * g_n[..., None] + jnp.einsum('bhck,bhcv->bhkv', k_n, v_n)
        return s, o_n

    xs = tuple(jnp.moveaxis(t, 2, 0) for t in (q_in, k_dec, v, attn, g_last))
    s_t, o = lax.scan(step, s0, xs)
    return from_chunks(o), s_t


def odd_mixer(hx, hc, w_in, gate_w2, gate_b, norm_w, w_out, need_ctx):
    cuts = _cuts(ODD_COLS)
    gate_w2 = gate_w2.astype(F32)
    gate_b = gate_b.astype(F32)

    def prep(p):
        b, n = p.shape[0], p.shape[1]
        q, k, v, r, lr = jnp.split(p.astype(F32), cuts, axis=-1)
        q = q.reshape(b, n, GLA_HEADS, GLA_DK) * GLA_DK ** -0.5
        k = k.reshape(b, n, GLA_HEADS, GLA_DK)
        v = v.reshape(b, n, GLA_HEADS, GLA_DV)
        dirs = []
        for d in range(2):
            z = lr[..., d * GLA_RANK:(d + 1) * GLA_RANK] @ gate_w2[d] + gate_b[d]
            dirs.append((jax.nn.log_sigmoid(z).reshape(b, n, GLA_HEADS, GLA_DK) / GLA_TAU,))
        return (q, k, v), dirs, r

    shared_x, dirs_x, r_x = prep(hx @ w_in)
    shared_c, dirs_c, r_c = prep(hc @ w_in)
    s0 = jnp.zeros((hx.shape[0], GLA_HEADS, GLA_DK, GLA_DV), F32)
    o_x, o_c = run_bidirectional(gla_rule, shared_x, shared_c, dirs_x, dirs_c, s0, need_ctx)

    def gate_out(o, r):
        b, n = r.shape[0], r.shape[1]
        y = rms_norm(o, norm_w) * jax.nn.silu(r.reshape(b, n, GLA_HEADS, GLA_DV))
        return y.reshape(b, n, GLA_VAL_WIDTH).astype(hx.dtype) @ w_out

    return gate_out(o_x, r_x), (gate_out(o_c, r_c) if need_ctx else None)


def moe_swiglu(h, w_router, w1, w3, w2):
    n_tok, d_model = h.shape
    n_asg = n_tok * TOP_K
    logits = (h @ w_router).astype(F32)
    top_logit, top_e = lax.top_k(logits, TOP_K)
    gate = jax.nn.softmax(top_logit, axis=-1).astype(h.dtype).reshape(n_asg)
    flat_e = top_e.reshape(n_asg).astype(jnp.int32)
    flat_tok = jnp.repeat(jnp.arange(n_tok, dtype=jnp.int32), TOP_K)
    order = jnp.argsort(flat_e)
    sorted_e, sorted_tok, sorted_gate = flat_e[order], flat_tok[order], gate[order]
    counts = jnp.zeros((N_EXPERTS,), jnp.int32).at[flat_e].add(1)
    padded = (counts + MOE_BLOCK - 1) // MOE_BLOCK * MOE_BLOCK
    start = jnp.cumsum(counts) - counts
    padded_end = jnp.cumsum(padded)
    padded_start = padded_end - padded
    dest = padded_start[sorted_e] + jnp.arange(n_asg, dtype=jnp.int32) - start[sorted_e]
    n_blocks = -(-n_asg // MOE_BLOCK) + N_EXPERTS
    n_rows = n_blocks * MOE_BLOCK
    row_tok = jnp.zeros((n_rows,), jnp.int32).at[dest].set(sorted_tok)
    xb = h[row_tok].reshape(n_blocks, MOE_BLOCK, d_model)
    block_e = jnp.minimum(jnp.searchsorted(padded_end, jnp.arange(n_blocks, dtype=jnp.int32) * MOE_BLOCK,
                                           side='right'), N_EXPERTS - 1)

    def expert_block(args):
        xblk, e = args
        return swiglu(xblk, w1[e], w3[e], w2[e])

    yb = lax.map(expert_block, (xb, block_e)).reshape(n_rows, d_model)
    return jnp.zeros_like(h).at[sorted_tok].add(yb[dest] * sorted_gate[:, None])


def setup_inputs(seed: int = 0):
    key = jax.random.key(seed)
    keys = iter(jax.random.split(key, 40))

    def normal(shape, scale):
        return jax.random.normal(next(keys), shape, F32) * scale

    def uniform(shape, lo, hi):
        return jax.random.uniform(next(keys), shape, F32, lo, hi)

    D = D_MODEL
    s5_shape = (N_EVEN, 2, S5_GROUPS, S5_STATE)
    dn_dt = jnp.exp(uniform((N_EVEN, 2, DN_HEADS), math.log(DN_DT_MIN), math.log(DN_DT_MAX)))
    return {
        'x': normal((BATCH, SEQ, D), 1.0),
        'c': normal((BATCH, D), 1.0),
        'ctx': normal((BATCH, CTX_LEN, D), 1.0),
        'c_ctx': normal((D,), 1.0),
        'ada_w': normal((DEPTH, D, N_MOD * D), 0.5 * D ** -0.5),
        'ada_b': normal((DEPTH, N_MOD * D), 0.02),
        'ln1_g': 1.0 + normal((DEPTH, D), 0.02),
        'ln1_b': normal((DEPTH, D), 0.02),
        'ln2_g': 1.0 + normal((DEPTH, D), 0.02),
        'ln2_b': normal((DEPTH, D), 0.02),
        'e_w_in': normal((N_EVEN, D, EVEN_IN), D ** -0.5),
        's5_lam_re': -0.5 + normal(s5_shape, 0.01),
        's5_lam_im': jnp.pi * jnp.arange(S5_STATE, dtype=F32) + normal(s5_shape, 0.01),
        's5_log_step': uniform((N_EVEN, 2, S5_GROUPS), math.log(S5_DT_MIN), math.log(S5_DT_MAX)),
        's5_b_re': normal((N_EVEN, 2, S5_GROUPS, S5_STATE, S5_GROUP), (2 * S5_GROUP) ** -0.5),
        's5_b_im': normal((N_EVEN, 2, S5_GROUPS, S5_STATE, S5_GROUP), (2 * S5_GROUP) ** -0.5),
        's5_c_re': normal((N_EVEN, 2, S5_GROUPS, S5_GROUP, S5_STATE), 0.5),
        's5_c_im': normal((N_EVEN, 2, S5_GROUPS, S5_GROUP, S5_STATE), 0.5),
        's5_d': normal((N_EVEN, S5_WIDTH), 0.5),
        'dn_conv': normal((N_EVEN, DN_CONV, DN_CONV, 3 * DN_WIDTH), 1.0 / DN_CONV),
        'dn_a_log': jnp.log(uniform((N_EVEN, 2, DN_HEADS), 1.0, 16.0)),
        'dn_dt_bias': dn_dt + jnp.log(-jnp.expm1(-dn_dt)),
        'dn_norm_w': 1.0 + normal((N_EVEN, DN_HEAD_DIM), 0.02),
        'e_w_out': normal((N_EVEN, EVEN_MIX, D), DEEPNORM_BETA * EVEN_MIX ** -0.5),
        'ffn_w1': normal((N_EVEN, D, FFN_HIDDEN), D ** -0.5),
        'ffn_w3': normal((N_EVEN, D, FFN_HIDDEN), D ** -0.5),
        'ffn_w2': normal((N_EVEN, FFN_HIDDEN, D), DEEPNORM_BETA * FFN_HIDDEN ** -0.5),
        'o_w_in': normal((N_ODD, D, ODD_IN), D ** -0.5),
        'gla_w2': normal((N_ODD, 2, GLA_RANK, GLA_KEY_WIDTH), GLA_RANK ** -0.5),
        'gla_b': normal((N_ODD, 2, GLA_KEY_WIDTH), 0.1),
        'gla_norm_w': 1.0 + normal((N_ODD, GLA_DV), 0.02),
        'o_w_out': normal((N_ODD, GLA_VAL_WIDTH, D), DEEPNORM_BETA * GLA_VAL_WIDTH ** -0.5),
        'moe_router': normal((N_ODD, D, N_EXPERTS), D ** -0.5),
        'moe_w1': normal((N_ODD, N_EXPERTS, D, EXPERT_HIDDEN), D ** -0.5),
        'moe_w3': normal((N_ODD, N_EXPERTS, D, EXPERT_HIDDEN), D ** -0.5),
        'moe_w2': normal((N_ODD, N_EXPERTS, EXPERT_HIDDEN, D), DEEPNORM_BETA * EXPERT_HIDDEN ** -0.5),
    }


def reference(x, c, ctx, c_ctx, ada_w, ada_b, ln1_g, ln1_b, ln2_g, ln2_b,
              e_w_in, s5_lam_re, s5_lam_im, s5_log_step, s5_b_re, s5_b_im,
              s5_c_re, s5_c_im, s5_d, dn_conv, dn_a_log, dn_dt_bias, dn_norm_w,
              e_w_out, ffn_w1, ffn_w3, ffn_w2, o_w_in, gla_w2, gla_b, gla_norm_w,
              o_w_out, moe_router, moe_w1, moe_w3, moe_w2):
    n_lat = x.shape[0] * x.shape[1]
    for i in range(DEPTH):
        j = i // 2
        need_ctx = i < DEPTH - 1
        sh1, sc1, gt1, sh2, sc2, gt2 = [m[:, None, :] for m in adaln(c, ada_w[i], ada_b[i])]
        csh1, csc1, cgt1, csh2, csc2, cgt2 = adaln(c_ctx, ada_w[i], ada_b[i])
        hx = modulate(x, sh1, sc1)
        hc = modulate(ctx, csh1, csc1)
        if i % 2 == 0:
            mix_x, mix_c = even_mixer(hx, hc, e_w_in[j], s5_lam_re[j], s5_lam_im[j], s5_log_step[j],
                                      s5_b_re[j], s5_b_im[j], s5_c_re[j], s5_c_im[j], s5_d[j],
                                      dn_conv[j], dn_a_log[j], dn_dt_bias[j], dn_norm_w[j], e_w_out[j], need_ctx)
        else:
            mix_x, mix_c = odd_mixer(hx, hc, o_w_in[j], gla_w2[j], gla_b[j], gla_norm_w[j], o_w_out[j], need_ctx)
        x = layer_norm(DEEPNORM_ALPHA * x + gt1 * mix_x, ln1_g[i], ln1_b[i])
        tokens = [modulate(x, sh2, sc2).reshape(n_lat, D_MODEL)]
        if need_ctx:
            ctx = layer_norm(DEEPNORM_ALPHA * ctx + cgt1 * mix_c, ln1_g[i], ln1_b[i])
            tokens.append(modulate(ctx, csh2, csc2).reshape(-1, D_MODEL))
        tokens = jnp.concatenate(tokens, axis=0)
        if i % 2 == 0:
            f = swiglu(tokens, ffn_w1[j], ffn_w3[j], ffn_w2[j])
        else:
            f = moe_swiglu(tokens, moe_router[j], moe_w1[j], moe_w3[j], moe_w2[j])
        x = layer_norm(DEEPNORM_ALPHA * x + gt2 * f[:n_lat].reshape(x.shape), ln2_g[i], ln2_b[i])
        if need_ctx:
            ctx = layer_norm(DEEPNORM_ALPHA * ctx + cgt2 * f[n_lat:].reshape(ctx.shape), ln2_g[i], ln2_b[i])
    return x
```

```python
from contextlib import ExitStack
import numpy as np
import concourse.bass as bass
import concourse.mybir as mybir
from concourse.bass_utils import run_bass_kernel_spmd

F32 = mybir.dt.float32
F32R = mybir.dt.float32r
ALU = mybir.AluOpType
AF = mybir.ActivationFunctionType
AX = mybir.AxisListType

ENGS = ("pe", "dve", "act", "pool", "sp")
SEM_LIMIT = 20000
NDMA_SEM = 6
D = 2048
KC = 16
ALPHA = float(4 ** 0.25)
EPS = 1e-6
NCORES = 8


class Res:
    __slots__ = ("name", "w", "r")

    def __init__(self, name=""):
        self.name = name
        self.w = None
        self.r = []


class T:
    def __init__(self, t, name):
        self.t = t
        self.res = Res(name)
        self.name = name

    def __getitem__(self, idx):
        return self.t[idx]


class Rot:
    def __init__(self, tiles):
        self.tiles = tiles
        self.i = 0

    def next(self):
        t = self.tiles[self.i]
        self.i = (self.i + 1) % len(self.tiles)
        return t


def _res(x):
    return x.res if isinstance(x, T) else x


class Prog:
    def __init__(self, nc):
        self.nc = nc
        self.es = ExitStack()
        self.q = {e: [] for e in ENGS}
        self.sems = {}
        self.cur = {}
        self.nsem = 0
        for e in ENGS:
            self._new_epoch(e)
        self.seen = {e: {} for e in ENGS}
        self.dma_sems = {}
        self.dma_cnt = {}
        self.dma_rr = {}
        self.n_inst = 0
        self.final_waits = []
        self.phase = None
        self.pending = {e: [] for e in ENGS}

    def _alloc_sem(self, name):
        h = self.es.enter_context(self.nc.semaphore(name))
        self.sems[name] = h
        self.nsem += 1
        return name

    def _new_epoch(self, e):
        k = self._alloc_sem(f"s_{e}_{self.nsem}")
        self.cur[e] = [k, 0]

    def sb(self, name, shape, dt=F32):
        st = self.phase if self.phase is not None else self.es
        t = st.enter_context(self.nc.sbuf_tensor(name, list(shape), dt))
        return T(t, name)

    def begin_phase(self):
        self.phase = ExitStack()

    def end_phase(self):
        self.barrier()
        self.phase.close()
        self.phase = None

    def barrier(self):
        toks = []
        for e in ENGS:
            k, c = self.cur[e]
            if c > 0:
                toks.append((k, c))
        for e, sems in self.dma_sems.items():
            for i, k in enumerate(sems):
                if self.dma_cnt[e][i] > 0:
                    toks.append((k, self.dma_cnt[e][i]))
        for e in ENGS:
            for (k, v) in toks:
                if k == self.cur[e][0]:
                    continue
                if self.seen[e].get(k, 0) < v:
                    self.seen[e][k] = v
                    self.pending[e].append((k, v))

    def ps(self, name, shape, dt=F32):
        t = self.es.enter_context(self.nc.psum_tensor(name, list(shape), dt))
        return T(t, name)

    def rot_sb(self, name, n, shape, dt=F32):
        return Rot([self.sb(f"{name}{i}", shape, dt) for i in range(n)])

    def rot_ps(self, name, n, shape, dt=F32):
        return Rot([self.ps(f"{name}{i}", shape, dt) for i in range(n)])

    def _deps(self, eng, reads, writes):
        need = {}

        def add(tok):
            if tok is None:
                return
            k, v = tok
            if need.get(k, 0) < v:
                need[k] = v
        for r in reads:
            add(_res(r).w)
        for w in writes:
            w = _res(w)
            add(w.w)
            for t in w.r:
                add(t)
        waits = []
        seen = self.seen[eng]
        own = self.cur[eng][0]
        for k, v in need.items():
            if eng == "pe" and k == own:
                continue
            if seen.get(k, 0) < v:
                seen[k] = v
                waits.append((k, v))
        return waits

    def _commit(self, tok, reads, writes):
        for r in reads:
            r = _res(r)
            r.r.append(tok)
            if len(r.r) > 48:
                d = {}
                for k, v in r.r:
                    if d.get(k, 0) < v:
                        d[k] = v
                r.r = list(d.items())
        for w in writes:
            w = _res(w)
            w.w = tok
            w.r = []

    def op(self, eng, fn, reads=(), writes=(), inc=True):
        waits = self._deps(eng, reads, writes)
        if self.pending[eng]:
            waits = self.pending[eng] + waits
            self.pending[eng] = []
        cur = self.cur[eng]
        tok = (cur[0], cur[1] + 1)
        if inc:
            cur[1] += 1
        self.q[eng].append((waits, fn, (cur[0], 1) if inc else None))
        self._commit(tok, reads, writes)
        self.n_inst += 1
        if inc and cur[1] >= SEM_LIMIT:
            self._new_epoch(eng)
        return tok

    def dma(self, eng, out_ap, in_ap, reads=(), writes=(), **kw):
        waits = self._deps(eng, reads, writes)
        if self.pending[eng]:
            waits = self.pending[eng] + waits
            self.pending[eng] = []
        if eng not in self.dma_sems:
            self.dma_sems[eng] = [self._alloc_sem(f"d_{eng}_{i}") for i in range(NDMA_SEM)]
            self.dma_cnt[eng] = [0] * NDMA_SEM
            self.dma_rr[eng] = 0
        i = self.dma_rr[eng]
        self.dma_rr[eng] = (i + 1) % NDMA_SEM
        self.dma_cnt[eng][i] += 16
        k = self.dma_sems[eng][i]
        tok = (k, self.dma_cnt[eng][i])

        def fn(e, out_ap=out_ap, in_ap=in_ap, kw=kw):
            return e.dma_start(out=out_ap, in_=in_ap, **kw)
        self.q[eng].append((waits, fn, (k, 16)))
        self._commit(tok, reads, writes)
        self.n_inst += 1
        return tok

    def wait_final(self, eng, toks):
        self.final_waits.append((eng, list(toks)))

    def check(self):
        cnt = {}
        ptr = {e: 0 for e in ENGS}
        prog = True
        while prog:
            prog = False
            for e in ENGS:
                q = self.q[e]
                while ptr[e] < len(q):
                    waits, fn, inc = q[ptr[e]]
                    if all(cnt.get(k, 0) >= v for k, v in waits):
                        if inc is not None:
                            cnt[inc[0]] = cnt.get(inc[0], 0) + inc[1]
                        ptr[e] += 1
                        prog = True
                    else:
                        break
        stuck = {e: (ptr[e], len(self.q[e])) for e in ENGS if ptr[e] < len(self.q[e])}
        if stuck:
            for e in stuck:
                waits, fn, inc = self.q[e][ptr[e]]
                print("STUCK", e, stuck[e], [(k, v, cnt.get(k, 0)) for k, v in waits])
        return not stuck

    def finish(self):
        nc = self.nc
        engmap = {"pe": "tensor", "dve": "vector", "act": "scalar", "pool": "gpsimd", "sp": "sync"}
        fin = {e: [] for e in ENGS}
        for e, toks in self.final_waits:
            fin[e].extend(toks)
        with nc.Block() as block:
            for e in ENGS:
                items = self.q[e]
                fw = fin[e]
                if not items and not fw:
                    continue

                def body(engine, items=items, fw=fw):
                    for waits, fn, inc in items:
                        for k, v in waits:
                            engine.wait_ge(self.sems[k], v)
                        ins = fn(engine)
                        if inc is not None:
                            ins.then_inc(self.sems[inc[0]], inc[1])
                    for k, v in fw:
                        engine.wait_ge(self.sems[k], v)
                getattr(block, engmap[e])(body)
        self.es.close()


def mm(p, out, lhsT, rhs, start, stop, reads, writes):
    p.op("pe", lambda e: e.matmul(out, lhsT, rhs, start=start, stop=stop), reads=reads, writes=writes, inc=True)


def act(p, out, in_, func, reads, writes, **kw):
    p.op("act", lambda e: e.activation(out=out, in_=in_, func=func, **kw), reads=reads, writes=writes)


def tt(p, out, in0, in1, op, reads, writes, eng="dve"):
    p.op(eng, lambda e: e.tensor_tensor(out=out, in0=in0, in1=in1, op=op), reads=reads, writes=writes)


def ts(p, out, in0, s1, s2, op0, op1, reads, writes, eng="dve"):
    if op1 is None:
        p.op(eng, lambda e: e.tensor_scalar(out, in0, s1, None, op0=op0), reads=reads, writes=writes)
    else:
        p.op(eng, lambda e: e.tensor_scalar(out, in0, s1, s2, op0=op0, op1=op1), reads=reads, writes=writes)


def stt(p, out, in0, scalar, in1, op0, op1, reads, writes):
    p.op("dve", lambda e: e.scalar_tensor_tensor(out=out, in0=in0, scalar=scalar, in1=in1, op0=op0, op1=op1),
         reads=reads, writes=writes)


def dram_in(nc, name, shape, dt=F32):
    return nc.dram_tensor(name, list(shape), dt, kind="ExternalInput").ap()


def dram_out(nc, name, shape, dt=F32):
    return nc.dram_tensor(name, list(shape), dt, kind="ExternalOutput").ap()


def blk(W):
    Din, Dout = W.shape
    kc, m = Din // 128, Dout // 128
    return np.ascontiguousarray(W.reshape(kc, 128, m, 128).transpose(2, 1, 0, 3)).reshape(m, 128, kc * 128)


def colT(v):
    return np.ascontiguousarray(v.reshape(-1, 128).T)


NB_A = 2 * 6 * D // 128 // NCORES


def build_mods():
    nc = bass.Bass("TRN2", target_bir_lowering=False)
    condT = dram_in(nc, "condT", [128, KC * 6])
    wb = dram_in(nc, "wb", [NB_A, 128, D], F32R)
    bias = dram_in(nc, "bias", [128, NB_A])
    out = dram_out(nc, "out", [128, NB_A * 6])
    p = Prog(nc)
    c_sb = p.sb("c_sb", [128, KC * 6])
    c_r = p.sb("c_r", [128, KC * 6], F32R)
    b_sb = p.sb("b_sb", [128, NB_A])
    o_sb = p.sb("o_sb", [128, NB_A * 6])
    wt = p.rot_sb("wt", 3, [128, D], F32R)
    pst = p.rot_ps("ps", 4, [128, 512])
    p.dma("sp", c_sb[:], condT, writes=[c_sb])
    p.dma("sp", b_sb[:], bias, writes=[b_sb])
    act(p, c_r[:], c_sb[:], AF.Silu, [c_sb], [c_r])
    for m in range(NB_A):
        w = wt.next()
        p.dma("pool", w[:], wb[m], writes=[w])
        ps = pst.next()
        for kc in range(KC):
            mm(p, ps[:, 0:6], w[:, kc * 128:(kc + 1) * 128], c_r[:, kc * 6:(kc + 1) * 6], kc == 0, kc == KC - 1, [w, c_r], [ps])
        act(p, o_sb[:, m * 6:(m + 1) * 6], ps[:, 0:6], AF.Identity, [ps, b_sb], [o_sb], bias=b_sb[:, m:m + 1])
    t = p.dma("sp", out, o_sb[:], reads=[o_sb])
    p.wait_final("sp", [t])
    p.finish()
    return nc


def run_mods(inp):
    c, c_ctx = inp["c"], inp["c_ctx"]
    cond = np.zeros((6, D), np.float32)
    cond[:4] = c
    cond[4] = c_ctx
    condT = np.ascontiguousarray(cond.T.reshape(KC, 128, 6).transpose(1, 0, 2)).reshape(128, KC * 6)
    W = np.concatenate([inp["ada_w"][0], inp["ada_w"][1]], axis=1)
    B = np.concatenate([inp["ada_b"][0], inp["ada_b"][1]], axis=0)
    Wb = blk(W)
    Bc = colT(B)
    nc = build_mods()
    in_maps = []
    for k in range(NCORES):
        in_maps.append({"condT": condT, "wb": np.ascontiguousarray(Wb[k * NB_A:(k + 1) * NB_A]),
                        "bias": np.ascontiguousarray(Bc[:, k * NB_A:(k + 1) * NB_A])})
    res = run_bass_kernel_spmd(nc, in_maps, core_ids=list(range(NCORES)))
    o = np.stack([r["out"].reshape(128, NB_A, 6) for r in res.results], axis=0)
    full = o.transpose(3, 0, 2, 1).reshape(6, 2 * 6 * D)
    mods = full.reshape(6, 2, 6, D)
    return mods


def modcols(mods, layer, row):
    return np.ascontiguousarray(np.concatenate([colT(mods[row, layer, i]) for i in range(6)], axis=1))


NP_MAX = 512


def build_ffn(segs, E, HC, moe, mode="full"):
    nc = bass.Bass("TRN2", target_bir_lowering=False)
    NT = sum(segs)
    nseg = len(segs)
    do_pre = mode in ("full", "pre")
    do_ffn = mode in ("full", "exp")
    do_post = mode in ("full", "post")
    if do_pre:
        mixT = dram_in(nc, "mixT", [D, NT], F32R)
        xT = dram_in(nc, "xT", [D, NT])
        woutb = dram_in(nc, "woutb", [KC, 128, D], F32R)
    if mode != "exp":
        modv = dram_in(nc, "modv", [128, nseg * 96])
        lnp = dram_in(nc, "lnp", [128, 64])
    if do_ffn:
        w1b = dram_in(nc, "w1b", [E * HC, 128, D], F32R)
        w3b = dram_in(nc, "w3b", [E * HC, 128, D], F32R)
        w2 = dram_in(nc, "w2", [E * HC * 128, D], F32R)
    if mode == "pre":
        router = dram_in(nc, "router", [128, KC * 8])
        ident_d = dram_in(nc, "ident", [128, 128])
        xl_out = dram_out(nc, "xl_out", [D, NT])
        h_out = dram_out(nc, "h_out", [D, NT])
        g_out = dram_out(nc, "g_out", [8, NT])
    if mode == "exp":
        hT_in = dram_in(nc, "hT_in", [D, NT], F32R)
        grow = dram_in(nc, "grow", [128, NT])
        y_out = dram_out(nc, "y_out", [D, NT])
    if mode == "post":
        parts = dram_in(nc, "parts", [8 * D, NT])
        xl_in = dram_in(nc, "xl_in", [D, NT])
    if do_post:
        xoT = dram_out(nc, "xoT", [D, NT])
    p = Prog(nc)

    A = p.sb("A", [128, KC, NP_MAX]) if mode != "pre" else None
    C = p.sb("C", [128, KC, NP_MAX], F32R) if mode != "exp" else None
    Dl = p.sb("Dl", [128, KC, NP_MAX]) if mode != "exp" else None
    H = p.sb("H", [128, KC, NP_MAX], F32R)
    ones = p.sb("ones", [128, 128], F32R)
    wts = p.rot_sb("wts", {"full": 6, "exp": 6, "pre": 3, "post": 1}[mode], [128, D], F32R)
    sq = p.rot_sb("sq", 2, [128, NP_MAX], F32R)
    tmp = p.rot_sb("tmp", 3, [128, NP_MAX])
    a2 = p.rot_sb("a2", 2, [128, NP_MAX], F32R)
    xin = p.rot_sb("xin", 2, [128, NP_MAX])
    xout = p.rot_sb("xout", 2, [128, NP_MAX])
    st_m = p.sb("st_m", [128, NP_MAX])
    st_r = p.sb("st_r", [128, NP_MAX])
    st_n = p.sb("st_n", [128, NP_MAX])
    pst = p.rot_ps("ps", 8, [128, 512])
    ones_f = p.sb("ones_f", [128, 128])
    p.op("dve", lambda e: e.memset(ones_f[:], 1.0 / D), writes=[ones_f])
    act(p, ones[:], ones_f[:], AF.Copy, [ones_f], [ones])
    if mode != "exp":
        mod_sb = p.sb("mod_sb", [128, nseg * 96])
        ln_sb = p.sb("ln_sb", [128, 64])
        p.dma("sp", mod_sb[:], modv, writes=[mod_sb])
        p.dma("sp", ln_sb[:], lnp, writes=[ln_sb])
        for s in range(nseg):
            for mi in (1, 4):
                o = s * 96 + mi * 16
                ts(p, mod_sb[:, o:o + 16], mod_sb[:, o:o + 16], 1.0, None, ALU.add, None, [mod_sb], [mod_sb])
    if mode == "pre":
        r_sb = p.sb("r_sb", [128, KC * 8])
        ident = p.sb("ident_sb", [128, 128])
        p.dma("sp", r_sb[:], router, writes=[r_sb])
        p.dma("sp", ident[:], ident_d, writes=[ident])
        gT = p.sb("gT", [8, NP_MAX])
        sm = p.rot_sb("sm", 2, [128, 40])
    if mode == "exp":
        G = p.rot_sb("G", 2, [128, NP_MAX])

    def mcol(s, mi, f):
        o = s * 96 + mi * 16 + f
        return mod_sb[:, o:o + 1]

    def lcol(li, f):
        return ln_sb[:, li * 16 + f:li * 16 + f + 1]

    def ln_stats(src, n):
        mps = pst.next()
        eps_ = pst.next()
        for kc in range(KC):
            s_ = sq.next()
            act(p, s_[:, :n], src[:, kc, :n], AF.Square, [src], [s_])
            mm(p, mps[:, :n], ones[:], src[:, kc, :n], kc == 0, kc == KC - 1, [ones, src], [mps])
            mm(p, eps_[:, :n], ones[:], s_[:, :n], kc == 0, kc == KC - 1, [ones, s_], [eps_])
        act(p, st_m[:, :n], mps[:, :n], AF.Copy, [mps], [st_m])
        tt(p, st_n[:, :n], st_m[:, :n], st_m[:, :n], ALU.mult, [st_m], [st_n])
        tt(p, st_r[:, :n], eps_[:, :n], st_n[:, :n], ALU.subtract, [eps_, st_n], [st_r])
        ts(p, st_r[:, :n], st_r[:, :n], EPS, None, ALU.add, None, [st_r], [st_r])
        act(p, st_r[:, :n], st_r[:, :n], AF.Sqrt, [st_r], [st_r])
        p.op("dve", lambda e: e.reciprocal(st_r[:, :n], st_r[:, :n]), reads=[st_r], writes=[st_r])
        stt(p, st_n[:, :n], st_m[:, :n], -1.0, st_r[:, :n], ALU.mult, ALU.mult, [st_m, st_r], [st_n])

    C_f = C.t[:].bitcast(F32) if C is not None else None
    H_f = H.t[:].bitcast(F32)
    out_toks = []
    t_base = 0
    for s, ns in enumerate(segs):
        for t0 in range(0, ns, NP_MAX):
            n = min(NP_MAX, ns - t0)
            g0 = t_base + t0
            if do_pre:
                p.dma("pool", H[:, :, :n], mixT[:, g0:g0 + n].rearrange("(kc q) n -> q kc n", q=128), writes=[H])
                for f in range(KC):
                    w = wts.next()
                    p.dma("pool", w[:], woutb[f], writes=[w])
                    xi = xin.next()
                    p.dma("sp", xi[:, :n], xT[f * 128:(f + 1) * 128, g0:g0 + n], writes=[xi])
                    ps = pst.next()
                    for kc in range(KC):
                        mm(p, ps[:, :n], w[:, kc * 128:(kc + 1) * 128], H[:, kc, :n], kc == 0, kc == KC - 1, [w, H], [ps])
                    act(p, xi[:, :n], xi[:, :n], AF.Copy, [xi], [xi], scale=ALPHA)
                    stt(p, C[:, f, :n], ps[:, :n], mcol(s, 2, f), xi[:, :n], ALU.mult, ALU.add, [ps, xi, mod_sb], [C])
                ln_stats(C, n)
                for f in range(KC):
                    tm = tmp.next()
                    tt(p, tm[:, :n], C_f[:, f, :n], st_r[:, :n], ALU.mult, [C, st_r], [tm])
                    tt(p, tm[:, :n], tm[:, :n], st_n[:, :n], ALU.add, [tm, st_n], [tm])
                    ts(p, Dl[:, f, :n], tm[:, :n], lcol(0, f), lcol(1, f), ALU.mult, ALU.add, [tm, ln_sb], [Dl])
                    ts(p, H[:, f, :n], Dl[:, f, :n], mcol(s, 4, f), mcol(s, 3, f), ALU.mult, ALU.add, [Dl, mod_sb], [H])
            if mode == "pre":
                for st in range(n // 128):
                    lps = pst.next()
                    for kc in range(KC):
                        mm(p, lps[:, 0:8], H_f[:, kc, st * 128:(st + 1) * 128], r_sb[:, kc * 8:(kc + 1) * 8], kc == 0, kc == KC - 1, [H, r_sb], [lps])
                    m_ = sm.next()
                    lg, mx, ex, nm, sv = m_[:, 0:8], m_[:, 8:16], m_[:, 16:24], m_[:, 24:25], m_[:, 25:26]
                    mk = m_[:, 32:40]
                    act(p, lg, lps[:, 0:8], AF.Copy, [lps], [m_])
                    p.op("dve", lambda e, mx=mx, lg=lg: e.max(out=mx, in_=lg), reads=[m_], writes=[m_])
                    ts(p, nm, mx[:, 0:1], -1.0, None, ALU.mult, None, [m_], [m_])
                    act(p, ex, lg, AF.Exp, [m_], [m_], bias=nm)
                    ts(p, mk, lg, mx[:, 1:2], None, ALU.is_ge, None, [m_], [m_])
                    tt(p, ex, ex, mk, ALU.mult, [m_], [m_])
                    p.op("dve", lambda e, sv=sv, ex=ex: e.reduce_sum(out=sv, in_=ex, axis=AX.X), reads=[m_], writes=[m_])
                    p.op("dve", lambda e, sv=sv: e.reciprocal(sv, sv), reads=[m_], writes=[m_])
                    ts(p, ex, ex, sv, None, ALU.mult, None, [m_], [m_])
                    tps = pst.next()
                    mm(p, tps[0:8, 0:128], ex, ident[:], True, True, [m_, ident], [tps])
                    act(p, gT[:, st * 128:(st + 1) * 128], tps[0:8, 0:128], AF.Copy, [tps], [gT])
                out_toks.append(p.dma("sp", g_out[:, g0:g0 + n], gT[:, :n], reads=[gT]))
                out_toks.append(p.dma("sp", xl_out[:, g0:g0 + n].rearrange("(kc q) n -> q kc n", q=128), Dl[:, :, :n], reads=[Dl]))
                out_toks.append(p.dma("sp", h_out[:, g0:g0 + n].rearrange("(kc q) n -> q kc n", q=128), H_f[:, :, :n], reads=[H]))
            if mode == "exp":
                p.dma("pool", H[:, :, :n], hT_in[:, g0:g0 + n].rearrange("(kc q) n -> q kc n", q=128), writes=[H])
                Ge = G.next()
                p.dma("sp", Ge[:, :n], grow[:, g0:g0 + n], writes=[Ge])
            if do_ffn:
                first = True
                for e_ in range(E):
                    for j in range(HC):
                        w1t, w3t, w2t = wts.next(), wts.next(), wts.next()
                        p.dma("pool", w1t[:], w1b[e_ * HC + j], writes=[w1t])
                        p.dma("pool", w3t[:], w3b[e_ * HC + j], writes=[w3t])
                        p.dma("pool", w2t[:], w2[(e_ * HC + j) * 128:(e_ * HC + j + 1) * 128, :], writes=[w2t])
                        h1, h3 = pst.next(), pst.next()
                        for kc in range(KC):
                            mm(p, h1[:, :n], w1t[:, kc * 128:(kc + 1) * 128], H[:, kc, :n], kc == 0, kc == KC - 1, [w1t, H], [h1])
                        for kc in range(KC):
                            mm(p, h3[:, :n], w3t[:, kc * 128:(kc + 1) * 128], H[:, kc, :n], kc == 0, kc == KC - 1, [w3t, H], [h3])
                        s1 = tmp.next()
                        act(p, s1[:, :n], h1[:, :n], AF.Silu, [h1], [s1])
                        a_ = a2.next()
                        if mode == "exp":
                            tt(p, s1[:, :n], s1[:, :n], h3[:, :n], ALU.mult, [s1, h3], [s1])
                            tt(p, a_[:, :n], s1[:, :n], Ge[:, :n], ALU.mult, [s1, Ge], [a_])
                        else:
                            tt(p, a_[:, :n], s1[:, :n], h3[:, :n], ALU.mult, [s1, h3], [a_])
                        for f in range(KC):
                            yp = pst.next()
                            mm(p, yp[:, :n], w2t[:, f * 128:(f + 1) * 128], a_[:, :n], True, True, [w2t, a_], [yp])
                            if first:
                                p.op("dve", lambda e, f=f, yp=yp, n=n: e.tensor_copy(A[:, f, :n], yp[:, :n]), reads=[yp], writes=[A])
                            else:
                                tt(p, A[:, f, :n], A[:, f, :n], yp[:, :n], ALU.add, [A, yp], [A])
                        first = False
            if mode == "exp":
                out_toks.append(p.dma("sp", y_out[:, g0:g0 + n].rearrange("(kc q) n -> q kc n", q=128), A[:, :, :n], reads=[A]))
            if mode == "post":
                p.dma("sp", Dl[:, :, :n], xl_in[:, g0:g0 + n].rearrange("(kc q) n -> q kc n", q=128), writes=[Dl])
                for e_ in range(8):
                    if e_ == 0:
                        p.dma("sp", A[:, :, :n], parts[0:D, g0:g0 + n].rearrange("(kc q) n -> q kc n", q=128), writes=[A])
                    else:
                        for hf in range(2):
                            pt = xin.next()
                        for kc in range(KC):
                            pt = xin.next()
                            p.dma("sp", pt[:, :n], parts[e_ * D + kc * 128:e_ * D + (kc + 1) * 128, g0:g0 + n], writes=[pt])
                            tt(p, A[:, kc, :n], A[:, kc, :n], pt[:, :n], ALU.add, [A, pt], [A])
            if do_post:
                for f in range(KC):
                    tm = tmp.next()
                    act(p, tm[:, :n], Dl[:, f, :n], AF.Copy, [Dl], [tm], scale=ALPHA)
                    stt(p, C[:, f, :n], A[:, f, :n], mcol(s, 5, f), tm[:, :n], ALU.mult, ALU.add, [A, tm, mod_sb], [C])
                ln_stats(C, n)
                for f in range(KC):
                    tm = tmp.next()
                    tt(p, tm[:, :n], C_f[:, f, :n], st_r[:, :n], ALU.mult, [C, st_r], [tm])
                    tt(p, tm[:, :n], tm[:, :n], st_n[:, :n], ALU.add, [tm, st_n], [tm])
                    xo = xout.next()
                    ts(p, xo[:, :n], tm[:, :n], lcol(2, f), lcol(3, f), ALU.mult, ALU.add, [tm, ln_sb], [xo])
                    out_toks.append(p.dma("sp", xoT[f * 128:(f + 1) * 128, g0:g0 + n], xo[:, :n], reads=[xo]))
        t_base += ns
    p.wait_final("sp", out_toks[-NDMA_SEM:])
    p.finish()
    return nc


_IDENT = np.eye(128, dtype=np.float32)


def run_ffn(layer, mix_x, mix_c, x, ctx, mods, inp):
    lnp = np.ascontiguousarray(np.concatenate([colT(inp["ln1_g"][layer]), colT(inp["ln1_b"][layer]),
                                               colT(inp["ln2_g"][layer]), colT(inp["ln2_b"][layer])], axis=1))
    cores = list(range(NCORES))
    if layer == 0:
        segs = [128, 2048]
        E, HC = 1, 44
        w1b, w3b = blk(inp["ffn_w1"][0]), blk(inp["ffn_w3"][0])
        w2f = np.ascontiguousarray(inp["ffn_w2"][0])
        woutb = blk(inp["e_w_out"][0])
        nc = build_ffn(segs, E, HC, False, "full")
        in_maps = []
        for k in cores:
            b, hf = k // 2, k % 2
            in_maps.append({
                "mixT": np.ascontiguousarray(np.concatenate([mix_c[b, hf * 128:(hf + 1) * 128], mix_x[b, hf * 2048:(hf + 1) * 2048]], axis=0).T),
                "xT": np.ascontiguousarray(np.concatenate([ctx[b, hf * 128:(hf + 1) * 128], x[b, hf * 2048:(hf + 1) * 2048]], axis=0).T),
                "modv": np.ascontiguousarray(np.concatenate([modcols(mods, 0, 4), modcols(mods, 0, b)], axis=1)),
                "lnp": lnp, "woutb": woutb, "w1b": w1b, "w3b": w3b, "w2": w2f})
        res = run_bass_kernel_spmd(nc, in_maps, core_ids=cores)
        x_new = np.empty_like(x)
        c_new = np.empty_like(ctx)
        for k in cores:
            b, hf = k // 2, k % 2
            o = res.results[k]["xoT"]
            x_new[b, hf * 2048:(hf + 1) * 2048] = o[:, 128:].T
            c_new[b, hf * 128:(hf + 1) * 128] = o[:, :128].T
        return x_new, c_new
    woutb = blk(inp["o_w_out"][0])
    r = inp["moe_router"][0]
    router = np.ascontiguousarray(r.reshape(KC, 128, 8).transpose(1, 0, 2)).reshape(128, KC * 8)
    nc = build_ffn([2048], 0, 0, True, "pre")
    in_maps = []
    for k in cores:
        b, hf = k // 2, k % 2
        in_maps.append({"mixT": np.ascontiguousarray(mix_x[b, hf * 2048:(hf + 1) * 2048].T),
                        "xT": np.ascontiguousarray(x[b, hf * 2048:(hf + 1) * 2048].T),
                        "modv": modcols(mods, 1, b), "lnp": lnp, "woutb": woutb, "router": router, "ident": _IDENT})
    res = run_bass_kernel_spmd(nc, in_maps, core_ids=cores)
    xl = [res.results[k]["xl_out"] for k in cores]
    hT_all = np.ascontiguousarray(np.concatenate([res.results[k]["h_out"] for k in cores], axis=1))
    g_all = np.concatenate([res.results[k]["g_out"] for k in cores], axis=1)
    NTA = hT_all.shape[1]
    nc = build_ffn([NTA], 1, 56, True, "exp")
    in_maps = []
    for e in cores:
        in_maps.append({"hT_in": hT_all, "grow": np.ascontiguousarray(np.broadcast_to(g_all[e], (128, NTA))),
                        "w1b": blk(inp["moe_w1"][0][e]), "w3b": blk(inp["moe_w3"][0][e]),
                        "w2": np.ascontiguousarray(inp["moe_w2"][0][e])})
    res = run_bass_kernel_spmd(nc, in_maps, core_ids=cores)
    ys = [res.results[e]["y_out"] for e in cores]
    nc = build_ffn([2048], 0, 0, True, "post")
    in_maps = []
    for k in cores:
        b, hf = k // 2, k % 2
        in_maps.append({"parts": np.ascontiguousarray(np.concatenate([ys[e][:, k * 2048:(k + 1) * 2048] for e in cores], axis=0)),
                        "xl_in": xl[k], "modv": modcols(mods, 1, b), "lnp": lnp})
    res = run_bass_kernel_spmd(nc, in_maps, core_ids=cores)
    x_new = np.empty_like(x)
    for k in cores:
        b, hf = k // 2, k % 2
        x_new[b, hf * 2048:(hf + 1) * 2048] = res.results[k]["xoT"].T
    return x_new, None


T_CTX = 256
T_LAT = 4096
T_ALL = T_CTX + T_LAT
GC = 128
NCH = T_ALL // GC


def _tri_masks():
    j = np.arange(128)[:, None]
    i = np.arange(128)[None, :]
    return np.concatenate([(i >= j).astype(np.float32), (i <= j).astype(np.float32)], axis=1)


def build_gla():
    nc = bass.Bass("TRN2", target_bir_lowering=False)
    xT = dram_in(nc, "xT", [D, T_ALL])
    modv = dram_in(nc, "modv", [128, 2 * 32])
    wfm = dram_in(nc, "wfm", [16, 128, D], F32R)
    wv = dram_in(nc, "wv", [2, 128, KC * 512], F32R)
    wlr = dram_in(nc, "wlr", [128, 2 * KC * 16], F32R)
    gw2 = dram_in(nc, "gw2", [16, 2 * 512], F32R)
    gb = dram_in(nc, "gb", [128, 8])
    nw = dram_in(nc, "nw", [128, 4])
    cst = dram_in(nc, "cst", [128, 512])
    out = dram_out(nc, "out", [1024, T_LAT])
    q_s = nc.dram_tensor("q_s", [512, T_ALL], F32, kind="Internal").ap()
    k_s = nc.dram_tensor("k_s", [512, T_ALL], F32, kind="Internal").ap()
    r_s = nc.dram_tensor("r_s", [1024, T_ALL], F32, kind="Internal").ap()
    g_s = nc.dram_tensor("g_s", [2, 512, T_ALL], F32, kind="Internal").ap()
    v_s = nc.dram_tensor("v_s", [T_ALL, 1024], F32, kind="Internal").ap()
    o_s = nc.dram_tensor("o_s", [512, T_LAT], F32, kind="Internal").ap()
    RQ, RK, RR, RG, RV, RO = Res("q_s"), Res("k_s"), Res("r_s"), Res("g_s"), Res("v_s"), Res("o_s")
    p = Prog(nc)

    mod_sb = p.sb("mod_sb", [128, 64])
    gb_sb = p.sb("gb_sb", [128, 8])
    nw_sb = p.sb("nw_sb", [128, 4])
    cst_sb = p.sb("cst_sb", [128, 512])
    cst_r = p.sb("cst_r", [128, 512], F32R)
    wlr_sb = p.sb("wlr_sb", [128, 2 * KC * 16], F32R)
    gw2_sb = p.sb("gw2_sb", [16, 1024], F32R)
    pst = p.rot_ps("ps", 8, [128, 512])
    p.dma("sp", mod_sb[:], modv, writes=[mod_sb])
    p.dma("sp", gb_sb[:], gb, writes=[gb_sb])
    p.dma("sp", nw_sb[:], nw, writes=[nw_sb])
    p.dma("sp", cst_sb[:], cst, writes=[cst_sb])
    p.dma("pool", wlr_sb[:], wlr, writes=[wlr_sb])
    p.dma("pool", gw2_sb[:], gw2, writes=[gw2_sb])
    act(p, cst_r[:], cst_sb[:], AF.Copy, [cst_sb], [cst_r])
    ts(p, gb_sb[:], gb_sb[:], -1.0, None, ALU.mult, None, [gb_sb], [gb_sb])
    for s in range(2):
        ts(p, mod_sb[:, s * 32 + 16:s * 32 + 32], mod_sb[:, s * 32 + 16:s * 32 + 32], 1.0, None, ALU.add, None, [mod_sb], [mod_sb])
    maskf, maskr = cst_sb[:, 0:128], cst_sb[:, 128:256]
    ident_r = cst_r[:, 256:384]
    ones_f = cst_sb[:, 384:512]

    xt = p.sb("xt", [128, KC, 512])
    hT = p.sb("hT", [128, KC, 512], F32R)
    wblk = p.rot_sb("wblk", 3, [128, D], F32R)
    wv_sb = p.sb("wv_sb", [128, KC * 512], F32R)
    ev = p.rot_sb("ev", 3, [128, 512])
    lr_sb = p.rot_sb("lr", 2, [16, 512], F32R)
    tiles = [(0, 0, T_CTX)] + [(1, T_CTX + i * 512, 512) for i in range(T_LAT // 512)]
    for (s, t0, n) in tiles:
        p.dma("sp", xt[:, :, :n], xT[:, t0:t0 + n].rearrange("(kc q) n -> q kc n", q=128), writes=[xt])
        for kc in range(KC):
            ts(p, hT[:, kc, :n], xt[:, kc, :n], mod_sb[:, s * 32 + 16 + kc:s * 32 + 17 + kc], mod_sb[:, s * 32 + kc:s * 32 + kc + 1],
               ALU.mult, ALU.add, [xt, mod_sb], [hT])
        for m in range(16):
            w = wblk.next()
            p.dma("pool", w[:], wfm[m], writes=[w])
            ps = pst.next()
            for kc in range(KC):
                mm(p, ps[:, :n], w[:, kc * 128:(kc + 1) * 128], hT[:, kc, :n], kc == 0, kc == KC - 1, [w, hT], [ps])
            e_ = ev.next()
            act(p, e_[:, :n], ps[:, :n], AF.Copy, [ps], [e_])
            if m < 4:
                dst, R_ = q_s[m * 128:(m + 1) * 128, t0:t0 + n], RQ
            elif m < 8:
                dst, R_ = k_s[(m - 4) * 128:(m - 3) * 128, t0:t0 + n], RK
            else:
                dst, R_ = r_s[(m - 8) * 128:(m - 7) * 128, t0:t0 + n], RR
            p.dma("sp", dst, e_[:, :n], reads=[e_], writes=[R_])
        for d in range(2):
            ps = pst.next()
            for kc in range(KC):
                o_ = (d * KC + kc) * 16
                mm(p, ps[0:16, :n], wlr_sb[:, o_:o_ + 16], hT[:, kc, :n], kc == 0, kc == KC - 1, [wlr_sb, hT], [ps])
            l_ = lr_sb.next()
            act(p, l_[:, :n], ps[0:16, :n], AF.Copy, [ps], [l_])
            for blk_ in range(4):
                ps2 = pst.next()
                mm(p, ps2[:, :n], gw2_sb[:, d * 512 + blk_ * 128:d * 512 + (blk_ + 1) * 128], l_[:, :n], True, True, [gw2_sb, l_], [ps2])
                e_ = ev.next()
                act(p, e_[:, :n], ps2[:, :n], AF.Exp, [ps2, gb_sb], [e_], scale=-1.0, bias=gb_sb[:, d * 4 + blk_:d * 4 + blk_ + 1])
                act(p, e_[:, :n], e_[:, :n], AF.Ln, [e_], [e_], bias=1.0)
                ts(p, e_[:, :n], e_[:, :n], -1.0 / 16.0, None, ALU.mult, None, [e_], [e_])
                p.dma("sp", g_s[d, blk_ * 128:(blk_ + 1) * 128, t0:t0 + n], e_[:, :n], reads=[e_], writes=[RG])
        for h in range(2):
            p.dma("pool", wv_sb[:], wv[h], writes=[wv_sb])
            for st in range(n // 128):
                ps = pst.next()
                for kc in range(KC):
                    mm(p, ps[:, :], hT[:, kc, st * 128:(st + 1) * 128], wv_sb[:, kc * 512:(kc + 1) * 512], kc == 0, kc == KC - 1, [wv_sb, hT], [ps])
                e_ = ev.next()
                act(p, e_[:, :], ps[:, :], AF.Copy, [ps], [e_])
                p.dma("sp", v_s[t0 + st * 128:t0 + (st + 1) * 128, h * 512:(h + 1) * 512], e_[:, :], reads=[e_], writes=[RV])

    S32 = p.sb("S32", [128, 2, 512])
    Sr = p.sb("Sr", [128, 2, 512], F32R)
    qc_p = p.rot_sb("qc", 2, [128, 2, GC])
    kc_p = p.rot_sb("kc", 2, [128, 2, GC])
    gc_p = p.rot_sb("gc", 2, [128, 2, GC])
    vc_p = p.rot_sb("vc", 2, [128, 512], F32R)
    cum_p = p.rot_sb("cum", 2, [128, 2, GC])
    e_p = p.rot_sb("e", 2, [128, 2, GC])
    ei_p = p.rot_sb("ei", 2, [128, 2, GC])
    qi_p = p.rot_sb("qi", 2, [128, 2, GC], F32R)
    ki_p = p.rot_sb("ki", 2, [128, 2, GC], F32R)
    kd_p = p.rot_sb("kd", 2, [128, 2, GC], F32R)
    kdT_p = p.rot_sb("kdT", 2, [128, 256], F32R)
    aT_p = p.rot_sb("aT", 2, [128, GC], F32R)
    o_p = p.rot_sb("o", 2, [128, 4, GC])
    o0_p = p.rot_sb("o0", 2, [128, 4, GC])
    rc_p = p.rot_sb("rc", 2, [128, 4, GC])
    sq_p = p.rot_sb("sq", 2, [128, 4, GC], F32R)
    rs_p = p.rot_sb("rs", 2, [128, GC])
    out_toks = []
    for h in range(2):
        for d in range(2):
            p.op("dve", lambda e: e.memset(S32[:], 0.0), writes=[S32])
            act(p, Sr[:], S32[:], AF.Copy, [S32], [Sr])
            ctx_ch = [0, 1]
            lat_ch = list(range(2, NCH))
            order = (ctx_ch + lat_ch) if d == 0 else (ctx_ch[::-1] + lat_ch[::-1])
            for c in order:
                t0 = c * GC
                is_lat = c >= 2
                qc, kc_, gcn, vc = qc_p.next(), kc_p.next(), gc_p.next(), vc_p.next()
                p.dma("sp", kc_[:], k_s[h * 256:(h + 1) * 256, t0:t0 + GC].rearrange("(c q) n -> q c n", q=128), reads=[RK], writes=[kc_])
                p.dma("sp", gcn[:], g_s[d, h * 256:(h + 1) * 256, t0:t0 + GC].rearrange("(c q) n -> q c n", q=128), reads=[RG], writes=[gcn])
                p.dma("pool", vc[:], v_s[t0:t0 + GC, h * 512:(h + 1) * 512], reads=[RV], writes=[vc])
                cum, e_, ei = cum_p.next(), e_p.next(), ei_p.next()
                for k2 in range(2):
                    if d == 0:
                        p.op("dve", lambda e, cum=cum, gcn=gcn, k2=k2: e.tensor_tensor_scan(out=cum[:, k2, :], data0=ones_f, data1=gcn[:, k2, :], initial=0.0, op0=ALU.mult, op1=ALU.add),
                             reads=[gcn, cst_sb], writes=[cum])
                    else:
                        p.op("dve", lambda e, cum=cum, gcn=gcn, k2=k2: e.tensor_tensor_scan(out=cum[:, k2, ::-1], data0=ones_f, data1=gcn[:, k2, ::-1], initial=0.0, op0=ALU.mult, op1=ALU.add),
                             reads=[gcn, cst_sb], writes=[cum])
                act(p, ei[:], cum[:], AF.Exp, [cum], [ei], scale=-1.0)
                act(p, e_[:], cum[:], AF.Exp, [cum], [e_])
                ki, kd = ki_p.next(), kd_p.next()
                tt(p, ki[:], kc_[:], ei[:], ALU.mult, [kc_, ei], [ki])
                last = GC - 1 if d == 0 else 0
                for k2 in range(2):
                    ts(p, kd[:, k2, :], ki[:, k2, :], e_[:, k2, last:last + 1], None, ALU.mult, None, [ki, e_], [kd])
                tp = pst.next()
                for k2 in range(2):
                    mm(p, tp[:, k2 * 128:(k2 + 1) * 128], kd[:, k2, :], ident_r, True, True, [kd, cst_r], [tp])
                kdT = kdT_p.next()
                act(p, kdT[:], tp[:, 0:256], AF.Copy, [tp], [kdT])
                if is_lat:
                    qc = qc_p.next()
                    p.dma("sp", qc[:], q_s[h * 256:(h + 1) * 256, t0:t0 + GC].rearrange("(c q) n -> q c n", q=128), reads=[RQ], writes=[qc])
                    qi = qi_p.next()
                    stt(p, qi[:], qc[:], 1.0 / 16.0, e_[:], ALU.mult, ALU.mult, [qc, e_], [qi])
                    ap_ = pst.next()
                    for k2 in range(2):
                        mm(p, ap_[:, 0:GC], ki[:, k2, :], qi[:, k2, :], k2 == 0, k2 == 1, [ki, qi], [ap_])
                    aT = aT_p.next()
                    tt(p, aT[:], ap_[:, 0:GC], maskf if d == 0 else maskr, ALU.mult, [ap_, cst_sb], [aT])
                    op_ = pst.next()
                    for vch in range(4):
                        for k2 in range(2):
                            mm(p, op_[:, vch * GC:(vch + 1) * GC], Sr[:, k2, vch * 128:(vch + 1) * 128], qi[:, k2, :], k2 == 0, False, [Sr, qi], [op_])
                        mm(p, op_[:, vch * GC:(vch + 1) * GC], vc[:, vch * 128:(vch + 1) * 128], aT[:], False, True, [vc, aT], [op_])
                    tl = t0 - T_CTX
                    if d == 0:
                        o_ = o_p.next()
                        act(p, o_[:].rearrange("q a b -> q (a b)"), op_[:, :], AF.Copy, [op_], [o_])
                        p.dma("sp", o_s[:, tl:tl + GC].rearrange("(c q) n -> q c n", q=128), o_[:], reads=[o_], writes=[RO])
                    else:
                        o0, rc = o0_p.next(), rc_p.next()
                        p.dma("sp", o0[:], o_s[:, tl:tl + GC].rearrange("(c q) n -> q c n", q=128), reads=[RO], writes=[o0])
                        p.dma("sp", rc[:], r_s[h * 512:(h + 1) * 512, t0:t0 + GC].rearrange("(c q) n -> q c n", q=128), reads=[RR], writes=[rc])
                        o_ = o_p.next()
                        tt(p, o_[:].rearrange("q a b -> q (a b)"), op_[:, :], o0[:].rearrange("q a b -> q (a b)"), ALU.add, [op_, o0], [o_])
                        sq_ = sq_p.next()
                        act(p, sq_[:], o_[:], AF.Square, [o_], [sq_])
                        mp = pst.next()
                        for vch in range(4):
                            mm(p, mp[:, 0:GC], cst_r[:, 384:512], sq_[:, vch, :], vch == 0, vch == 3, [cst_r, sq_], [mp])
                        rs = rs_p.next()
                        ts(p, rs[:], mp[:, 0:GC], 1.0 / 512.0, EPS, ALU.mult, ALU.add, [mp], [rs])
                        act(p, rs[:], rs[:], AF.Sqrt, [rs], [rs])
                        p.op("dve", lambda e, rs=rs: e.reciprocal(rs[:], rs[:]), reads=[rs], writes=[rs])
                        act(p, rc[:], rc[:], AF.Silu, [rc], [rc])
                        for vch in range(4):
                            tt(p, o_[:, vch, :], o_[:, vch, :], rs[:], ALU.mult, [o_, rs], [o_])
                            stt(p, o_[:, vch, :], o_[:, vch, :], nw_sb[:, vch:vch + 1], rc[:, vch, :], ALU.mult, ALU.mult, [o_, nw_sb, rc], [o_])
                        out_toks.append(p.dma("sp", out[h * 512:(h + 1) * 512, tl:tl + GC].rearrange("(c q) n -> q c n", q=128), o_[:], reads=[o_]))
                for k2 in range(2):
                    sp_ = pst.next()
                    mm(p, sp_[:, :], kdT[:, k2 * 128:(k2 + 1) * 128], vc[:], True, True, [kdT, vc], [sp_])
                    stt(p, S32[:, k2, :], S32[:, k2, :], e_[:, k2, last:last + 1], sp_[:, :], ALU.mult, ALU.add, [S32, e_, sp_], [S32])
                act(p, Sr[:], S32[:], AF.Copy, [S32], [Sr])
    p.wait_final("sp", out_toks[-NDMA_SEM:])
    p.finish()
    return nc


def run_gla(x, ctx, mods, inp):
    w_in = inp["o_w_in"][0]
    cst = np.ascontiguousarray(np.concatenate([_tri_masks(), np.eye(128, dtype=np.float32), np.ones((128, 128), np.float32)], axis=1))
    nc = build_gla()
    in_maps = []
    for k in range(NCORES):
        b, hh = k // 2, k % 2
        heads = [2 * hh, 2 * hh + 1]
        cols = []
        for h in heads:
            cols += list(range(h * 256, (h + 1) * 256))
        for h in heads:
            cols += list(range(1024 + h * 256, 1024 + (h + 1) * 256))
        for h in heads:
            cols += list(range(4096 + h * 512, 4096 + (h + 1) * 512))
        wfm = blk(np.ascontiguousarray(w_in[:, cols]))
        wv = np.stack([np.ascontiguousarray(w_in[:, 2048 + h * 512:2048 + (h + 1) * 512].reshape(KC, 128, 512).transpose(1, 0, 2)).reshape(128, KC * 512) for h in heads])
        wlr = np.ascontiguousarray(w_in[:, 6144:6176].reshape(KC, 128, 2, 16).transpose(1, 2, 0, 3)).reshape(128, 2 * KC * 16)
        dk0 = heads[0] * 256
        gw2 = np.ascontiguousarray(inp["gla_w2"][0][:, :, dk0:dk0 + 512].transpose(1, 0, 2)).reshape(16, 1024)
        gbv = inp["gla_b"][0][:, dk0:dk0 + 512]
        gb = np.ascontiguousarray(gbv.reshape(2, 4, 128).transpose(2, 0, 1)).reshape(128, 8)
        nw = colT(inp["gla_norm_w"][0])
        mv = np.concatenate([colT(mods[4, 1, 0]), colT(mods[4, 1, 1]), colT(mods[b, 1, 0]), colT(mods[b, 1, 1])], axis=1)
        xT = np.ascontiguousarray(np.concatenate([ctx[b], x[b]], axis=0).T)
        in_maps.append({"xT": xT, "modv": np.ascontiguousarray(mv), "wfm": wfm, "wv": wv, "wlr": wlr, "gw2": gw2, "gb": gb,
                        "nw": np.ascontiguousarray(nw), "cst": cst})
    res = run_bass_kernel_spmd(nc, in_maps, core_ids=list(range(NCORES)))
    mix = np.empty((4, T_LAT, D), np.float32)
    for k in range(NCORES):
        b, hh = k // 2, k % 2
        mix[b, :, hh * 1024:(hh + 1) * 1024] = res.results[k]["out"].T
    return mix


MAGIC = 12582912.0
TWO_PI = float(2 * np.pi)
CW1 = 6.28125
CW2 = float(2 * np.pi - 6.28125)
HALF_PI = float(np.pi / 2)
SCH = 1024


def emit_sin(p, out, x, k_t, reads, n_slice):
    rd = list(reads)
    ts(p, k_t, x, 1.0 / TWO_PI, MAGIC, ALU.mult, ALU.add, rd, [n_slice[0]])
    ts(p, k_t, k_t, -MAGIC, None, ALU.add, None, [n_slice[0]], [n_slice[0]])
    stt(p, x, k_t, -CW1, x, ALU.mult, ALU.add, [n_slice[0]] + rd, rd)
    stt(p, x, k_t, -CW2, x, ALU.mult, ALU.add, [n_slice[0]] + rd, rd)
    ts(p, x, x, 3.14159, -3.14159, ALU.min, ALU.max, rd, rd)
    act(p, out, x, AF.Sin, rd, [n_slice[1]])


def build_mix0(do_s5=True, do_dn=True):
    nc = bass.Bass("TRN2", target_bir_lowering=False)
    xT = dram_in(nc, "xT", [D, T_ALL])
    modv = dram_in(nc, "modv", [128, 64])
    wfm = dram_in(nc, "wfm", [20, 128, D], F32R)
    wtm = dram_in(nc, "wtm", [128, KC * 528], F32R)
    s5par = dram_in(nc, "s5par", [128, 3 * 32])
    s5bw = dram_in(nc, "s5bw", [4, 128, 2048], F32R)
    s5cw = dram_in(nc, "s5cw", [4, 128, 2048], F32R)
    s5d = dram_in(nc, "s5d", [128, 4])
    iota_d = dram_in(nc, "iota", [128, T_LAT])
    out_s5 = dram_out(nc, "out_s5", [512, T_ALL])
    dnpar_d = dram_in(nc, "dnpar", [128, 16])
    dncw_d = dram_in(nc, "dncw", [128, 12 * 9])
    dnnw_d = dram_in(nc, "dnnw", [128, 128])
    dncst_d = dram_in(nc, "dncst", [128, 6 * 128])
    out_dn = dram_out(nc, "out_dn", [T_ALL, 512])
    o0_s = nc.dram_tensor("o0_s", [T_ALL, 128], F32, kind="Internal").ap()
    RO0 = Res("o0_s")
    fm_s = nc.dram_tensor("fm_s", [20 * 128, T_ALL], F32, kind="Internal").ap()
    tm_s = nc.dram_tensor("tm_s", [T_ALL, 528], F32, kind="Internal").ap()
    RFM, RTM = Res("fm_s"), Res("tm_s")
    p = Prog(nc)
    pst = p.rot_ps("ps", 8, [128, 512])
    mod_sb = p.sb("mod_sb", [128, 64])
    p.dma("sp", mod_sb[:], modv, writes=[mod_sb])
    for s in range(2):
        ts(p, mod_sb[:, s * 32 + 16:s * 32 + 32], mod_sb[:, s * 32 + 16:s * 32 + 32], 1.0, None, ALU.add, None, [mod_sb], [mod_sb])

    p.begin_phase()
    xt = p.sb("xt", [128, KC, 512])
    hT = p.sb("hT", [128, KC, 512], F32R)
    wblk = p.rot_sb("wblk", 3, [128, D], F32R)
    wtm_sb = p.sb("wtm_sb", [128, KC * 528], F32R)
    ev = p.rot_sb("ev", 3, [128, 528])
    p.dma("pool", wtm_sb[:], wtm, writes=[wtm_sb])
    tiles = [(0, 0, T_CTX)] + [(1, T_CTX + i * 512, 512) for i in range(T_LAT // 512)]
    for (s, t0, n) in tiles:
        p.dma("sp", xt[:, :, :n], xT[:, t0:t0 + n].rearrange("(kc q) n -> q kc n", q=128), writes=[xt])
        for kc in range(KC):
            ts(p, hT[:, kc, :n], xt[:, kc, :n], mod_sb[:, s * 32 + 16 + kc:s * 32 + 17 + kc], mod_sb[:, s * 32 + kc:s * 32 + kc + 1],
               ALU.mult, ALU.add, [xt, mod_sb], [hT])
        for m in range(20):
            w = wblk.next()
            p.dma("pool", w[:], wfm[m], writes=[w])
            ps = pst.next()
            for kc in range(KC):
                mm(p, ps[:, :n], w[:, kc * 128:(kc + 1) * 128], hT[:, kc, :n], kc == 0, kc == KC - 1, [w, hT], [ps])
            e_ = ev.next()
            act(p, e_[:, :n], ps[:, :n], AF.Copy, [ps], [e_])
            p.dma("sp", fm_s[m * 128:(m + 1) * 128, t0:t0 + n], e_[:, :n], reads=[e_], writes=[RFM])
        for st in range(n // 128):
            ps = pst.next()
            ps2 = pst.next()
            for kc in range(KC):
                mm(p, ps[:, :], hT[:, kc, st * 128:(st + 1) * 128], wtm_sb[:, kc * 528:kc * 528 + 512], kc == 0, kc == KC - 1, [wtm_sb, hT], [ps])
            for kc in range(KC):
                mm(p, ps2[:, 0:16], hT[:, kc, st * 128:(st + 1) * 128], wtm_sb[:, kc * 528 + 512:kc * 528 + 528], kc == 0, kc == KC - 1, [wtm_sb, hT], [ps2])
            e_ = ev.next()
            act(p, e_[:, 0:512], ps[:, :], AF.Copy, [ps], [e_])
            act(p, e_[:, 512:528], ps2[:, 0:16], AF.Copy, [ps2], [e_])
            p.dma("sp", tm_s[t0 + st * 128:t0 + (st + 1) * 128, :], e_[:, :], reads=[e_], writes=[RTM])

    p.end_phase()
    out_toks = []
    if do_s5:
        p.begin_phase()
        par = p.sb("par", [128, 96])
        iota = p.sb("iota_sb", [128, T_LAT])
        d_sb = p.sb("d_sb", [128, 4])
        p.dma("sp", par[:], s5par, writes=[par])
        p.dma("sp", iota[:], iota_d, writes=[iota])
        p.dma("sp", d_sb[:], s5d, writes=[d_sb])
        cs = p.sb("cs", [128, 12, 32])
        LRE, LIM, LS = par[:, 0:32], par[:, 32:64], par[:, 64:96]
        (DEL, MAG, ANG, SINA, COSA, ARE, AIM, RDEN, FRE, FIM, T1, T2) = [cs[:, i, :] for i in range(12)]
        RC, RP = [cs], [par]
        act(p, DEL, LS, AF.Exp, RP, RC)
        tt(p, MAG, LRE, DEL, ALU.mult, RP + RC, RC)
        act(p, MAG, MAG, AF.Exp, RC, RC)
        tt(p, ANG, LIM, DEL, ALU.mult, RP + RC, RC)
        ts(p, T1, ANG, 1.0, None, ALU.mult, None, RC, RC)
        emit_sin(p, SINA, T1, T2, RC, (cs, cs))
        ts(p, T1, ANG, HALF_PI, None, ALU.add, None, RC, RC)
        emit_sin(p, COSA, T1, T2, RC, (cs, cs))
        tt(p, ARE, MAG, COSA, ALU.mult, RC, RC)
        tt(p, AIM, MAG, SINA, ALU.mult, RC, RC)
        tt(p, T1, LRE, LRE, ALU.mult, RP, RC)
        tt(p, T2, LIM, LIM, ALU.mult, RP, RC)
        tt(p, RDEN, T1, T2, ALU.add, RC, RC)
        p.op("dve", lambda e: e.reciprocal(RDEN, RDEN), reads=RC, writes=RC)
        ts(p, T1, ARE, -1.0, None, ALU.add, None, RC, RC)
        tt(p, FRE, T1, LRE, ALU.mult, RC + RP, RC)
        tt(p, T2, AIM, LIM, ALU.mult, RC + RP, RC)
        tt(p, FRE, FRE, T2, ALU.add, RC, RC)
        tt(p, FRE, FRE, RDEN, ALU.mult, RC, RC)
        tt(p, FIM, AIM, LRE, ALU.mult, RC + RP, RC)
        tt(p, T2, T1, LIM, ALU.mult, RC + RP, RC)
        tt(p, FIM, FIM, T2, ALU.subtract, RC, RC)
        tt(p, FIM, FIM, RDEN, ALU.mult, RC, RC)

        U = p.sb("U", [128, T_ALL], F32R)
        Y = p.sb("Y", [128, T_ALL])
        bw = p.sb("bw", [128, 2048], F32R)
        cw = p.sb("cw", [128, 2048], F32R)
        tabs = p.sb("tabs", [128, 4, SCH])
        kt = p.sb("kt", [128, SCH])
        bu = p.rot_sb("bu", 2, [128, 2, SCH])
        vv = p.rot_sb("vv", 2, [128, 2, SCH])
        gg = p.rot_sb("gg", 2, [128, 2, SCH])
        hh_ = p.rot_sb("hh", 2, [128, 2, SCH], F32R)
        tq = p.rot_sb("tq", 2, [128, SCH])
        ini = p.rot_sb("ini", 2, [128, 4])
        S_t, C_t, Ec, Es = tabs[:, 0, :], tabs[:, 1, :], tabs[:, 2, :], tabs[:, 3, :]
        for blk_ in range(4):
            p.dma("pool", U[:], fm_s[(12 + blk_) * 128:(13 + blk_) * 128, :], reads=[RFM], writes=[U])
            p.dma("pool", bw[:], s5bw[blk_], writes=[bw])
            p.dma("pool", cw[:], s5cw[blk_], writes=[cw])
            first_y = True
            for r4 in range(4):
                r = blk_ * 4 + r4
                half = r4 // 2
                for d in range(2):
                    col = d * 16 + r
                    th = ANG[:, col:col + 1]
                    rr = MAG[:, col:col + 1]
                    carry = None
                    chunks = [(0, 0, T_CTX)] + [(1, i * SCH, SCH) for i in range(T_LAT // SCH)]
                    for (seg, tau0, n) in chunks:
                        seg_off, seg_len = (0, T_CTX) if seg == 0 else (T_CTX, T_LAT)
                        if d == 0:
                            n0 = seg_off + tau0
                        else:
                            n0 = seg_off + seg_len - tau0 - n
                        rev = (d == 1)

                        def nat(ap):
                            return ap[:, ::-1] if rev else ap
                        ts(p, S_t[:, :n], iota[:, tau0:tau0 + n], th, None, ALU.mult, None, [iota, cs], [tabs])
                        ts(p, C_t[:, :n], S_t[:, :n], HALF_PI, None, ALU.add, None, [tabs], [tabs])
                        emit_sin(p, S_t[:, :n], S_t[:, :n], kt[:, :n], [tabs], (kt, tabs))
                        emit_sin(p, C_t[:, :n], C_t[:, :n], kt[:, :n], [tabs], (kt, tabs))
                        fre, fim = FRE[:, col:col + 1], FIM[:, col:col + 1]
                        ts(p, Ec[:, :n], C_t[:, :n], fre, None, ALU.mult, None, [tabs, cs], [tabs])
                        stt(p, Ec[:, :n], S_t[:, :n], fim, Ec[:, :n], ALU.mult, ALU.add, [tabs, cs], [tabs])
                        ts(p, Es[:, :n], S_t[:, :n], fre, None, ALU.mult, None, [tabs, cs], [tabs])
                        stt(p, Es[:, :n], C_t[:, :n], fim, Es[:, :n], ALU.mult, ALU.subtract, [tabs, cs], [tabs])
                        b_ = bu.next()
                        for reim in range(2):
                            wcol = ((d * 4 + r4) * 2 + reim) * 128
                            for c0 in range(0, n, 512):
                                cn = min(512, n - c0)
                                ps = pst.next()
                                mm(p, ps[:, :cn], bw[64 * half:64 * half + 64, wcol:wcol + 128], U[64 * half:64 * half + 64, n0 + c0:n0 + c0 + cn],
                                   True, True, [bw, U], [ps])
                                act(p, b_[:, reim, c0:c0 + cn], ps[:, :cn], AF.Copy, [ps], [b_])
                        bre, bim = nat(b_[:, 0, :n]), nat(b_[:, 1, :n])
                        v_ = vv.next()
                        t1, t2 = tq.next(), tq.next()
                        tt(p, t1[:, :n], Ec[:, :n], bre, ALU.mult, [tabs, b_], [t1])
                        tt(p, t2[:, :n], Es[:, :n], bim, ALU.mult, [tabs, b_], [t2])
                        tt(p, v_[:, 0, :n], t1[:, :n], t2[:, :n], ALU.subtract, [t1, t2], [v_])
                        tt(p, t1[:, :n], Ec[:, :n], bim, ALU.mult, [tabs, b_], [t1])
                        tt(p, t2[:, :n], Es[:, :n], bre, ALU.mult, [tabs, b_], [t2])
                        tt(p, v_[:, 1, :n], t1[:, :n], t2[:, :n], ALU.add, [t1, t2], [v_])
                        g_ = gg.next()
                        if seg == 1 and tau0 == 0:
                            i_ = ini.next()
                            pg = carry
                            c256, s256 = C_t[:, 256:257], S_t[:, 256:257]
                            tt(p, i_[:, 0:1], pg[:, 0, T_CTX - 1:T_CTX], c256, ALU.mult, [pg, tabs], [i_])
                            tt(p, i_[:, 1:2], pg[:, 1, T_CTX - 1:T_CTX], s256, ALU.mult, [pg, tabs], [i_])
                            tt(p, i_[:, 2:3], i_[:, 0:1], i_[:, 1:2], ALU.subtract, [i_], [i_])
                            tt(p, i_[:, 0:1], pg[:, 0, T_CTX - 1:T_CTX], s256, ALU.mult, [pg, tabs], [i_])
                            tt(p, i_[:, 1:2], pg[:, 1, T_CTX - 1:T_CTX], c256, ALU.mult, [pg, tabs], [i_])
                            tt(p, i_[:, 3:4], i_[:, 0:1], i_[:, 1:2], ALU.add, [i_], [i_])
                            inits = (i_[:, 2:3], i_[:, 3:4], i_)
                        elif carry is None:
                            inits = (0.0, 0.0, None)
                        else:
                            pn = carry_n
                            inits = (carry[:, 0, pn - 1:pn], carry[:, 1, pn - 1:pn], carry)
                        for reim in range(2):
                            rd = [v_, cs] + ([inits[2]] if inits[2] is not None else [])
                            p.op("dve", lambda e, g_=g_, v_=v_, reim=reim, n=n, ini_=inits[reim], rr=rr: e.tensor_tensor_scan(
                                out=g_[:, reim, :n], data0=rr.to_broadcast([128, n]), data1=v_[:, reim, :n], initial=ini_, op0=ALU.mult, op1=ALU.add),
                                reads=rd, writes=[g_])
                        carry, carry_n = g_, n
                        h_ = hh_.next()
                        t1, t2 = tq.next(), tq.next()
                        tt(p, t1[:, :n], C_t[:, :n], g_[:, 0, :n], ALU.mult, [tabs, g_], [t1])
                        tt(p, t2[:, :n], S_t[:, :n], g_[:, 1, :n], ALU.mult, [tabs, g_], [t2])
                        tt(p, nat(h_[:, 0, :n]), t1[:, :n], t2[:, :n], ALU.subtract, [t1, t2], [h_])
                        tt(p, t1[:, :n], S_t[:, :n], g_[:, 0, :n], ALU.mult, [tabs, g_], [t1])
                        tt(p, t2[:, :n], C_t[:, :n], g_[:, 1, :n], ALU.mult, [tabs, g_], [t2])
                        stt(p, nat(h_[:, 1, :n]), t1[:, :n], -1.0, t2[:, :n], ALU.mult, ALU.subtract, [t1, t2], [h_])
                        for c0 in range(0, n, 512):
                            cn = min(512, n - c0)
                            ps = pst.next()
                            for reim in range(2):
                                wcol = ((d * 4 + r4) * 2 + reim) * 128
                                mm(p, ps[:, :cn], cw[:, wcol:wcol + 128], h_[:, reim, c0:c0 + cn], reim == 0, reim == 1, [cw, h_], [ps])
                            ysl = Y[:, n0 + c0:n0 + c0 + cn]
                            if first_y:
                                p.op("dve", lambda e, ysl=ysl, ps=ps, cn=cn: e.tensor_copy(ysl, ps[:, :cn]), reads=[ps], writes=[Y])
                            else:
                                tt(p, ysl, ysl, ps[:, :cn], ALU.add, [Y, ps], [Y])
                    first_y = False
            for c0 in range(0, T_ALL, SCH):
                cn = min(SCH, T_ALL - c0)
                g1, g2 = tq.next(), tq.next()
                U_f = U.t[:].bitcast(F32)
                stt(p, Y[:, c0:c0 + cn], U_f[:, c0:c0 + cn], d_sb[:, blk_:blk_ + 1], Y[:, c0:c0 + cn], ALU.mult, ALU.add, [U, d_sb, Y], [Y])
                tt(p, g1[:, :cn], Y[:, c0:c0 + cn], Y[:, c0:c0 + cn], ALU.mult, [Y], [g1])
                ts(p, g1[:, :cn], g1[:, :cn], 0.044715, 1.0, ALU.mult, ALU.add, [g1], [g1])
                tt(p, g1[:, :cn], g1[:, :cn], Y[:, c0:c0 + cn], ALU.mult, [g1, Y], [g1])
                act(p, g1[:, :cn], g1[:, :cn], AF.Sigmoid, [g1], [g1], scale=1.5957691216057308)
                tt(p, g1[:, :cn], g1[:, :cn], Y[:, c0:c0 + cn], ALU.mult, [g1, Y], [g1])
                p.dma("sp", g2[:, :cn], fm_s[(16 + blk_) * 128:(17 + blk_) * 128, c0:c0 + cn], reads=[RFM], writes=[g2])
                act(p, g2[:, :cn], g2[:, :cn], AF.Sigmoid, [g2], [g2])
                tt(p, g1[:, :cn], g1[:, :cn], g2[:, :cn], ALU.mult, [g1, g2], [g1])
                out_toks.append(p.dma("sp", out_s5[blk_ * 128:(blk_ + 1) * 128, c0:c0 + cn], g1[:, :cn], reads=[g1]))
        p.end_phase()

    if do_dn:
        p.begin_phase()
        dnpar = p.sb("dnpar_sb", [128, 16])
        dncw = p.sb("dncw_sb", [128, 108])
        dnnw = p.sb("dnnw_sb", [128, 128])
        cst = p.sb("dncst_sb", [128, 768])
        for t_, d_ in ((dnpar, dnpar_d), (dncw, dncw_d), (dnnw, dnnw_d), (cst, dncst_d)):
            p.dma("sp", t_[:], d_, writes=[t_])
        LE, GE, GT, LT, IDN, ONE = [cst[:, i * 128:(i + 1) * 128] for i in range(6)]
        nexpa = p.sb("nexpa", [128, 8])
        act(p, nexpa[:], dnpar[:, 0:8], AF.Exp, [dnpar], [nexpa])
        ts(p, nexpa[:], nexpa[:], -1.0, None, ALU.mult, None, [nexpa], [nexpa])
        raw = p.sb("raw", [128, T_ALL])
        qkv = [p.sb(f"qkv{i}", [128, T_ALL]) for i in range(3)]
        sqb = p.rot_sb("sqb", 2, [128, 512])
        rsb = p.rot_sb("rsb", 2, [128, 512])
        S = p.sb("S_dn", [128, 128])
        tmc_p = p.rot_sb("tmc", 2, [128, 528])
        col_p = p.rot_sb("col", 2, [128, 16])
        names = ["gcb", "DT", "Dm", "X", "XT", "IX", "MT", "attnT", "ktok", "vtok", "kbe", "vb", "WT", "U", "vnew", "o2", "kg", "o", "o0", "zz"]
        sq_ = {nm: p.rot_sb("dn_" + nm, 2, [128, 128]) for nm in names}

        _nh, _nc, _it = 4, 999, 6
        for hl in range(_nh):
            for qi_ in range(3):
                blk_i = qi_ * 4 + hl
                dst = qkv[qi_]
                p.dma("sp", raw[:], fm_s[blk_i * 128:(blk_i + 1) * 128, :], reads=[RFM], writes=[raw])
                wc = lambda i, j: dncw[:, blk_i * 9 + i * 3 + j:blk_i * 9 + i * 3 + j + 1]
                rl = raw[:, T_CTX:T_ALL].rearrange("q (r c) -> q r c", c=64)
                dl = dst[:, T_CTX:T_ALL].rearrange("q (r c) -> q r c", c=64)
                ts(p, dst[:, T_CTX:T_ALL], raw[:, T_CTX:T_ALL], wc(1, 1), None, ALU.mult, None, [raw, dncw], [dst])
                for i in range(3):
                    for j in range(3):
                        if i == 1 and j == 1:
                            continue
                        di, dj = i - 1, j - 1
                        r0, r1 = max(0, -di), 64 - max(0, di)
                        c0, c1 = max(0, -dj), 64 - max(0, dj)
                        stt(p, dl[:, r0:r1, c0:c1], rl[:, r0 + di:r1 + di, c0 + dj:c1 + dj], wc(i, j), dl[:, r0:r1, c0:c1], ALU.mult, ALU.add, [raw, dncw, dst], [dst])
                ts(p, dst[:, 0:T_CTX], raw[:, 0:T_CTX], wc(1, 1), None, ALU.mult, None, [raw, dncw], [dst])
                stt(p, dst[:, 1:T_CTX], raw[:, 0:T_CTX - 1], wc(1, 0), dst[:, 1:T_CTX], ALU.mult, ALU.add, [raw, dncw, dst], [dst])
                stt(p, dst[:, 0:T_CTX - 1], raw[:, 1:T_CTX], wc(1, 2), dst[:, 0:T_CTX - 1], ALU.mult, ALU.add, [raw, dncw, dst], [dst])
                act(p, dst[:], dst[:], AF.Silu, [dst], [dst])
                if qi_ < 2:
                    for c0 in range(0, T_ALL, 512):
                        cn = min(512, T_ALL - c0)
                        sb_, rb_ = sqb.next(), rsb.next()
                        act(p, sb_[:, :cn], dst[:, c0:c0 + cn], AF.Square, [dst], [sb_])
                        ps = pst.next()
                        mm(p, ps[:, :cn], ONE, sb_[:, :cn], True, True, [cst, sb_], [ps])
                        ts(p, rb_[:, :cn], ps[:, :cn], EPS, None, ALU.add, None, [ps], [rb_])
                        act(p, rb_[:, :cn], rb_[:, :cn], AF.Sqrt, [rb_], [rb_])
                        p.op("dve", lambda e, rb_=rb_, cn=cn: e.reciprocal(rb_[:, :cn], rb_[:, :cn]), reads=[rb_], writes=[rb_])
                        if qi_ == 0:
                            stt(p, dst[:, c0:c0 + cn], dst[:, c0:c0 + cn], float(128 ** -0.5), rb_[:, :cn], ALU.mult, ALU.mult, [dst, rb_], [dst])
                        else:
                            tt(p, dst[:, c0:c0 + cn], dst[:, c0:c0 + cn], rb_[:, :cn], ALU.mult, [dst, rb_], [dst])
            qn, kn, vs = qkv
            for d in range(2):
                TRI, MI, MS = (LE, LE, GT) if d == 0 else (GE, GE, LT)
                last = 127 if d == 0 else 0
                pi = d * 4 + hl
                p.op("dve", lambda e: e.memset(S[:], 0.0), writes=[S])
                order = [0, 1] + list(range(2, NCH)) if d == 0 else [1, 0] + list(range(NCH - 1, 1, -1))
                for c in order[:_nc]:
                    t0 = c * GC
                    tmc = tmc_p.next()
                    p.dma("sp", tmc[:], tm_s[t0:t0 + GC, :], reads=[RTM], writes=[tmc])
                    cl = col_p.next()
                    beta, nbeta, g, gc, ngc, egc, sc, gl, kgc, egl = [cl[:, i:i + 1] for i in range(10)]
                    R_ = [cl]
                    act(p, beta, tmc[:, 512 + pi:513 + pi], AF.Sigmoid, [tmc], R_)
                    ts(p, nbeta, beta, -1.0, None, ALU.mult, None, R_, R_)
                    act(p, g, tmc[:, 520 + pi:521 + pi], AF.Exp, [tmc, dnpar], R_, bias=dnpar[:, 8 + pi:9 + pi])
                    act(p, g, g, AF.Ln, R_, R_, bias=1.0)
                    ts(p, g, g, nexpa[:, pi:pi + 1], None, ALU.mult, None, R_ + [nexpa], R_)
                    ps = pst.next()
                    mm(p, ps[:, 0:1], TRI, g, True, True, [cst, cl], [ps])
                    act(p, gc, ps[:, 0:1], AF.Copy, [ps], R_)
                    ts(p, ngc, gc, -1.0, None, ALU.mult, None, R_, R_)
                    act(p, egc, gc, AF.Exp, R_, R_)
                    tt(p, sc, beta, egc, ALU.mult, R_, R_)
                    gcb = sq_["gcb"].next()
                    ts(p, gcb[:], ONE, gc, None, ALU.mult, None, [cst] + R_, [gcb])
                    bc = pst.next()
                    mm(p, bc[:, 0:128], gcb[:], IDN, True, True, [gcb, cst], [bc])
                    DT, Dm = sq_["DT"].next(), sq_["Dm"].next()
                    act(p, DT[:], bc[:, 0:128], AF.Exp, [bc] + R_, [DT], bias=ngc)
                    stt(p, DT[:], DT[:], 1.0, MI, ALU.min, ALU.mult, [DT, cst], [DT])
                    act(p, Dm[:], bc[:, 0:128], AF.Exp, [bc] + R_, [Dm], scale=-1.0, bias=gc)
                    stt(p, Dm[:], Dm[:], 1.0, MS, ALU.min, ALU.mult, [Dm, cst], [Dm])
                    act(p, gl, bc[:, last:last + 1], AF.Copy, [bc], R_)
                    act(p, kgc, gc, AF.Exp, R_, R_, scale=-1.0, bias=gl)
                    act(p, egl, gl, AF.Exp, R_, R_)
                    kT, qT, vT = kn[:, t0:t0 + GC], qn[:, t0:t0 + GC], vs[:, t0:t0 + GC]
                    kk = pst.next()
                    mm(p, kk[:, 0:128], kT, kT, True, True, [kn], [kk])
                    X, XT = sq_["X"].next(), sq_["XT"].next()
                    stt(p, X[:], kk[:, 0:128], nbeta, Dm[:], ALU.mult, ALU.mult, [kk, Dm] + R_, [X])
                    tp = pst.next()
                    mm(p, tp[:, 0:128], X[:], IDN, True, True, [X, cst], [tp])
                    act(p, XT[:], tp[:, 0:128], AF.Copy, [tp], [XT])
                    qk = pst.next()
                    mm(p, qk[:, 0:128], kT, qT, True, True, [kn, qn], [qk])
                    attnT = sq_["attnT"].next()
                    tt(p, attnT[:], qk[:, 0:128], DT[:], ALU.mult, [qk, DT], [attnT])
                    ktok, vtok = sq_["ktok"].next(), sq_["vtok"].next()
                    tp = pst.next()
                    mm(p, tp[:, 0:128], kT, IDN, True, True, [kn, cst], [tp])
                    act(p, ktok[:], tp[:, 0:128], AF.Copy, [tp], [ktok])
                    tp = pst.next()
                    mm(p, tp[:, 0:128], vT, IDN, True, True, [vs, cst], [tp])
                    act(p, vtok[:], tp[:, 0:128], AF.Copy, [tp], [vtok])
                    kbe, vb, kg = sq_["kbe"].next(), sq_["vb"].next(), sq_["kg"].next()
                    ts(p, kbe[:], ktok[:], sc, None, ALU.mult, None, [ktok] + R_, [kbe])
                    ts(p, vb[:], vtok[:], beta, None, ALU.mult, None, [vtok] + R_, [vb])
                    ts(p, kg[:], ktok[:], kgc, None, ALU.mult, None, [ktok] + R_, [kg])
                    MT = sq_["MT"].next()
                    tt(p, MT[:], XT[:], IDN, ALU.add, [XT, cst], [MT])
                    _stg = 4
                    for it in range(_it):
                        x2 = pst.next()
                        mm(p, x2[:, 0:128], XT[:], X[:], True, True, [XT, X], [x2])
                        Xn = sq_["X"].next()
                        XTn = sq_["XT"].next()
                        if it < 5 and _stg >= 2:
                            xt2 = pst.next()
                            mm(p, xt2[:, 0:128], X[:], XT[:], True, True, [X, XT], [xt2])
                            act(p, XTn[:], xt2[:, 0:128], AF.Copy, [xt2], [XTn])
                        act(p, Xn[:], x2[:, 0:128], AF.Copy, [x2], [Xn])
                        IX = sq_["IX"].next()
                        tt(p, IX[:], Xn[:], IDN, ALU.add, [Xn, cst], [IX])
                        if it < 5:
                            X, XT = Xn, XTn
                        if _stg >= 4:
                            mp = pst.next()
                            mm(p, mp[:, 0:128], IX[:], MT[:], True, True, [IX, MT], [mp])
                            MT = sq_["MT"].next()
                            act(p, MT[:], mp[:, 0:128], AF.Copy, [mp], [MT])
                    WT, U_ = sq_["WT"].next(), sq_["U"].next()
                    wp = pst.next()
                    mm(p, wp[:, 0:128], kbe[:], MT[:], True, True, [kbe, MT], [wp])
                    act(p, WT[:], wp[:, 0:128], AF.Copy, [wp], [WT])
                    up = pst.next()
                    mm(p, up[:, 0:128], MT[:], vb[:], True, True, [MT, vb], [up])
                    act(p, U_[:], up[:, 0:128], AF.Copy, [up], [U_])
                    wsp = pst.next()
                    mm(p, wsp[:, 0:128], WT[:], S[:], True, True, [WT, S], [wsp])
                    vnew = sq_["vnew"].next()
                    tt(p, vnew[:], U_[:], wsp[:, 0:128], ALU.subtract, [U_, wsp], [vnew])
                    o1 = pst.next()
                    mm(p, o1[:, 0:128], qT, S[:], True, True, [qn, S], [o1])
                    o2p = pst.next()
                    mm(p, o2p[:, 0:128], attnT[:], vnew[:], True, True, [attnT, vnew], [o2p])
                    o2 = sq_["o2"].next()
                    act(p, o2[:], o2p[:, 0:128], AF.Copy, [o2p], [o2])
                    o_ = sq_["o"].next()
                    stt(p, o_[:], o1[:, 0:128], egc, o2[:], ALU.mult, ALU.add, [o1, o2] + R_, [o_])
                    sp_ = pst.next()
                    mm(p, sp_[:, 0:128], kg[:], vnew[:], True, True, [kg, vnew], [sp_])
                    stt(p, S[:], S[:], egl, sp_[:, 0:128], ALU.mult, ALU.add, [S, sp_] + R_, [S])
                    if d == 0:
                        p.dma("sp", o0_s[t0:t0 + GC, :], o_[:], reads=[o_], writes=[RO0])
                    else:
                        o0 = sq_["o0"].next()
                        p.dma("sp", o0[:], o0_s[t0:t0 + GC, :], reads=[RO0], writes=[o0])
                        tt(p, o_[:], o_[:], o0[:], ALU.add, [o_, o0], [o_])
                        zz = sq_["zz"].next()
                        tt(p, zz[:], o_[:], o_[:], ALU.mult, [o_], [zz])
                        p.op("dve", lambda e, cl=cl, zz=zz: e.reduce_sum(out=cl[:, 10:11], in_=zz[:], axis=AX.X), reads=[zz], writes=R_)
                        ts(p, cl[:, 10:11], cl[:, 10:11], 1.0 / 128.0, EPS, ALU.mult, ALU.add, R_, R_)
                        act(p, cl[:, 10:11], cl[:, 10:11], AF.Sqrt, R_, R_)
                        p.op("dve", lambda e, cl=cl: e.reciprocal(cl[:, 10:11], cl[:, 10:11]), reads=R_, writes=R_)
                        act(p, zz[:], tmc[:, hl * 128:(hl + 1) * 128], AF.Silu, [tmc], [zz])
                        stt(p, o_[:], o_[:], cl[:, 10:11], dnnw[:], ALU.mult, ALU.mult, [o_, dnnw] + R_, [o_])
                        tt(p, o_[:], o_[:], zz[:], ALU.mult, [o_, zz], [o_])
                        out_toks.append(p.dma("sp", out_dn[t0:t0 + GC, hl * 128:(hl + 1) * 128], o_[:], reads=[o_]))
        p.end_phase()
    p.wait_final("sp", out_toks[-NDMA_SEM:])
    p.finish()
    return nc


def mix0_inputs(k, x, ctx, mods, inp):
    b, hh = k // 2, k % 2
    w_in = inp["e_w_in"][0]
    H = [4 * hh + i for i in range(4)]
    cols = []
    for base in (0, 1024, 2048):
        for h in H:
            cols += list(range(base + h * 128, base + (h + 1) * 128))
    cols += list(range(4096 + hh * 512, 4096 + (hh + 1) * 512))
    cols += list(range(5120 + hh * 512, 5120 + (hh + 1) * 512))
    wfm = blk(np.ascontiguousarray(w_in[:, cols]))
    tcols = []
    for h in H:
        tcols += list(range(3072 + h * 128, 3072 + (h + 1) * 128))
    tcols += [6144 + h for h in H] + [6152 + h for h in H] + [6160 + h for h in H] + [6168 + h for h in H]
    wtm = np.ascontiguousarray(w_in[:, tcols].reshape(KC, 128, 528).transpose(1, 0, 2)).reshape(128, KC * 528)
    g0 = 32 * hh
    lre, lim, lst = inp["s5_lam_re"][0], inp["s5_lam_im"][0], inp["s5_log_step"][0]
    par = np.zeros((128, 3, 2, 16), np.float32)
    for d in range(2):
        for r in range(16):
            for gip in range(2):
                g = g0 + 2 * r + gip
                par[gip * 64:(gip + 1) * 64, 0, d, r] = lre[d, g]
                par[gip * 64:(gip + 1) * 64, 1, d, r] = lim[d, g]
                par[gip * 64:(gip + 1) * 64, 2, d, r] = lst[d, g]
    bre, bim, cre, cim = inp["s5_b_re"][0], inp["s5_b_im"][0], inp["s5_c_re"][0], inp["s5_c_im"][0]
    bw = np.zeros((4, 128, 2, 4, 2, 128), np.float32)
    cw = np.zeros((4, 128, 2, 4, 2, 128), np.float32)
    for bl in range(4):
        for gl in range(8):
            g = g0 + 8 * bl + gl
            r4, gip = gl // 2, gl % 2
            for d in range(2):
                bw[bl, gl * 16:(gl + 1) * 16, d, r4, 0, gip * 64:(gip + 1) * 64] = bre[d, g].T
                bw[bl, gl * 16:(gl + 1) * 16, d, r4, 1, gip * 64:(gip + 1) * 64] = bim[d, g].T
                cw[bl, gip * 64:(gip + 1) * 64, d, r4, 0, gl * 16:(gl + 1) * 16] = cre[d, g].T
                cw[bl, gip * 64:(gip + 1) * 64, d, r4, 1, gl * 16:(gl + 1) * 16] = cim[d, g].T
    s5d = np.ascontiguousarray(inp["s5_d"][0][hh * 512:(hh + 1) * 512].reshape(4, 128).T)
    mv = np.concatenate([colT(mods[4, 0, 0]), colT(mods[4, 0, 1]), colT(mods[b, 0, 0]), colT(mods[b, 0, 1])], axis=1)
    xT = np.ascontiguousarray(np.concatenate([ctx[b], x[b]], axis=0).T)
    dnpar = np.zeros((128, 16), np.float32)
    for d in range(2):
        for hl, h in enumerate(H):
            dnpar[:, d * 4 + hl] = inp["dn_a_log"][0][d, h]
            dnpar[:, 8 + d * 4 + hl] = inp["dn_dt_bias"][0][d, h]
    cwv = inp["dn_conv"][0]
    dncw = np.zeros((128, 12, 9), np.float32)
    for qi_, base in enumerate((0, 1024, 2048)):
        for hl, h in enumerate(H):
            dncw[:, qi_ * 4 + hl, :] = cwv[:, :, base + h * 128:base + (h + 1) * 128].reshape(9, 128).T
    dnnw = np.ascontiguousarray(np.broadcast_to(inp["dn_norm_w"][0], (128, 128)))
    pp = np.arange(128)[:, None]
    ff = np.arange(128)[None, :]
    dncst = np.ascontiguousarray(np.concatenate([(pp <= ff), (pp >= ff), (pp > ff), (pp < ff), (pp == ff), np.ones((128, 128), bool)], axis=1).astype(np.float32))
    return {"dnpar": dnpar, "dncw": dncw.reshape(128, 108), "dnnw": dnnw, "dncst": dncst,
            "xT": xT, "modv": np.ascontiguousarray(mv), "wfm": wfm, "wtm": wtm,
            "s5par": np.ascontiguousarray(par.reshape(128, 96)), "s5bw": bw.reshape(4, 128, 2048), "s5cw": cw.reshape(4, 128, 2048),
            "s5d": s5d, "iota": np.ascontiguousarray(np.broadcast_to(np.arange(T_LAT, dtype=np.float32), (128, T_LAT)))}


def run_mix0(x, ctx, mods, inp):
    nc = build_mix0(True, True)
    in_maps = [mix0_inputs(k, x, ctx, mods, inp) for k in range(NCORES)]
    res = run_bass_kernel_spmd(nc, in_maps, core_ids=list(range(NCORES)))
    mix_x = np.empty((4, T_LAT, D), np.float32)
    mix_c = np.empty((4, T_CTX, D), np.float32)
    for k in range(NCORES):
        b, hh = k // 2, k % 2
        s5 = res.results[k]["out_s5"]
        dn = res.results[k]["out_dn"]
        mix_c[b, :, hh * 512:(hh + 1) * 512] = s5[:, :T_CTX].T
        mix_x[b, :, hh * 512:(hh + 1) * 512] = s5[:, T_CTX:].T
        mix_c[b, :, 1024 + hh * 512:1024 + (hh + 1) * 512] = dn[:T_CTX]
        mix_x[b, :, 1024 + hh * 512:1024 + (hh + 1) * 512] = dn[T_CTX:]
    return mix_x, mix_c


def kernel(**inputs):
    inp = {k: np.asarray(v) for k, v in inputs.items()}
    x = np.ascontiguousarray(inp["x"], dtype=np.float32)
    ctx = np.ascontiguousarray(inp["ctx"], dtype=np.float32)
    mods = run_mods(inp)
    mix_x, mix_c = run_mix0(x, ctx, mods, inp)
    x1, c1 = run_ffn(0, mix_x, mix_c, x, ctx, mods, inp)
    mix1 = run_gla(x1, c1, mods, inp)
    x2, _ = run_ffn(1, mix1, None, x1, None, mods, inp)
    return x2.astype(np.float32)
```

```python
from contextlib import ExitStack
import numpy as np
import concourse.bass as bass
import concourse.mybir as mybir
from concourse.bass_utils import run_bass_kernel_spmd

F32 = mybir.dt.float32
F32R = mybir.dt.float32r
ALU = mybir.AluOpType
AF = mybir.ActivationFunctionType
AX = mybir.AxisListType

ENGS = ("pe", "dve", "act", "pool", "sp")
SEM_LIMIT = 20000
NDMA_SEM = 6
D = 2048
KC = 16
ALPHA = float(4 ** 0.25)
EPS = 1e-6
NCORES = 8


class Res:
    __slots__ = ("name", "w", "r")

    def __init__(self, name=""):
        self.name = name
        self.w = None
        self.r = []


class T:
    def __init__(self, t, name):
        self.t = t
        self.res = Res(name)
        self.name = name

    def __getitem__(self, idx):
        return self.t[idx]


class Rot:
    def __init__(self, tiles):
        self.tiles = tiles
        self.i = 0

    def next(self):
        t = self.tiles[self.i]
        self.i = (self.i + 1) % len(self.tiles)
        return t


def _res(x):
    return x.res if isinstance(x, T) else x


class Prog:
    def __init__(self, nc):
        self.nc = nc
        self.es = ExitStack()
        self.q = {e: [] for e in ENGS}
        self.sems = {}
        self.cur = {}
        self.nsem = 0
        for e in ENGS:
            self._new_epoch(e)
        self.seen = {e: {} for e in ENGS}
        self.dma_sems = {}
        self.dma_cnt = {}
        self.dma_rr = {}
        self.n_inst = 0
        self.final_waits = []
        self.phase = None
        self.pending = {e: [] for e in ENGS}

    def _alloc_sem(self, name):
        h = self.es.enter_context(self.nc.semaphore(name))
        self.sems[name] = h
        self.nsem += 1
        return name

    def _new_epoch(self, e):
        k = self._alloc_sem(f"s_{e}_{self.nsem}")
        self.cur[e] = [k, 0]

    def sb(self, name, shape, dt=F32):
        st = self.phase if self.phase is not None else self.es
        t = st.enter_context(self.nc.sbuf_tensor(name, list(shape), dt))
        return T(t, name)

    def begin_phase(self):
        self.phase = ExitStack()

    def end_phase(self):
        self.barrier()
        self.phase.close()
        self.phase = None

    def barrier(self):
        toks = []
        for e in ENGS:
            k, c = self.cur[e]
            if c > 0:
                toks.append((k, c))
        for e, sems in self.dma_sems.items():
            for i, k in enumerate(sems):
                if self.dma_cnt[e][i] > 0:
                    toks.append((k, self.dma_cnt[e][i]))
        for e in ENGS:
            for (k, v) in toks:
                if k == self.cur[e][0]:
                    continue
                if self.seen[e].get(k, 0) < v:
                    self.seen[e][k] = v
                    self.pending[e].append((k, v))

    def ps(self, name, shape, dt=F32):
        t = self.es.enter_context(self.nc.psum_tensor(name, list(shape), dt))
        return T(t, name)

    def rot_sb(self, name, n, shape, dt=F32):
        return Rot([self.sb(f"{name}{i}", shape, dt) for i in range(n)])

    def rot_ps(self, name, n, shape, dt=F32):
        return Rot([self.ps(f"{name}{i}", shape, dt) for i in range(n)])

    def _deps(self, eng, reads, writes):
        need = {}

        def add(tok):
            if tok is None:
                return
            k, v = tok
            if need.get(k, 0) < v:
                need[k] = v
        for r in reads:
            add(_res(r).w)
        for w in writes:
            w = _res(w)
            add(w.w)
            for t in w.r:
                add(t)
        waits = []
        seen = self.seen[eng]
        own = self.cur[eng][0]
        for k, v in need.items():
            if eng == "pe" and k == own:
                continue
            if seen.get(k, 0) < v:
                seen[k] = v
                waits.append((k, v))
        return waits

    def _commit(self, tok, reads, writes):
        for r in reads:
            r = _res(r)
            r.r.append(tok)
            if len(r.r) > 48:
                d = {}
                for k, v in r.r:
                    if d.get(k, 0) < v:
                        d[k] = v
                r.r = list(d.items())
        for w in writes:
            w = _res(w)
            w.w = tok
            w.r = []

    def op(self, eng, fn, reads=(), writes=(), inc=True):
        waits = self._deps(eng, reads, writes)
        if self.pending[eng]:
            waits = self.pending[eng] + waits
            self.pending[eng] = []
        cur = self.cur[eng]
        tok = (cur[0], cur[1] + 1)
        if inc:
            cur[1] += 1
        self.q[eng].append((waits, fn, (cur[0], 1) if inc else None))
        self._commit(tok, reads, writes)
        self.n_inst += 1
        if inc and cur[1] >= SEM_LIMIT:
            self._new_epoch(eng)
        return tok

    def dma(self, eng, out_ap, in_ap, reads=(), writes=(), **kw):
        waits = self._deps(eng, reads, writes)
        if self.pending[eng]:
            waits = self.pending[eng] + waits
            self.pending[eng] = []
        if eng not in self.dma_sems:
            self.dma_sems[eng] = [self._alloc_sem(f"d_{eng}_{i}") for i in range(NDMA_SEM)]
            self.dma_cnt[eng] = [0] * NDMA_SEM
            self.dma_rr[eng] = 0
        i = self.dma_rr[eng]
        self.dma_rr[eng] = (i + 1) % NDMA_SEM
        self.dma_cnt[eng][i] += 16
        k = self.dma_sems[eng][i]
        tok = (k, self.dma_cnt[eng][i])

        def fn(e, out_ap=out_ap, in_ap=in_ap, kw=kw):
            return e.dma_start(out=out_ap, in_=in_ap, **kw)
        self.q[eng].append((waits, fn, (k, 16)))
        self._commit(tok, reads, writes)
        self.n_inst += 1
        return tok

    def wait_final(self, eng, toks):
        d = {}
        for k, v in toks:
            if d.get(k, 0) < v:
                d[k] = v
        self.final_waits.append((eng, list(d.items())))

    def check(self):
        cnt = {}
        ptr = {e: 0 for e in ENGS}
        prog = True
        while prog:
            prog = False
            for e in ENGS:
                q = self.q[e]
                while ptr[e] < len(q):
                    waits, fn, inc = q[ptr[e]]
                    if all(cnt.get(k, 0) >= v for k, v in waits):
                        if inc is not None:
                            cnt[inc[0]] = cnt.get(inc[0], 0) + inc[1]
                        ptr[e] += 1
                        prog = True
                    else:
                        break
        stuck = {e: (ptr[e], len(self.q[e])) for e in ENGS if ptr[e] < len(self.q[e])}
        if stuck:
            for e in stuck:
                waits, fn, inc = self.q[e][ptr[e]]
                print("STUCK", e, stuck[e], [(k, v, cnt.get(k, 0)) for k, v in waits])
        return not stuck

    def finish(self):
        nc = self.nc
        engmap = {"pe": "tensor", "dve": "vector", "act": "scalar", "pool": "gpsimd", "sp": "sync"}
        fin = {e: [] for e in ENGS}
        for e, toks in self.final_waits:
            fin[e].extend(toks)
        with nc.Block() as block:
            for e in ENGS:
                items = self.q[e]
                fw = fin[e]
                if not items and not fw:
                    continue

                def body(engine, items=items, fw=fw):
                    for waits, fn, inc in items:
                        for k, v in waits:
                            engine.wait_ge(self.sems[k], v)
                        ins = fn(engine)
                        if inc is not None:
                            ins.then_inc(self.sems[inc[0]], inc[1])
                    for k, v in fw:
                        engine.wait_ge(self.sems[k], v)
                getattr(block, engmap[e])(body)
        self.es.close()


def mm(p, out, lhsT, rhs, start, stop, reads, writes):
    p.op("pe", lambda e: e.matmul(out, lhsT, rhs, start=start, stop=stop), reads=reads, writes=writes, inc=True)


def act(p, out, in_, func, reads, writes, **kw):
    p.op("act", lambda e: e.activation(out=out, in_=in_, func=func, **kw), reads=reads, writes=writes)


def tt(p, out, in0, in1, op, reads, writes, eng="dve"):
    p.op(eng, lambda e: e.tensor_tensor(out=out, in0=in0, in1=in1, op=op), reads=reads, writes=writes)


def ts(p, out, in0, s1, s2, op0, op1, reads, writes, eng="dve"):
    if op1 is None:
        p.op(eng, lambda e: e.tensor_scalar(out, in0, s1, None, op0=op0), reads=reads, writes=writes)
    else:
        p.op(eng, lambda e: e.tensor_scalar(out, in0, s1, s2, op0=op0, op1=op1), reads=reads, writes=writes)


def stt(p, out, in0, scalar, in1, op0, op1, reads, writes):
    p.op("dve", lambda e: e.scalar_tensor_tensor(out=out, in0=in0, scalar=scalar, in1=in1, op0=op0, op1=op1),
         reads=reads, writes=writes)


def dram_in(nc, name, shape, dt=F32):
    return nc.dram_tensor(name, list(shape), dt, kind="ExternalInput").ap()


def dram_out(nc, name, shape, dt=F32):
    return nc.dram_tensor(name, list(shape), dt, kind="ExternalOutput").ap()


def blk(W):
    Din, Dout = W.shape
    kc, m = Din // 128, Dout // 128
    return np.ascontiguousarray(W.reshape(kc, 128, m, 128).transpose(2, 1, 0, 3)).reshape(m, 128, kc * 128)


def colT(v):
    return np.ascontiguousarray(v.reshape(-1, 128).T)


NB_A = 2 * 6 * D // 128 // NCORES


def build_mods():
    nc = bass.Bass("TRN2", target_bir_lowering=False)
    condT = dram_in(nc, "condT", [128, KC * 6])
    wb = dram_in(nc, "wb", [NB_A, 128, D], F32R)
    bias = dram_in(nc, "bias", [128, NB_A])
    out = dram_out(nc, "out", [128, NB_A * 6])
    p = Prog(nc)
    c_sb = p.sb("c_sb", [128, KC * 6])
    c_r = p.sb("c_r", [128, KC * 6], F32R)
    b_sb = p.sb("b_sb", [128, NB_A])
    o_sb = p.sb("o_sb", [128, NB_A * 6])
    wt = p.rot_sb("wt", 3, [128, D], F32R)
    pst = p.rot_ps("ps", 4, [128, 512])
    p.dma("sp", c_sb[:], condT, writes=[c_sb])
    p.dma("sp", b_sb[:], bias, writes=[b_sb])
    act(p, c_r[:], c_sb[:], AF.Silu, [c_sb], [c_r])
    for m in range(NB_A):
        w = wt.next()
        p.dma("pool", w[:], wb[m], writes=[w])
        ps = pst.next()
        for kc in range(KC):
            mm(p, ps[:, 0:6], w[:, kc * 128:(kc + 1) * 128], c_r[:, kc * 6:(kc + 1) * 6], kc == 0, kc == KC - 1, [w, c_r], [ps])
        act(p, o_sb[:, m * 6:(m + 1) * 6], ps[:, 0:6], AF.Identity, [ps, b_sb], [o_sb], bias=b_sb[:, m:m + 1])
    t = p.dma("sp", out, o_sb[:], reads=[o_sb])
    p.wait_final("sp", [t])
    p.finish()
    return nc


def run_mods(inp):
    c, c_ctx = inp["c"], inp["c_ctx"]
    cond = np.zeros((6, D), np.float32)
    cond[:4] = c
    cond[4] = c_ctx
    condT = np.ascontiguousarray(cond.T.reshape(KC, 128, 6).transpose(1, 0, 2)).reshape(128, KC * 6)
    W = np.concatenate([inp["ada_w"][0], inp["ada_w"][1]], axis=1)
    B = np.concatenate([inp["ada_b"][0], inp["ada_b"][1]], axis=0)
    Wb = blk(W)
    Bc = colT(B)
    nc = build_mods()
    in_maps = []
    for k in range(NCORES):
        in_maps.append({"condT": condT, "wb": np.ascontiguousarray(Wb[k * NB_A:(k + 1) * NB_A]),
                        "bias": np.ascontiguousarray(Bc[:, k * NB_A:(k + 1) * NB_A])})
    res = run_bass_kernel_spmd(nc, in_maps, core_ids=list(range(NCORES)))
    o = np.stack([r["out"].reshape(128, NB_A, 6) for r in res.results], axis=0)
    full = o.transpose(3, 0, 2, 1).reshape(6, 2 * 6 * D)
    mods = full.reshape(6, 2, 6, D)
    return mods


def modcols(mods, layer, row):
    return np.ascontiguousarray(np.concatenate([colT(mods[row, layer, i]) for i in range(6)], axis=1))


NP_MAX = 512


def build_ffn(segs, E, HC, moe, mode="full"):
    nc = bass.Bass("TRN2", target_bir_lowering=False)
    NT = sum(segs)
    nseg = len(segs)
    do_pre = mode in ("full", "pre")
    do_ffn = mode in ("full", "exp")
    do_post = mode in ("full", "post")
    if do_pre:
        mixT = dram_in(nc, "mixT", [D, NT], F32R)
        xT = dram_in(nc, "xT", [D, NT])
        woutb = dram_in(nc, "woutb", [KC, 128, D], F32R)
    if mode != "exp":
        modv = dram_in(nc, "modv", [128, nseg * 96])
        lnp = dram_in(nc, "lnp", [128, 64])
    if do_ffn:
        w1b = dram_in(nc, "w1b", [E * HC, 128, D], F32R)
        w3b = dram_in(nc, "w3b", [E * HC, 128, D], F32R)
        w2 = dram_in(nc, "w2", [E * HC * 128, D], F32R)
    if mode == "pre":
        router = dram_in(nc, "router", [128, KC * 8])
        ident_d = dram_in(nc, "ident", [128, 128])
        xl_out = dram_out(nc, "xl_out", [D, NT])
        h_out = dram_out(nc, "h_out", [D, NT])
        g_out = dram_out(nc, "g_out", [8, NT])
    if mode == "exp":
        hT_in = dram_in(nc, "hT_in", [D, NT], F32R)
        grow = dram_in(nc, "grow", [128, NT])
        y_out = dram_out(nc, "y_out", [D, NT])
    if mode == "post":
        parts = dram_in(nc, "parts", [8 * D, NT])
        xl_in = dram_in(nc, "xl_in", [D, NT])
    if do_post:
        xoT = dram_out(nc, "xoT", [D, NT])
    p = Prog(nc)

    A = p.sb("A", [128, KC, NP_MAX]) if mode != "pre" else None
    C = p.sb("C", [128, KC, NP_MAX], F32R) if mode != "exp" else None
    Dl = p.sb("Dl", [128, KC, NP_MAX]) if mode != "exp" else None
    H = p.sb("H", [128, KC, NP_MAX], F32R)
    ones = p.sb("ones", [128, 128], F32R)
    wts = p.rot_sb("wts", {"full": 6, "exp": 6, "pre": 3, "post": 1}[mode], [128, D], F32R)
    sq = p.rot_sb("sq", 2, [128, NP_MAX], F32R)
    tmp = p.rot_sb("tmp", 3, [128, NP_MAX])
    a2 = p.rot_sb("a2", 2, [128, NP_MAX], F32R)
    xin = p.rot_sb("xin", 2, [128, NP_MAX])
    xout = p.rot_sb("xout", 2, [128, NP_MAX])
    st_m = p.sb("st_m", [128, NP_MAX])
    st_r = p.sb("st_r", [128, NP_MAX])
    st_n = p.sb("st_n", [128, NP_MAX])
    pst = p.rot_ps("ps", 8, [128, 512])
    ones_f = p.sb("ones_f", [128, 128])
    p.op("dve", lambda e: e.memset(ones_f[:], 1.0 / D), writes=[ones_f])
    act(p, ones[:], ones_f[:], AF.Copy, [ones_f], [ones])
    if mode != "exp":
        mod_sb = p.sb("mod_sb", [128, nseg * 96])
        ln_sb = p.sb("ln_sb", [128, 64])
        p.dma("sp", mod_sb[:], modv, writes=[mod_sb])
        p.dma("sp", ln_sb[:], lnp, writes=[ln_sb])
        for s in range(nseg):
            for mi in (1, 4):
                o = s * 96 + mi * 16
                ts(p, mod_sb[:, o:o + 16], mod_sb[:, o:o + 16], 1.0, None, ALU.add, None, [mod_sb], [mod_sb])
    if mode == "pre":
        r_sb = p.sb("r_sb", [128, KC * 8])
        ident = p.sb("ident_sb", [128, 128])
        p.dma("sp", r_sb[:], router, writes=[r_sb])
        p.dma("sp", ident[:], ident_d, writes=[ident])
        gT = p.sb("gT", [8, NP_MAX])
        sm = p.rot_sb("sm", 2, [128, 40])
    if mode == "exp":
        G = p.rot_sb("G", 2, [128, NP_MAX])

    def mcol(s, mi, f):
        o = s * 96 + mi * 16 + f
        return mod_sb[:, o:o + 1]

    def lcol(li, f):
        return ln_sb[:, li * 16 + f:li * 16 + f + 1]

    def ln_stats(src, n):
        mps = pst.next()
        eps_ = pst.next()
        for kc in range(KC):
            s_ = sq.next()
            act(p, s_[:, :n], src[:, kc, :n], AF.Square, [src], [s_])
            mm(p, mps[:, :n], ones[:], src[:, kc, :n], kc == 0, kc == KC - 1, [ones, src], [mps])
            mm(p, eps_[:, :n], ones[:], s_[:, :n], kc == 0, kc == KC - 1, [ones, s_], [eps_])
        act(p, st_m[:, :n], mps[:, :n], AF.Copy, [mps], [st_m])
        tt(p, st_n[:, :n], st_m[:, :n], st_m[:, :n], ALU.mult, [st_m], [st_n])
        tt(p, st_r[:, :n], eps_[:, :n], st_n[:, :n], ALU.subtract, [eps_, st_n], [st_r])
        ts(p, st_r[:, :n], st_r[:, :n], EPS, None, ALU.add, None, [st_r], [st_r])
        act(p, st_r[:, :n], st_r[:, :n], AF.Sqrt, [st_r], [st_r])
        p.op("dve", lambda e: e.reciprocal(st_r[:, :n], st_r[:, :n]), reads=[st_r], writes=[st_r])
        stt(p, st_n[:, :n], st_m[:, :n], -1.0, st_r[:, :n], ALU.mult, ALU.mult, [st_m, st_r], [st_n])

    C_f = C.t[:].bitcast(F32) if C is not None else None
    H_f = H.t[:].bitcast(F32)
    out_toks = []
    t_base = 0
    for s, ns in enumerate(segs):
        for t0 in range(0, ns, NP_MAX):
            n = min(NP_MAX, ns - t0)
            g0 = t_base + t0
            if do_pre:
                p.dma("pool", H[:, :, :n], mixT[:, g0:g0 + n].rearrange("(kc q) n -> q kc n", q=128), writes=[H])
                for f in range(KC):
                    w = wts.next()
                    p.dma("pool", w[:], woutb[f], writes=[w])
                    xi = xin.next()
                    p.dma("sp", xi[:, :n], xT[f * 128:(f + 1) * 128, g0:g0 + n], writes=[xi])
                    ps = pst.next()
                    for kc in range(KC):
                        mm(p, ps[:, :n], w[:, kc * 128:(kc + 1) * 128], H[:, kc, :n], kc == 0, kc == KC - 1, [w, H], [ps])
                    act(p, xi[:, :n], xi[:, :n], AF.Copy, [xi], [xi], scale=ALPHA)
                    stt(p, C[:, f, :n], ps[:, :n], mcol(s, 2, f), xi[:, :n], ALU.mult, ALU.add, [ps, xi, mod_sb], [C])
                ln_stats(C, n)
                for f in range(KC):
                    tm = tmp.next()
                    tt(p, tm[:, :n], C_f[:, f, :n], st_r[:, :n], ALU.mult, [C, st_r], [tm])
                    tt(p, tm[:, :n], tm[:, :n], st_n[:, :n], ALU.add, [tm, st_n], [tm])
                    ts(p, Dl[:, f, :n], tm[:, :n], lcol(0, f), lcol(1, f), ALU.mult, ALU.add, [tm, ln_sb], [Dl])
                    ts(p, H[:, f, :n], Dl[:, f, :n], mcol(s, 4, f), mcol(s, 3, f), ALU.mult, ALU.add, [Dl, mod_sb], [H])
            if mode == "pre":
                for st in range(n // 128):
                    lps = pst.next()
                    for kc in range(KC):
                        mm(p, lps[:, 0:8], H_f[:, kc, st * 128:(st + 1) * 128], r_sb[:, kc * 8:(kc + 1) * 8], kc == 0, kc == KC - 1, [H, r_sb], [lps])
                    m_ = sm.next()
                    lg, mx, ex, nm, sv = m_[:, 0:8], m_[:, 8:16], m_[:, 16:24], m_[:, 24:25], m_[:, 25:26]
                    mk = m_[:, 32:40]
                    act(p, lg, lps[:, 0:8], AF.Copy, [lps], [m_])
                    p.op("dve", lambda e, mx=mx, lg=lg: e.max(out=mx, in_=lg), reads=[m_], writes=[m_])
                    ts(p, nm, mx[:, 0:1], -1.0, None, ALU.mult, None, [m_], [m_])
                    act(p, ex, lg, AF.Exp, [m_], [m_], bias=nm)
                    ts(p, mk, lg, mx[:, 1:2], None, ALU.is_ge, None, [m_], [m_])
                    tt(p, ex, ex, mk, ALU.mult, [m_], [m_])
                    p.op("dve", lambda e, sv=sv, ex=ex: e.reduce_sum(out=sv, in_=ex, axis=AX.X), reads=[m_], writes=[m_])
                    p.op("dve", lambda e, sv=sv: e.reciprocal(sv, sv), reads=[m_], writes=[m_])
                    ts(p, ex, ex, sv, None, ALU.mult, None, [m_], [m_])
                    tps = pst.next()
                    mm(p, tps[0:8, 0:128], ex, ident[:], True, True, [m_, ident], [tps])
                    act(p, gT[:, st * 128:(st + 1) * 128], tps[0:8, 0:128], AF.Copy, [tps], [gT])
                out_toks.append(p.dma("sp", g_out[:, g0:g0 + n], gT[:, :n], reads=[gT]))
                out_toks.append(p.dma("sp", xl_out[:, g0:g0 + n].rearrange("(kc q) n -> q kc n", q=128), Dl[:, :, :n], reads=[Dl]))
                out_toks.append(p.dma("sp", h_out[:, g0:g0 + n].rearrange("(kc q) n -> q kc n", q=128), H_f[:, :, :n], reads=[H]))
            if mode == "exp":
                p.dma("pool", H[:, :, :n], hT_in[:, g0:g0 + n].rearrange("(kc q) n -> q kc n", q=128), writes=[H])
                Ge = G.next()
                p.dma("sp", Ge[:, :n], grow[:, g0:g0 + n], writes=[Ge])
            if do_ffn:
                first = True
                for e_ in range(E):
                    for j in range(HC):
                        w1t, w3t, w2t = wts.next(), wts.next(), wts.next()
                        p.dma("pool", w1t[:], w1b[e_ * HC + j], writes=[w1t])
                        p.dma("pool", w3t[:], w3b[e_ * HC + j], writes=[w3t])
                        p.dma("pool", w2t[:], w2[(e_ * HC + j) * 128:(e_ * HC + j + 1) * 128, :], writes=[w2t])
                        h1, h3 = pst.next(), pst.next()
                        for kc in range(KC):
                            mm(p, h1[:, :n], w1t[:, kc * 128:(kc + 1) * 128], H[:, kc, :n], kc == 0, kc == KC - 1, [w1t, H], [h1])
                        for kc in range(KC):
                            mm(p, h3[:, :n], w3t[:, kc * 128:(kc + 1) * 128], H[:, kc, :n], kc == 0, kc == KC - 1, [w3t, H], [h3])
                        s1 = tmp.next()
                        act(p, s1[:, :n], h1[:, :n], AF.Silu, [h1], [s1])
                        a_ = a2.next()
                        if mode == "exp":
                            tt(p, s1[:, :n], s1[:, :n], h3[:, :n], ALU.mult, [s1, h3], [s1])
                            tt(p, a_[:, :n], s1[:, :n], Ge[:, :n], ALU.mult, [s1, Ge], [a_])
                        else:
                            tt(p, a_[:, :n], s1[:, :n], h3[:, :n], ALU.mult, [s1, h3], [a_])
                        for f in range(KC):
                            yp = pst.next()
                            mm(p, yp[:, :n], w2t[:, f * 128:(f + 1) * 128], a_[:, :n], True, True, [w2t, a_], [yp])
                            if first:
                                p.op("dve", lambda e, f=f, yp=yp, n=n: e.tensor_copy(A[:, f, :n], yp[:, :n]), reads=[yp], writes=[A])
                            else:
                                tt(p, A[:, f, :n], A[:, f, :n], yp[:, :n], ALU.add, [A, yp], [A])
                        first = False
            if mode == "exp":
                out_toks.append(p.dma("sp", y_out[:, g0:g0 + n].rearrange("(kc q) n -> q kc n", q=128), A[:, :, :n], reads=[A]))
            if mode == "post":
                p.dma("sp", Dl[:, :, :n], xl_in[:, g0:g0 + n].rearrange("(kc q) n -> q kc n", q=128), writes=[Dl])
                for e_ in range(8):
                    if e_ == 0:
                        p.dma("sp", A[:, :, :n], parts[0:D, g0:g0 + n].rearrange("(kc q) n -> q kc n", q=128), writes=[A])
                    else:
                        for hf in range(2):
                            pt = xin.next()
                        for kc in range(KC):
                            pt = xin.next()
                            p.dma("sp", pt[:, :n], parts[e_ * D + kc * 128:e_ * D + (kc + 1) * 128, g0:g0 + n], writes=[pt])
                            tt(p, A[:, kc, :n], A[:, kc, :n], pt[:, :n], ALU.add, [A, pt], [A])
            if do_post:
                for f in range(KC):
                    tm = tmp.next()
                    act(p, tm[:, :n], Dl[:, f, :n], AF.Copy, [Dl], [tm], scale=ALPHA)
                    stt(p, C[:, f, :n], A[:, f, :n], mcol(s, 5, f), tm[:, :n], ALU.mult, ALU.add, [A, tm, mod_sb], [C])
                ln_stats(C, n)
                for f in range(KC):
                    tm = tmp.next()
                    tt(p, tm[:, :n], C_f[:, f, :n], st_r[:, :n], ALU.mult, [C, st_r], [tm])
                    tt(p, tm[:, :n], tm[:, :n], st_n[:, :n], ALU.add, [tm, st_n], [tm])
                    xo = xout.next()
                    ts(p, xo[:, :n], tm[:, :n], lcol(2, f), lcol(3, f), ALU.mult, ALU.add, [tm, ln_sb], [xo])
                    out_toks.append(p.dma("sp", xoT[f * 128:(f + 1) * 128, g0:g0 + n], xo[:, :n], reads=[xo]))
        t_base += ns
    p.wait_final("sp", out_toks)
    p.finish()
    return nc


NPX = 1024


def build_exp(NT, HC):
    nc = bass.Bass("TRN2", target_bir_lowering=False)
    hT_in = dram_in(nc, "hT_in", [D, NT], F32R)
    grow = dram_in(nc, "grow", [128, NT])
    w1b = dram_in(nc, "w1b", [HC, 128, D], F32R)
    w3b = dram_in(nc, "w3b", [HC, 128, D], F32R)
    w2 = dram_in(nc, "w2", [HC * 128, D], F32R)
    y_out = dram_out(nc, "y_out", [D, NT])
    p = Prog(nc)
    H = p.sb("H", [128, KC, NPX], F32R)
    A = p.sb("A", [128, KC, NPX])
    wts = p.rot_sb("wts", 6, [128, D], F32R)
    tmp = p.rot_sb("tmp", 2, [128, 512])
    a2 = p.rot_sb("a2", 2, [128, 512], F32R)
    G = p.rot_sb("G", 2, [128, NPX])
    pst = p.rot_ps("ps", 8, [128, 512])
    out_toks = []
    for t0 in range(0, NT, NPX):
        n = min(NPX, NT - t0)
        p.dma("pool", H[:, :, :n], hT_in[:, t0:t0 + n].rearrange("(kc q) n -> q kc n", q=128), writes=[H])
        Ge = G.next()
        p.dma("sp", Ge[:, :n], grow[:, t0:t0 + n], writes=[Ge])
        for j in range(HC):
            w1t, w3t, w2t = wts.next(), wts.next(), wts.next()
            p.dma("pool", w1t[:], w1b[j], writes=[w1t])
            p.dma("pool", w3t[:], w3b[j], writes=[w3t])
            p.dma("pool", w2t[:], w2[j * 128:(j + 1) * 128, :], writes=[w2t])
            for c0 in range(0, n, 512):
                cn = min(512, n - c0)
                h1, h3 = pst.next(), pst.next()
                for kc in range(KC):
                    mm(p, h1[:, :cn], w1t[:, kc * 128:(kc + 1) * 128], H[:, kc, c0:c0 + cn], kc == 0, kc == KC - 1, [w1t, H], [h1])
                for kc in range(KC):
                    mm(p, h3[:, :cn], w3t[:, kc * 128:(kc + 1) * 128], H[:, kc, c0:c0 + cn], kc == 0, kc == KC - 1, [w3t, H], [h3])
                s1 = tmp.next()
                act(p, s1[:, :cn], h1[:, :cn], AF.Silu, [h1], [s1])
                tt(p, s1[:, :cn], s1[:, :cn], h3[:, :cn], ALU.mult, [s1, h3], [s1])
                a_ = a2.next()
                tt(p, a_[:, :cn], s1[:, :cn], Ge[:, c0:c0 + cn], ALU.mult, [s1, Ge], [a_])
                for f in range(KC):
                    yp = pst.next()
                    mm(p, yp[:, :cn], w2t[:, f * 128:(f + 1) * 128], a_[:, :cn], True, True, [w2t, a_], [yp])
                    if j == 0:
                        p.op("dve", lambda e, f=f, yp=yp, c0=c0, cn=cn: e.tensor_copy(A[:, f, c0:c0 + cn], yp[:, :cn]), reads=[yp], writes=[A])
                    else:
                        tt(p, A[:, f, c0:c0 + cn], A[:, f, c0:c0 + cn], yp[:, :cn], ALU.add, [A, yp], [A])
        out_toks.append(p.dma("sp", y_out[:, t0:t0 + n].rearrange("(kc q) n -> q kc n", q=128), A[:, :, :n], reads=[A]))
    p.wait_final("sp", out_toks)
    p.finish()
    return nc


_IDENT = np.eye(128, dtype=np.float32)


def run_ffn(layer, mix_x, mix_c, x, ctx, mods, inp):
    lnp = np.ascontiguousarray(np.concatenate([colT(inp["ln1_g"][layer]), colT(inp["ln1_b"][layer]),
                                               colT(inp["ln2_g"][layer]), colT(inp["ln2_b"][layer])], axis=1))
    cores = list(range(NCORES))
    if layer == 0:
        segs = [128, 2048]
        E, HC = 1, 44
        w1b, w3b = blk(inp["ffn_w1"][0]), blk(inp["ffn_w3"][0])
        w2f = np.ascontiguousarray(inp["ffn_w2"][0])
        woutb = blk(inp["e_w_out"][0])
        nc = build_ffn(segs, E, HC, False, "full")
        in_maps = []
        for k in cores:
            b, hf = k // 2, k % 2
            in_maps.append({
                "mixT": np.ascontiguousarray(np.concatenate([mix_c[b, hf * 128:(hf + 1) * 128], mix_x[b, hf * 2048:(hf + 1) * 2048]], axis=0).T),
                "xT": np.ascontiguousarray(np.concatenate([ctx[b, hf * 128:(hf + 1) * 128], x[b, hf * 2048:(hf + 1) * 2048]], axis=0).T),
                "modv": np.ascontiguousarray(np.concatenate([modcols(mods, 0, 4), modcols(mods, 0, b)], axis=1)),
                "lnp": lnp, "woutb": woutb, "w1b": w1b, "w3b": w3b, "w2": w2f})
        res = run_bass_kernel_spmd(nc, in_maps, core_ids=cores)
        x_new = np.empty_like(x)
        c_new = np.empty_like(ctx)
        for k in cores:
            b, hf = k // 2, k % 2
            o = res.results[k]["xoT"]
            x_new[b, hf * 2048:(hf + 1) * 2048] = o[:, 128:].T
            c_new[b, hf * 128:(hf + 1) * 128] = o[:, :128].T
        return x_new, c_new
    woutb = blk(inp["o_w_out"][0])
    r = inp["moe_router"][0]
    router = np.ascontiguousarray(r.reshape(KC, 128, 8).transpose(1, 0, 2)).reshape(128, KC * 8)
    nc = build_ffn([2048], 0, 0, True, "pre")
    in_maps = []
    for k in cores:
        b, hf = k // 2, k % 2
        in_maps.append({"mixT": np.ascontiguousarray(mix_x[b, hf * 2048:(hf + 1) * 2048].T),
                        "xT": np.ascontiguousarray(x[b, hf * 2048:(hf + 1) * 2048].T),
                        "modv": modcols(mods, 1, b), "lnp": lnp, "woutb": woutb, "router": router, "ident": _IDENT})
    res = run_bass_kernel_spmd(nc, in_maps, core_ids=cores)
    xl = [res.results[k]["xl_out"] for k in cores]
    hT_all = np.ascontiguousarray(np.concatenate([res.results[k]["h_out"] for k in cores], axis=1))
    g_all = np.concatenate([res.results[k]["g_out"] for k in cores], axis=1)
    NTA = hT_all.shape[1]
    nc = build_exp(NTA, 56)
    in_maps = []
    for e in cores:
        in_maps.append({"hT_in": hT_all, "grow": np.ascontiguousarray(np.broadcast_to(g_all[e], (128, NTA))),
                        "w1b": blk(inp["moe_w1"][0][e]), "w3b": blk(inp["moe_w3"][0][e]),
                        "w2": np.ascontiguousarray(inp["moe_w2"][0][e])})
    res = run_bass_kernel_spmd(nc, in_maps, core_ids=cores)
    ys = [res.results[e]["y_out"] for e in cores]
    nc = build_ffn([2048], 0, 0, True, "post")
    in_maps = []
    for k in cores:
        b, hf = k // 2, k % 2
        in_maps.append({"parts": np.ascontiguousarray(np.concatenate([ys[e][:, k * 2048:(k + 1) * 2048] for e in cores], axis=0)),
                        "xl_in": xl[k], "modv": modcols(mods, 1, b), "lnp": lnp})
    res = run_bass_kernel_spmd(nc, in_maps, core_ids=cores)
    x_new = np.empty_like(x)
    for k in cores:
        b, hf = k // 2, k % 2
        x_new[b, hf * 2048:(hf + 1) * 2048] = res.results[k]["xoT"].T
    return x_new, None


T_CTX = 256
T_LAT = 4096
T_ALL = T_CTX + T_LAT
GC = 128
NCH = T_ALL // GC


def _tri_masks():
    j = np.arange(128)[:, None]
    i = np.arange(128)[None, :]
    return np.concatenate([(i >= j).astype(np.float32), (i <= j).astype(np.float32)], axis=1)


def build_gla():
    nc = bass.Bass("TRN2", target_bir_lowering=False)
    xT = dram_in(nc, "xT", [D, T_ALL])
    modv = dram_in(nc, "modv", [128, 2 * 32])
    wfm = dram_in(nc, "wfm", [16, 128, D], F32R)
    wv = dram_in(nc, "wv", [2, 128, KC * 512], F32R)
    wlr = dram_in(nc, "wlr", [128, 2 * KC * 16], F32R)
    gw2 = dram_in(nc, "gw2", [16, 2 * 512], F32R)
    gb = dram_in(nc, "gb", [128, 8])
    nw = dram_in(nc, "nw", [128, 4])
    cst = dram_in(nc, "cst", [128, 512])
    out = dram_out(nc, "out", [1024, T_LAT])
    q_s = nc.dram_tensor("q_s", [512, T_ALL], F32, kind="Internal").ap()
    k_s = nc.dram_tensor("k_s", [512, T_ALL], F32, kind="Internal").ap()
    r_s = nc.dram_tensor("r_s", [1024, T_ALL], F32, kind="Internal").ap()
    g_s = nc.dram_tensor("g_s", [2, 512, T_ALL], F32, kind="Internal").ap()
    v_s = nc.dram_tensor("v_s", [T_ALL, 1024], F32, kind="Internal").ap()
    o_s = nc.dram_tensor("o_s", [512, T_LAT], F32, kind="Internal").ap()
    RQ, RK, RR, RG, RV, RO = Res("q_s"), Res("k_s"), Res("r_s"), Res("g_s"), Res("v_s"), Res("o_s")
    p = Prog(nc)

    mod_sb = p.sb("mod_sb", [128, 64])
    gb_sb = p.sb("gb_sb", [128, 8])
    nw_sb = p.sb("nw_sb", [128, 4])
    cst_sb = p.sb("cst_sb", [128, 512])
    cst_r = p.sb("cst_r", [128, 512], F32R)
    wlr_sb = p.sb("wlr_sb", [128, 2 * KC * 16], F32R)
    gw2_sb = p.sb("gw2_sb", [16, 1024], F32R)
    pst = p.rot_ps("ps", 8, [128, 512])
    p.dma("sp", mod_sb[:], modv, writes=[mod_sb])
    p.dma("sp", gb_sb[:], gb, writes=[gb_sb])
    p.dma("sp", nw_sb[:], nw, writes=[nw_sb])
    p.dma("sp", cst_sb[:], cst, writes=[cst_sb])
    p.dma("pool", wlr_sb[:], wlr, writes=[wlr_sb])
    p.dma("pool", gw2_sb[:], gw2, writes=[gw2_sb])
    act(p, cst_r[:], cst_sb[:], AF.Copy, [cst_sb], [cst_r])
    ts(p, gb_sb[:], gb_sb[:], -1.0, None, ALU.mult, None, [gb_sb], [gb_sb])
    for s in range(2):
        ts(p, mod_sb[:, s * 32 + 16:s * 32 + 32], mod_sb[:, s * 32 + 16:s * 32 + 32], 1.0, None, ALU.add, None, [mod_sb], [mod_sb])
    maskf, maskr = cst_sb[:, 0:128], cst_sb[:, 128:256]
    ident_r = cst_r[:, 256:384]
    ones_f = cst_sb[:, 384:512]

    xt = p.sb("xt", [128, KC, 512])
    hT = p.sb("hT", [128, KC, 512], F32R)
    wblk = p.rot_sb("wblk", 3, [128, D], F32R)
    wv_sb = p.sb("wv_sb", [128, KC * 512], F32R)
    ev = p.rot_sb("ev", 3, [128, 512])
    lr_sb = p.rot_sb("lr", 2, [16, 512], F32R)
    tiles = [(0, 0, T_CTX)] + [(1, T_CTX + i * 512, 512) for i in range(T_LAT // 512)]
    for (s, t0, n) in tiles:
        p.dma("sp", xt[:, :, :n], xT[:, t0:t0 + n].rearrange("(kc q) n -> q kc n", q=128), writes=[xt])
        for kc in range(KC):
            ts(p, hT[:, kc, :n], xt[:, kc, :n], mod_sb[:, s * 32 + 16 + kc:s * 32 + 17 + kc], mod_sb[:, s * 32 + kc:s * 32 + kc + 1],
               ALU.mult, ALU.add, [xt, mod_sb], [hT])
        for m in range(16):
            w = wblk.next()
            p.dma("pool", w[:], wfm[m], writes=[w])
            ps = pst.next()
            for kc in range(KC):
                mm(p, ps[:, :n], w[:, kc * 128:(kc + 1) * 128], hT[:, kc, :n], kc == 0, kc == KC - 1, [w, hT], [ps])
            e_ = ev.next()
            act(p, e_[:, :n], ps[:, :n], AF.Copy, [ps], [e_])
            if m < 4:
                dst, R_ = q_s[m * 128:(m + 1) * 128, t0:t0 + n], RQ
            elif m < 8:
                dst, R_ = k_s[(m - 4) * 128:(m - 3) * 128, t0:t0 + n], RK
            else:
                dst, R_ = r_s[(m - 8) * 128:(m - 7) * 128, t0:t0 + n], RR
            p.dma("sp", dst, e_[:, :n], reads=[e_], writes=[R_])
        for d in range(2):
            ps = pst.next()
            for kc in range(KC):
                o_ = (d * KC + kc) * 16
                mm(p, ps[0:16, :n], wlr_sb[:, o_:o_ + 16], hT[:, kc, :n], kc == 0, kc == KC - 1, [wlr_sb, hT], [ps])
            l_ = lr_sb.next()
            act(p, l_[:, :n], ps[0:16, :n], AF.Copy, [ps], [l_])
            for blk_ in range(4):
                ps2 = pst.next()
                mm(p, ps2[:, :n], gw2_sb[:, d * 512 + blk_ * 128:d * 512 + (blk_ + 1) * 128], l_[:, :n], True, True, [gw2_sb, l_], [ps2])
                e_ = ev.next()
                act(p, e_[:, :n], ps2[:, :n], AF.Exp, [ps2, gb_sb], [e_], scale=-1.0, bias=gb_sb[:, d * 4 + blk_:d * 4 + blk_ + 1])
                act(p, e_[:, :n], e_[:, :n], AF.Ln, [e_], [e_], bias=1.0)
                ts(p, e_[:, :n], e_[:, :n], -1.0 / 16.0, None, ALU.mult, None, [e_], [e_])
                p.dma("sp", g_s[d, blk_ * 128:(blk_ + 1) * 128, t0:t0 + n], e_[:, :n], reads=[e_], writes=[RG])
        for h in range(2):
            p.dma("pool", wv_sb[:], wv[h], writes=[wv_sb])
            for st in range(n // 128):
                ps = pst.next()
                for kc in range(KC):
                    mm(p, ps[:, :], hT[:, kc, st * 128:(st + 1) * 128], wv_sb[:, kc * 512:(kc + 1) * 512], kc == 0, kc == KC - 1, [wv_sb, hT], [ps])
                e_ = ev.next()
                act(p, e_[:, :], ps[:, :], AF.Copy, [ps], [e_])
                p.dma("sp", v_s[t0 + st * 128:t0 + (st + 1) * 128, h * 512:(h + 1) * 512], e_[:, :], reads=[e_], writes=[RV])

    S32 = p.sb("S32", [128, 2, 512])
    Sr = p.sb("Sr", [128, 2, 512], F32R)
    qc_p = p.rot_sb("qc", 2, [128, 2, GC])
    kc_p = p.rot_sb("kc", 2, [128, 2, GC])
    gc_p = p.rot_sb("gc", 2, [128, 2, GC])
    vc_p = p.rot_sb("vc", 2, [128, 512], F32R)
    cum_p = p.rot_sb("cum", 2, [128, 2, GC])
    e_p = p.rot_sb("e", 2, [128, 2, GC])
    ei_p = p.rot_sb("ei", 2, [128, 2, GC])
    qi_p = p.rot_sb("qi", 2, [128, 2, GC], F32R)
    ki_p = p.rot_sb("ki", 2, [128, 2, GC], F32R)
    kd_p = p.rot_sb("kd", 2, [128, 2, GC], F32R)
    kdT_p = p.rot_sb("kdT", 2, [128, 256], F32R)
    aT_p = p.rot_sb("aT", 2, [128, GC], F32R)
    o_p = p.rot_sb("o", 2, [128, 4, GC])
    o0_p = p.rot_sb("o0", 2, [128, 4, GC])
    rc_p = p.rot_sb("rc", 2, [128, 4, GC])
    sq_p = p.rot_sb("sq", 2, [128, 4, GC], F32R)
    rs_p = p.rot_sb("rs", 2, [128, GC])
    out_toks = []
    for h in range(2):
        for d in range(2):
            p.op("dve", lambda e: e.memset(S32[:], 0.0), writes=[S32])
            act(p, Sr[:], S32[:], AF.Copy, [S32], [Sr])
            ctx_ch = [0, 1]
            lat_ch = list(range(2, NCH))
            order = (ctx_ch + lat_ch) if d == 0 else (ctx_ch[::-1] + lat_ch[::-1])
            for c in order:
                t0 = c * GC
                is_lat = c >= 2
                qc, kc_, gcn, vc = qc_p.next(), kc_p.next(), gc_p.next(), vc_p.next()
                p.dma("sp", kc_[:], k_s[h * 256:(h + 1) * 256, t0:t0 + GC].rearrange("(c q) n -> q c n", q=128), reads=[RK], writes=[kc_])
                p.dma("sp", gcn[:], g_s[d, h * 256:(h + 1) * 256, t0:t0 + GC].rearrange("(c q) n -> q c n", q=128), reads=[RG], writes=[gcn])
                p.dma("pool", vc[:], v_s[t0:t0 + GC, h * 512:(h + 1) * 512], reads=[RV], writes=[vc])
                cum, e_, ei = cum_p.next(), e_p.next(), ei_p.next()
                for k2 in range(2):
                    if d == 0:
                        p.op("dve", lambda e, cum=cum, gcn=gcn, k2=k2: e.tensor_tensor_scan(out=cum[:, k2, :], data0=ones_f, data1=gcn[:, k2, :], initial=0.0, op0=ALU.mult, op1=ALU.add),
                             reads=[gcn, cst_sb], writes=[cum])
                    else:
                        p.op("dve", lambda e, cum=cum, gcn=gcn, k2=k2: e.tensor_tensor_scan(out=cum[:, k2, ::-1], data0=ones_f, data1=gcn[:, k2, ::-1], initial=0.0, op0=ALU.mult, op1=ALU.add),
                             reads=[gcn, cst_sb], writes=[cum])
                act(p, ei[:], cum[:], AF.Exp, [cum], [ei], scale=-1.0)
                act(p, e_[:], cum[:], AF.Exp, [cum], [e_])
                ki, kd = ki_p.next(), kd_p.next()
                tt(p, ki[:], kc_[:], ei[:], ALU.mult, [kc_, ei], [ki])
                last = GC - 1 if d == 0 else 0
                for k2 in range(2):
                    ts(p, kd[:, k2, :], ki[:, k2, :], e_[:, k2, last:last + 1], None, ALU.mult, None, [ki, e_], [kd])
                tp = pst.next()
                for k2 in range(2):
                    mm(p, tp[:, k2 * 128:(k2 + 1) * 128], kd[:, k2, :], ident_r, True, True, [kd, cst_r], [tp])
                kdT = kdT_p.next()
                act(p, kdT[:], tp[:, 0:256], AF.Copy, [tp], [kdT])
                if is_lat:
                    qc = qc_p.next()
                    p.dma("sp", qc[:], q_s[h * 256:(h + 1) * 256, t0:t0 + GC].rearrange("(c q) n -> q c n", q=128), reads=[RQ], writes=[qc])
                    qi = qi_p.next()
                    stt(p, qi[:], qc[:], 1.0 / 16.0, e_[:], ALU.mult, ALU.mult, [qc, e_], [qi])
                    ap_ = pst.next()
                    for k2 in range(2):
                        mm(p, ap_[:, 0:GC], ki[:, k2, :], qi[:, k2, :], k2 == 0, k2 == 1, [ki, qi], [ap_])
                    aT = aT_p.next()
                    tt(p, aT[:], ap_[:, 0:GC], maskf if d == 0 else maskr, ALU.mult, [ap_, cst_sb], [aT])
                    op_ = pst.next()
                    for vch in range(4):
                        for k2 in range(2):
                            mm(p, op_[:, vch * GC:(vch + 1) * GC], Sr[:, k2, vch * 128:(vch + 1) * 128], qi[:, k2, :], k2 == 0, False, [Sr, qi], [op_])
                        mm(p, op_[:, vch * GC:(vch + 1) * GC], vc[:, vch * 128:(vch + 1) * 128], aT[:], False, True, [vc, aT], [op_])
                    tl = t0 - T_CTX
                    if d == 0:
                        o_ = o_p.next()
                        act(p, o_[:].rearrange("q a b -> q (a b)"), op_[:, :], AF.Copy, [op_], [o_])
                        p.dma("sp", o_s[:, tl:tl + GC].rearrange("(c q) n -> q c n", q=128), o_[:], reads=[o_], writes=[RO])
                    else:
                        o0, rc = o0_p.next(), rc_p.next()
                        p.dma("sp", o0[:], o_s[:, tl:tl + GC].rearrange("(c q) n -> q c n", q=128), reads=[RO], writes=[o0])
                        p.dma("sp", rc[:], r_s[h * 512:(h + 1) * 512, t0:t0 + GC].rearrange("(c q) n -> q c n", q=128), reads=[RR], writes=[rc])
                        o_ = o_p.next()
                        tt(p, o_[:].rearrange("q a b -> q (a b)"), op_[:, :], o0[:].rearrange("q a b -> q (a b)"), ALU.add, [op_, o0], [o_])
                        sq_ = sq_p.next()
                        act(p, sq_[:], o_[:], AF.Square, [o_], [sq_])
                        mp = pst.next()
                        for vch in range(4):
                            mm(p, mp[:, 0:GC], cst_r[:, 384:512], sq_[:, vch, :], vch == 0, vch == 3, [cst_r, sq_], [mp])
                        rs = rs_p.next()
                        ts(p, rs[:], mp[:, 0:GC], 1.0 / 512.0, EPS, ALU.mult, ALU.add, [mp], [rs])
                        act(p, rs[:], rs[:], AF.Sqrt, [rs], [rs])
                        p.op("dve", lambda e, rs=rs: e.reciprocal(rs[:], rs[:]), reads=[rs], writes=[rs])
                        act(p, rc[:], rc[:], AF.Silu, [rc], [rc])
                        for vch in range(4):
                            tt(p, o_[:, vch, :], o_[:, vch, :], rs[:], ALU.mult, [o_, rs], [o_])
                            stt(p, o_[:, vch, :], o_[:, vch, :], nw_sb[:, vch:vch + 1], rc[:, vch, :], ALU.mult, ALU.mult, [o_, nw_sb, rc], [o_])
                        out_toks.append(p.dma("sp", out[h * 512:(h + 1) * 512, tl:tl + GC].rearrange("(c q) n -> q c n", q=128), o_[:], reads=[o_]))
                for k2 in range(2):
                    sp_ = pst.next()
                    mm(p, sp_[:, :], kdT[:, k2 * 128:(k2 + 1) * 128], vc[:], True, True, [kdT, vc], [sp_])
                    stt(p, S32[:, k2, :], S32[:, k2, :], e_[:, k2, last:last + 1], sp_[:, :], ALU.mult, ALU.add, [S32, e_, sp_], [S32])
                act(p, Sr[:], S32[:], AF.Copy, [S32], [Sr])
    p.wait_final("sp", out_toks)
    p.finish()
    return nc


def run_gla(x, ctx, mods, inp):
    w_in = inp["o_w_in"][0]
    cst = np.ascontiguousarray(np.concatenate([_tri_masks(), np.eye(128, dtype=np.float32), np.ones((128, 128), np.float32)], axis=1))
    nc = build_gla()
    in_maps = []
    for k in range(NCORES):
        b, hh = k // 2, k % 2
        heads = [2 * hh, 2 * hh + 1]
        cols = []
        for h in heads:
            cols += list(range(h * 256, (h + 1) * 256))
        for h in heads:
            cols += list(range(1024 + h * 256, 1024 + (h + 1) * 256))
        for h in heads:
            cols += list(range(4096 + h * 512, 4096 + (h + 1) * 512))
        wfm = blk(np.ascontiguousarray(w_in[:, cols]))
        wv = np.stack([np.ascontiguousarray(w_in[:, 2048 + h * 512:2048 + (h + 1) * 512].reshape(KC, 128, 512).transpose(1, 0, 2)).reshape(128, KC * 512) for h in heads])
        wlr = np.ascontiguousarray(w_in[:, 6144:6176].reshape(KC, 128, 2, 16).transpose(1, 2, 0, 3)).reshape(128, 2 * KC * 16)
        dk0 = heads[0] * 256
        gw2 = np.ascontiguousarray(inp["gla_w2"][0][:, :, dk0:dk0 + 512].transpose(1, 0, 2)).reshape(16, 1024)
        gbv = inp["gla_b"][0][:, dk0:dk0 + 512]
        gb = np.ascontiguousarray(gbv.reshape(2, 4, 128).transpose(2, 0, 1)).reshape(128, 8)
        nw = colT(inp["gla_norm_w"][0])
        mv = np.concatenate([colT(mods[4, 1, 0]), colT(mods[4, 1, 1]), colT(mods[b, 1, 0]), colT(mods[b, 1, 1])], axis=1)
        xT = np.ascontiguousarray(np.concatenate([ctx[b], x[b]], axis=0).T)
        in_maps.append({"xT": xT, "modv": np.ascontiguousarray(mv), "wfm": wfm, "wv": wv, "wlr": wlr, "gw2": gw2, "gb": gb,
                        "nw": np.ascontiguousarray(nw), "cst": cst})
    res = run_bass_kernel_spmd(nc, in_maps, core_ids=list(range(NCORES)))
    mix = np.empty((4, T_LAT, D), np.float32)
    for k in range(NCORES):
        b, hh = k // 2, k % 2
        mix[b, :, hh * 1024:(hh + 1) * 1024] = res.results[k]["out"].T
    return mix


MAGIC = 12582912.0
TWO_PI = float(2 * np.pi)
CW1 = 6.28125
CW2 = float(2 * np.pi - 6.28125)
HALF_PI = float(np.pi / 2)
SCH = 1024


def emit_sin(p, out, x, k_t, reads, n_slice):
    rd = list(reads)
    ts(p, k_t, x, 1.0 / TWO_PI, MAGIC, ALU.mult, ALU.add, rd, [n_slice[0]])
    ts(p, k_t, k_t, -MAGIC, None, ALU.add, None, [n_slice[0]], [n_slice[0]])
    stt(p, x, k_t, -CW1, x, ALU.mult, ALU.add, [n_slice[0]] + rd, rd)
    stt(p, x, k_t, -CW2, x, ALU.mult, ALU.add, [n_slice[0]] + rd, rd)
    ts(p, x, x, 3.14159, -3.14159, ALU.min, ALU.max, rd, rd)
    act(p, out, x, AF.Sin, rd, [n_slice[1]])


def build_mix0(do_s5=True, do_dn=True):
    nc = bass.Bass("TRN2", target_bir_lowering=False)
    xT = dram_in(nc, "xT", [D, T_ALL])
    modv = dram_in(nc, "modv", [128, 64])
    wfm = dram_in(nc, "wfm", [20, 128, D], F32R)
    wtm = dram_in(nc, "wtm", [128, KC * 528], F32R)
    s5par = dram_in(nc, "s5par", [128, 3 * 32])
    s5bw = dram_in(nc, "s5bw", [4, 128, 2048], F32R)
    s5cw = dram_in(nc, "s5cw", [4, 128, 2048], F32R)
    s5d = dram_in(nc, "s5d", [128, 4])
    iota_d = dram_in(nc, "iota", [128, T_LAT])
    out_s5 = dram_out(nc, "out_s5", [512, T_ALL])
    dnpar_d = dram_in(nc, "dnpar", [128, 16])
    dncw_d = dram_in(nc, "dncw", [128, 12 * 9])
    dnnw_d = dram_in(nc, "dnnw", [128, 128])
    dncst_d = dram_in(nc, "dncst", [128, 6 * 128])
    out_dn = dram_out(nc, "out_dn", [T_ALL, 512])
    o0_s = nc.dram_tensor("o0_s", [T_ALL, 256], F32, kind="Internal").ap()
    RO0 = Res("o0_s")
    fm_s = nc.dram_tensor("fm_s", [20 * 128, T_ALL], F32, kind="Internal").ap()
    tm_s = nc.dram_tensor("tm_s", [T_ALL, 528], F32, kind="Internal").ap()
    RFM, RTM = Res("fm_s"), Res("tm_s")
    p = Prog(nc)
    pst = p.rot_ps("ps", 8, [128, 512])
    mod_sb = p.sb("mod_sb", [128, 64])
    p.dma("sp", mod_sb[:], modv, writes=[mod_sb])
    for s in range(2):
        ts(p, mod_sb[:, s * 32 + 16:s * 32 + 32], mod_sb[:, s * 32 + 16:s * 32 + 32], 1.0, None, ALU.add, None, [mod_sb], [mod_sb])

    p.begin_phase()
    xt = p.sb("xt", [128, KC, 512])
    hT = p.sb("hT", [128, KC, 512], F32R)
    wblk = p.rot_sb("wblk", 3, [128, D], F32R)
    wtm_sb = p.sb("wtm_sb", [128, KC * 528], F32R)
    ev = p.rot_sb("ev", 3, [128, 528])
    p.dma("pool", wtm_sb[:], wtm, writes=[wtm_sb])
    tiles = [(0, 0, T_CTX)] + [(1, T_CTX + i * 512, 512) for i in range(T_LAT // 512)]
    for (s, t0, n) in tiles:
        p.dma("sp", xt[:, :, :n], xT[:, t0:t0 + n].rearrange("(kc q) n -> q kc n", q=128), writes=[xt])
        for kc in range(KC):
            ts(p, hT[:, kc, :n], xt[:, kc, :n], mod_sb[:, s * 32 + 16 + kc:s * 32 + 17 + kc], mod_sb[:, s * 32 + kc:s * 32 + kc + 1],
               ALU.mult, ALU.add, [xt, mod_sb], [hT])
        for m in range(20):
            w = wblk.next()
            p.dma("pool", w[:], wfm[m], writes=[w])
            ps = pst.next()
            for kc in range(KC):
                mm(p, ps[:, :n], w[:, kc * 128:(kc + 1) * 128], hT[:, kc, :n], kc == 0, kc == KC - 1, [w, hT], [ps])
            e_ = ev.next()
            act(p, e_[:, :n], ps[:, :n], AF.Copy, [ps], [e_])
            p.dma("sp", fm_s[m * 128:(m + 1) * 128, t0:t0 + n], e_[:, :n], reads=[e_], writes=[RFM])
        for st in range(n // 128):
            ps = pst.next()
            ps2 = pst.next()
            for kc in range(KC):
                mm(p, ps[:, :], hT[:, kc, st * 128:(st + 1) * 128], wtm_sb[:, kc * 528:kc * 528 + 512], kc == 0, kc == KC - 1, [wtm_sb, hT], [ps])
            for kc in range(KC):
                mm(p, ps2[:, 0:16], hT[:, kc, st * 128:(st + 1) * 128], wtm_sb[:, kc * 528 + 512:kc * 528 + 528], kc == 0, kc == KC - 1, [wtm_sb, hT], [ps2])
            e_ = ev.next()
            act(p, e_[:, 0:512], ps[:, :], AF.Copy, [ps], [e_])
            act(p, e_[:, 512:528], ps2[:, 0:16], AF.Copy, [ps2], [e_])
            p.dma("sp", tm_s[t0 + st * 128:t0 + (st + 1) * 128, :], e_[:, :], reads=[e_], writes=[RTM])

    p.end_phase()
    out_toks = []
    if do_s5:
        p.begin_phase()
        par = p.sb("par", [128, 96])
        iota = p.sb("iota_sb", [128, T_LAT])
        d_sb = p.sb("d_sb", [128, 4])
        p.dma("sp", par[:], s5par, writes=[par])
        p.dma("sp", iota[:], iota_d, writes=[iota])
        p.dma("sp", d_sb[:], s5d, writes=[d_sb])
        cs = p.sb("cs", [128, 12, 32])
        LRE, LIM, LS = par[:, 0:32], par[:, 32:64], par[:, 64:96]
        (DEL, MAG, ANG, SINA, COSA, ARE, AIM, RDEN, FRE, FIM, T1, T2) = [cs[:, i, :] for i in range(12)]
        RC, RP = [cs], [par]
        act(p, DEL, LS, AF.Exp, RP, RC)
        tt(p, MAG, LRE, DEL, ALU.mult, RP + RC, RC)
        act(p, MAG, MAG, AF.Exp, RC, RC)
        tt(p, ANG, LIM, DEL, ALU.mult, RP + RC, RC)
        ts(p, T1, ANG, 1.0, None, ALU.mult, None, RC, RC)
        emit_sin(p, SINA, T1, T2, RC, (cs, cs))
        ts(p, T1, ANG, HALF_PI, None, ALU.add, None, RC, RC)
        emit_sin(p, COSA, T1, T2, RC, (cs, cs))
        tt(p, ARE, MAG, COSA, ALU.mult, RC, RC)
        tt(p, AIM, MAG, SINA, ALU.mult, RC, RC)
        tt(p, T1, LRE, LRE, ALU.mult, RP, RC)
        tt(p, T2, LIM, LIM, ALU.mult, RP, RC)
        tt(p, RDEN, T1, T2, ALU.add, RC, RC)
        p.op("dve", lambda e: e.reciprocal(RDEN, RDEN), reads=RC, writes=RC)
        ts(p, T1, ARE, -1.0, None, ALU.add, None, RC, RC)
        tt(p, FRE, T1, LRE, ALU.mult, RC + RP, RC)
        tt(p, T2, AIM, LIM, ALU.mult, RC + RP, RC)
        tt(p, FRE, FRE, T2, ALU.add, RC, RC)
        tt(p, FRE, FRE, RDEN, ALU.mult, RC, RC)
        tt(p, FIM, AIM, LRE, ALU.mult, RC + RP, RC)
        tt(p, T2, T1, LIM, ALU.mult, RC + RP, RC)
        tt(p, FIM, FIM, T2, ALU.subtract, RC, RC)
        tt(p, FIM, FIM, RDEN, ALU.mult, RC, RC)

        U = p.sb("U", [128, T_ALL], F32R)
        Y = p.sb("Y", [128, T_ALL])
        bw = p.sb("bw", [128, 2048], F32R)
        cw = p.sb("cw", [128, 2048], F32R)
        tabs = p.sb("tabs", [128, 4, SCH])
        kt = p.sb("kt", [128, SCH])
        bu = p.rot_sb("bu", 2, [128, 2, SCH])
        vv = p.rot_sb("vv", 2, [128, 2, SCH])
        gg = p.rot_sb("gg", 2, [128, 2, SCH])
        hh_ = p.rot_sb("hh", 2, [128, 2, SCH], F32R)
        tq = p.rot_sb("tq", 2, [128, SCH])
        ini = p.rot_sb("ini", 2, [128, 4])
        S_t, C_t, Ec, Es = tabs[:, 0, :], tabs[:, 1, :], tabs[:, 2, :], tabs[:, 3, :]
        for blk_ in range(4):
            p.dma("pool", U[:], fm_s[(12 + blk_) * 128:(13 + blk_) * 128, :], reads=[RFM], writes=[U])
            p.dma("pool", bw[:], s5bw[blk_], writes=[bw])
            p.dma("pool", cw[:], s5cw[blk_], writes=[cw])
            first_y = True
            for r4 in range(4):
                r = blk_ * 4 + r4
                half = r4 // 2
                for d in range(2):
                    col = d * 16 + r
                    th = ANG[:, col:col + 1]
                    rr = MAG[:, col:col + 1]
                    carry = None
                    chunks = [(0, 0, T_CTX)] + [(1, i * SCH, SCH) for i in range(T_LAT // SCH)]
                    for (seg, tau0, n) in chunks:
                        seg_off, seg_len = (0, T_CTX) if seg == 0 else (T_CTX, T_LAT)
                        if d == 0:
                            n0 = seg_off + tau0
                        else:
                            n0 = seg_off + seg_len - tau0 - n
                        rev = (d == 1)

                        def nat(ap):
                            return ap[:, ::-1] if rev else ap
                        ts(p, S_t[:, :n], iota[:, tau0:tau0 + n], th, None, ALU.mult, None, [iota, cs], [tabs])
                        ts(p, C_t[:, :n], S_t[:, :n], HALF_PI, None, ALU.add, None, [tabs], [tabs])
                        emit_sin(p, S_t[:, :n], S_t[:, :n], kt[:, :n], [tabs], (kt, tabs))
                        emit_sin(p, C_t[:, :n], C_t[:, :n], kt[:, :n], [tabs], (kt, tabs))
                        fre, fim = FRE[:, col:col + 1], FIM[:, col:col + 1]
                        ts(p, Ec[:, :n], C_t[:, :n], fre, None, ALU.mult, None, [tabs, cs], [tabs])
                        stt(p, Ec[:, :n], S_t[:, :n], fim, Ec[:, :n], ALU.mult, ALU.add, [tabs, cs], [tabs])
                        ts(p, Es[:, :n], S_t[:, :n], fre, None, ALU.mult, None, [tabs, cs], [tabs])
                        stt(p, Es[:, :n], C_t[:, :n], fim, Es[:, :n], ALU.mult, ALU.subtract, [tabs, cs], [tabs])
                        b_ = bu.next()
                        for reim in range(2):
                            wcol = ((d * 4 + r4) * 2 + reim) * 128
                            for c0 in range(0, n, 512):
                                cn = min(512, n - c0)
                                ps = pst.next()
                                mm(p, ps[:, :cn], bw[64 * half:64 * half + 64, wcol:wcol + 128], U[64 * half:64 * half + 64, n0 + c0:n0 + c0 + cn],
                                   True, True, [bw, U], [ps])
                                act(p, b_[:, reim, c0:c0 + cn], ps[:, :cn], AF.Copy, [ps], [b_])
                        bre, bim = nat(b_[:, 0, :n]), nat(b_[:, 1, :n])
                        v_ = vv.next()
                        t1, t2 = tq.next(), tq.next()
                        tt(p, t1[:, :n], Ec[:, :n], bre, ALU.mult, [tabs, b_], [t1])
                        tt(p, t2[:, :n], Es[:, :n], bim, ALU.mult, [tabs, b_], [t2])
                        tt(p, v_[:, 0, :n], t1[:, :n], t2[:, :n], ALU.subtract, [t1, t2], [v_])
                        tt(p, t1[:, :n], Ec[:, :n], bim, ALU.mult, [tabs, b_], [t1])
                        tt(p, t2[:, :n], Es[:, :n], bre, ALU.mult, [tabs, b_], [t2])
                        tt(p, v_[:, 1, :n], t1[:, :n], t2[:, :n], ALU.add, [t1, t2], [v_])
                        g_ = gg.next()
                        if seg == 1 and tau0 == 0:
                            i_ = ini.next()
                            pg = carry
                            c256, s256 = C_t[:, 256:257], S_t[:, 256:257]
                            tt(p, i_[:, 0:1], pg[:, 0, T_CTX - 1:T_CTX], c256, ALU.mult, [pg, tabs], [i_])
                            tt(p, i_[:, 1:2], pg[:, 1, T_CTX - 1:T_CTX], s256, ALU.mult, [pg, tabs], [i_])
                            tt(p, i_[:, 2:3], i_[:, 0:1], i_[:, 1:2], ALU.subtract, [i_], [i_])
                            tt(p, i_[:, 0:1], pg[:, 0, T_CTX - 1:T_CTX], s256, ALU.mult, [pg, tabs], [i_])
                            tt(p, i_[:, 1:2], pg[:, 1, T_CTX - 1:T_CTX], c256, ALU.mult, [pg, tabs], [i_])
                            tt(p, i_[:, 3:4], i_[:, 0:1], i_[:, 1:2], ALU.add, [i_], [i_])
                            inits = (i_[:, 2:3], i_[:, 3:4], i_)
                        elif carry is None:
                            inits = (0.0, 0.0, None)
                        else:
                            pn = carry_n
                            inits = (carry[:, 0, pn - 1:pn], carry[:, 1, pn - 1:pn], carry)
                        for reim in range(2):
                            rd = [v_, cs] + ([inits[2]] if inits[2] is not None else [])
                            p.op("dve", lambda e, g_=g_, v_=v_, reim=reim, n=n, ini_=inits[reim], rr=rr: e.tensor_tensor_scan(
                                out=g_[:, reim, :n], data0=rr.to_broadcast([128, n]), data1=v_[:, reim, :n], initial=ini_, op0=ALU.mult, op1=ALU.add),
                                reads=rd, writes=[g_])
                        carry, carry_n = g_, n
                        h_ = hh_.next()
                        t1, t2 = tq.next(), tq.next()
                        tt(p, t1[:, :n], C_t[:, :n], g_[:, 0, :n], ALU.mult, [tabs, g_], [t1])
                        tt(p, t2[:, :n], S_t[:, :n], g_[:, 1, :n], ALU.mult, [tabs, g_], [t2])
                        tt(p, nat(h_[:, 0, :n]), t1[:, :n], t2[:, :n], ALU.subtract, [t1, t2], [h_])
                        tt(p, t1[:, :n], S_t[:, :n], g_[:, 0, :n], ALU.mult, [tabs, g_], [t1])
                        tt(p, t2[:, :n], C_t[:, :n], g_[:, 1, :n], ALU.mult, [tabs, g_], [t2])
                        stt(p, nat(h_[:, 1, :n]), t1[:, :n], -1.0, t2[:, :n], ALU.mult, ALU.subtract, [t1, t2], [h_])
                        for c0 in range(0, n, 512):
                            cn = min(512, n - c0)
                            ps = pst.next()
                            for reim in range(2):
                                wcol = ((d * 4 + r4) * 2 + reim) * 128
                                mm(p, ps[:, :cn], cw[:, wcol:wcol + 128], h_[:, reim, c0:c0 + cn], reim == 0, reim == 1, [cw, h_], [ps])
                            ysl = Y[:, n0 + c0:n0 + c0 + cn]
                            if first_y:
                                p.op("dve", lambda e, ysl=ysl, ps=ps, cn=cn: e.tensor_copy(ysl, ps[:, :cn]), reads=[ps], writes=[Y])
                            else:
                                tt(p, ysl, ysl, ps[:, :cn], ALU.add, [Y, ps], [Y])
                    first_y = False
            for c0 in range(0, T_ALL, SCH):
                cn = min(SCH, T_ALL - c0)
                g1, g2 = tq.next(), tq.next()
                U_f = U.t[:].bitcast(F32)
                stt(p, Y[:, c0:c0 + cn], U_f[:, c0:c0 + cn], d_sb[:, blk_:blk_ + 1], Y[:, c0:c0 + cn], ALU.mult, ALU.add, [U, d_sb, Y], [Y])
                tt(p, g1[:, :cn], Y[:, c0:c0 + cn], Y[:, c0:c0 + cn], ALU.mult, [Y], [g1])
                ts(p, g1[:, :cn], g1[:, :cn], 0.044715, 1.0, ALU.mult, ALU.add, [g1], [g1])
                tt(p, g1[:, :cn], g1[:, :cn], Y[:, c0:c0 + cn], ALU.mult, [g1, Y], [g1])
                act(p, g1[:, :cn], g1[:, :cn], AF.Sigmoid, [g1], [g1], scale=1.5957691216057308)
                tt(p, g1[:, :cn], g1[:, :cn], Y[:, c0:c0 + cn], ALU.mult, [g1, Y], [g1])
                p.dma("sp", g2[:, :cn], fm_s[(16 + blk_) * 128:(17 + blk_) * 128, c0:c0 + cn], reads=[RFM], writes=[g2])
                act(p, g2[:, :cn], g2[:, :cn], AF.Sigmoid, [g2], [g2])
                tt(p, g1[:, :cn], g1[:, :cn], g2[:, :cn], ALU.mult, [g1, g2], [g1])
                out_toks.append(p.dma("sp", out_s5[blk_ * 128:(blk_ + 1) * 128, c0:c0 + cn], g1[:, :cn], reads=[g1]))
        p.end_phase()

    if do_dn:
        p.begin_phase()
        dnpar = p.sb("dnpar_sb", [128, 16])
        dncw = p.sb("dncw_sb", [128, 108])
        dnnw = p.sb("dnnw_sb", [128, 128])
        cst = p.sb("dncst_sb", [128, 768])
        for t_, d_ in ((dnpar, dnpar_d), (dncw, dncw_d), (dnnw, dnnw_d), (cst, dncst_d)):
            p.dma("sp", t_[:], d_, writes=[t_])
        LE, GE, GT, LT, IDN, ONE = [cst[:, i * 128:(i + 1) * 128] for i in range(6)]
        nexpa = p.sb("nexpa", [128, 8])
        act(p, nexpa[:], dnpar[:, 0:8], AF.Exp, [dnpar], [nexpa])
        ts(p, nexpa[:], nexpa[:], -1.0, None, ALU.mult, None, [nexpa], [nexpa])
        raw = p.sb("raw", [128, T_ALL])
        qkvs = [[p.sb(f"qkv{a}_{i}", [128, T_ALL]) for i in range(3)] for a in range(2)]
        sqb = p.rot_sb("sqb", 2, [128, 512])
        rsb = p.rot_sb("rsb", 2, [128, 512])
        Ss = [p.sb(f"S_dn{a}", [128, 128]) for a in range(2)]
        tmc_p = p.rot_sb("tmc", 2, [128, 528])
        col_p = p.rot_sb("col", 4, [128, 16])
        names = ["gcb", "DT", "Dm", "X", "XT", "IX", "MT", "attnT", "ktok", "vtok", "kbe", "vb", "WT", "U", "vnew", "o2", "kg", "o", "o0", "zz"]
        sq_ = {nm: p.rot_sb("dn_" + nm, 4, [128, 128]) for nm in names}

        _nh, _nc, _it = 4, 999, 6
        for hp in range(2):
            for hi in range(2):
                hl = hp * 2 + hi
                qkv = qkvs[hi]
                for qi_ in range(3):
                    blk_i = qi_ * 4 + hl
                    dst = qkv[qi_]
                    p.dma("sp", raw[:], fm_s[blk_i * 128:(blk_i + 1) * 128, :], reads=[RFM], writes=[raw])
                    wc = lambda i, j: dncw[:, blk_i * 9 + i * 3 + j:blk_i * 9 + i * 3 + j + 1]
                    rl = raw[:, T_CTX:T_ALL].rearrange("q (r c) -> q r c", c=64)
                    dl = dst[:, T_CTX:T_ALL].rearrange("q (r c) -> q r c", c=64)
                    ts(p, dst[:, T_CTX:T_ALL], raw[:, T_CTX:T_ALL], wc(1, 1), None, ALU.mult, None, [raw, dncw], [dst])
                    for i in range(3):
                        for j in range(3):
                            if i == 1 and j == 1:
                                continue
                            di, dj = i - 1, j - 1
                            r0, r1 = max(0, -di), 64 - max(0, di)
                            c0, c1 = max(0, -dj), 64 - max(0, dj)
                            stt(p, dl[:, r0:r1, c0:c1], rl[:, r0 + di:r1 + di, c0 + dj:c1 + dj], wc(i, j), dl[:, r0:r1, c0:c1], ALU.mult, ALU.add, [raw, dncw, dst], [dst])
                    ts(p, dst[:, 0:T_CTX], raw[:, 0:T_CTX], wc(1, 1), None, ALU.mult, None, [raw, dncw], [dst])
                    stt(p, dst[:, 1:T_CTX], raw[:, 0:T_CTX - 1], wc(1, 0), dst[:, 1:T_CTX], ALU.mult, ALU.add, [raw, dncw, dst], [dst])
                    stt(p, dst[:, 0:T_CTX - 1], raw[:, 1:T_CTX], wc(1, 2), dst[:, 0:T_CTX - 1], ALU.mult, ALU.add, [raw, dncw, dst], [dst])
                    act(p, dst[:], dst[:], AF.Silu, [dst], [dst])
                    if qi_ < 2:
                        for c0 in range(0, T_ALL, 512):
                            cn = min(512, T_ALL - c0)
                            sb_, rb_ = sqb.next(), rsb.next()
                            act(p, sb_[:, :cn], dst[:, c0:c0 + cn], AF.Square, [dst], [sb_])
                            ps = pst.next()
                            mm(p, ps[:, :cn], ONE, sb_[:, :cn], True, True, [cst, sb_], [ps])
                            ts(p, rb_[:, :cn], ps[:, :cn], EPS, None, ALU.add, None, [ps], [rb_])
                            act(p, rb_[:, :cn], rb_[:, :cn], AF.Sqrt, [rb_], [rb_])
                            p.op("dve", lambda e, rb_=rb_, cn=cn: e.reciprocal(rb_[:, :cn], rb_[:, :cn]), reads=[rb_], writes=[rb_])
                            if qi_ == 0:
                                stt(p, dst[:, c0:c0 + cn], dst[:, c0:c0 + cn], float(128 ** -0.5), rb_[:, :cn], ALU.mult, ALU.mult, [dst, rb_], [dst])
                            else:
                                tt(p, dst[:, c0:c0 + cn], dst[:, c0:c0 + cn], rb_[:, :cn], ALU.mult, [dst, rb_], [dst])
            for d in range(2):
                TRI, MI, MS = (LE, LE, GT) if d == 0 else (GE, GE, LT)
                last = 127 if d == 0 else 0
                for S in Ss:
                    p.op("dve", lambda e, S=S: e.memset(S[:], 0.0), writes=[S])
                order = [0, 1] + list(range(2, NCH)) if d == 0 else [1, 0] + list(range(NCH - 1, 1, -1))
                for c in order[:_nc]:
                    t0 = c * GC
                    tmc = tmc_p.next()
                    p.dma("sp", tmc[:], tm_s[t0:t0 + GC, :], reads=[RTM], writes=[tmc])
                    for hi in range(2):
                        hl = hp * 2 + hi
                        pi = d * 4 + hl
                        qn, kn, vs = qkvs[hi]
                        S = Ss[hi]
                        cl = col_p.next()
                        beta, nbeta, g, gc, ngc, egc, sc, gl, kgc, egl = [cl[:, i:i + 1] for i in range(10)]
                        R_ = [cl]
                        act(p, beta, tmc[:, 512 + pi:513 + pi], AF.Exp, [tmc], R_, scale=-1.0)
                        ts(p, beta, beta, 1.0, None, ALU.add, None, R_, R_)
                        p.op("dve", lambda e, beta=beta: e.reciprocal(beta, beta), reads=R_, writes=R_)
                        ts(p, nbeta, beta, -1.0, None, ALU.mult, None, R_, R_)
                        act(p, g, tmc[:, 520 + pi:521 + pi], AF.Exp, [tmc, dnpar], R_, bias=dnpar[:, 8 + pi:9 + pi])
                        act(p, g, g, AF.Ln, R_, R_, bias=1.0)
                        ts(p, g, g, nexpa[:, pi:pi + 1], None, ALU.mult, None, R_ + [nexpa], R_)
                        ps = pst.next()
                        mm(p, ps[:, 0:1], TRI, g, True, True, [cst, cl], [ps])
                        act(p, gc, ps[:, 0:1], AF.Copy, [ps], R_)
                        ts(p, ngc, gc, -1.0, None, ALU.mult, None, R_, R_)
                        act(p, egc, gc, AF.Exp, R_, R_)
                        tt(p, sc, beta, egc, ALU.mult, R_, R_)
                        gcb = sq_["gcb"].next()
                        ts(p, gcb[:], ONE, gc, None, ALU.mult, None, [cst] + R_, [gcb])
                        bc = pst.next()
                        mm(p, bc[:, 0:128], gcb[:], IDN, True, True, [gcb, cst], [bc])
                        DT, Dm = sq_["DT"].next(), sq_["Dm"].next()
                        act(p, DT[:], bc[:, 0:128], AF.Exp, [bc] + R_, [DT], bias=ngc)
                        stt(p, DT[:], DT[:], 1.0, MI, ALU.min, ALU.mult, [DT, cst], [DT])
                        act(p, Dm[:], bc[:, 0:128], AF.Exp, [bc] + R_, [Dm], scale=-1.0, bias=gc)
                        stt(p, Dm[:], Dm[:], 1.0, MS, ALU.min, ALU.mult, [Dm, cst], [Dm])
                        act(p, gl, bc[:, last:last + 1], AF.Copy, [bc], R_)
                        act(p, kgc, gc, AF.Exp, R_, R_, scale=-1.0, bias=gl)
                        act(p, egl, gl, AF.Exp, R_, R_)
                        kT, qT, vT = kn[:, t0:t0 + GC], qn[:, t0:t0 + GC], vs[:, t0:t0 + GC]
                        kk = pst.next()
                        mm(p, kk[:, 0:128], kT, kT, True, True, [kn], [kk])
                        X, XT = sq_["X"].next(), sq_["XT"].next()
                        stt(p, X[:], kk[:, 0:128], nbeta, Dm[:], ALU.mult, ALU.mult, [kk, Dm] + R_, [X])
                        tp = pst.next()
                        mm(p, tp[:, 0:128], X[:], IDN, True, True, [X, cst], [tp])
                        act(p, XT[:], tp[:, 0:128], AF.Copy, [tp], [XT])
                        qk = pst.next()
                        mm(p, qk[:, 0:128], kT, qT, True, True, [kn, qn], [qk])
                        attnT = sq_["attnT"].next()
                        tt(p, attnT[:], qk[:, 0:128], DT[:], ALU.mult, [qk, DT], [attnT])
                        ktok, vtok = sq_["ktok"].next(), sq_["vtok"].next()
                        tp = pst.next()
                        mm(p, tp[:, 0:128], kT, IDN, True, True, [kn, cst], [tp])
                        act(p, ktok[:], tp[:, 0:128], AF.Copy, [tp], [ktok])
                        tp = pst.next()
                        mm(p, tp[:, 0:128], vT, IDN, True, True, [vs, cst], [tp])
                        act(p, vtok[:], tp[:, 0:128], AF.Copy, [tp], [vtok])
                        kbe, vb, kg = sq_["kbe"].next(), sq_["vb"].next(), sq_["kg"].next()
                        ts(p, kbe[:], ktok[:], sc, None, ALU.mult, None, [ktok] + R_, [kbe])
                        ts(p, vb[:], vtok[:], beta, None, ALU.mult, None, [vtok] + R_, [vb])
                        ts(p, kg[:], ktok[:], kgc, None, ALU.mult, None, [ktok] + R_, [kg])
                        MT = sq_["MT"].next()
                        tt(p, MT[:], XT[:], IDN, ALU.add, [XT, cst], [MT])
                        _stg = 4
                        for it in range(_it):
                            x2 = pst.next()
                            mm(p, x2[:, 0:128], XT[:], X[:], True, True, [XT, X], [x2])
                            Xn = sq_["X"].next()
                            XTn = sq_["XT"].next()
                            if it < 5 and _stg >= 2:
                                xt2 = pst.next()
                                mm(p, xt2[:, 0:128], X[:], XT[:], True, True, [X, XT], [xt2])
                                act(p, XTn[:], xt2[:, 0:128], AF.Copy, [xt2], [XTn])
                            p.op("dve", lambda e, Xn=Xn, x2=x2: e.tensor_copy(Xn[:], x2[:, 0:128]), reads=[x2], writes=[Xn])
                            IX = sq_["IX"].next()
                            tt(p, IX[:], Xn[:], IDN, ALU.add, [Xn, cst], [IX])
                            if it < 5:
                                X, XT = Xn, XTn
                            if _stg >= 4:
                                mp = pst.next()
                                mm(p, mp[:, 0:128], IX[:], MT[:], True, True, [IX, MT], [mp])
                                MT = sq_["MT"].next()
                                p.op("dve", lambda e, MT=MT, mp=mp: e.tensor_copy(MT[:], mp[:, 0:128]), reads=[mp], writes=[MT])
                        WT, U_ = sq_["WT"].next(), sq_["U"].next()
                        wp = pst.next()
                        mm(p, wp[:, 0:128], kbe[:], MT[:], True, True, [kbe, MT], [wp])
                        act(p, WT[:], wp[:, 0:128], AF.Copy, [wp], [WT])
                        up = pst.next()
                        mm(p, up[:, 0:128], MT[:], vb[:], True, True, [MT, vb], [up])
                        act(p, U_[:], up[:, 0:128], AF.Copy, [up], [U_])
                        wsp = pst.next()
                        mm(p, wsp[:, 0:128], WT[:], S[:], True, True, [WT, S], [wsp])
                        vnew = sq_["vnew"].next()
                        tt(p, vnew[:], U_[:], wsp[:, 0:128], ALU.subtract, [U_, wsp], [vnew])
                        o1 = pst.next()
                        mm(p, o1[:, 0:128], qT, S[:], True, True, [qn, S], [o1])
                        o2p = pst.next()
                        mm(p, o2p[:, 0:128], attnT[:], vnew[:], True, True, [attnT, vnew], [o2p])
                        o2 = sq_["o2"].next()
                        act(p, o2[:], o2p[:, 0:128], AF.Copy, [o2p], [o2])
                        o_ = sq_["o"].next()
                        stt(p, o_[:], o1[:, 0:128], egc, o2[:], ALU.mult, ALU.add, [o1, o2] + R_, [o_])
                        sp_ = pst.next()
                        mm(p, sp_[:, 0:128], kg[:], vnew[:], True, True, [kg, vnew], [sp_])
                        stt(p, S[:], S[:], egl, sp_[:, 0:128], ALU.mult, ALU.add, [S, sp_] + R_, [S])
                        if d == 0:
                            p.dma("sp", o0_s[t0:t0 + GC, hi * 128:(hi + 1) * 128], o_[:], reads=[o_], writes=[RO0])
                        else:
                            o0 = sq_["o0"].next()
                            p.dma("sp", o0[:], o0_s[t0:t0 + GC, hi * 128:(hi + 1) * 128], reads=[RO0], writes=[o0])
                            tt(p, o_[:], o_[:], o0[:], ALU.add, [o_, o0], [o_])
                            zz = sq_["zz"].next()
                            tt(p, zz[:], o_[:], o_[:], ALU.mult, [o_], [zz])
                            p.op("dve", lambda e, cl=cl, zz=zz: e.reduce_sum(out=cl[:, 10:11], in_=zz[:], axis=AX.X), reads=[zz], writes=R_)
                            ts(p, cl[:, 10:11], cl[:, 10:11], 1.0 / 128.0, EPS, ALU.mult, ALU.add, R_, R_)
                            act(p, cl[:, 10:11], cl[:, 10:11], AF.Ln, R_, R_)
                            act(p, cl[:, 10:11], cl[:, 10:11], AF.Exp, R_, R_, scale=-0.5)
                            act(p, zz[:], tmc[:, hl * 128:(hl + 1) * 128], AF.Exp, [tmc], [zz], scale=-1.0)
                            ts(p, zz[:], zz[:], 1.0, None, ALU.add, None, [zz], [zz])
                            p.op("dve", lambda e, zz=zz: e.reciprocal(zz[:], zz[:]), reads=[zz], writes=[zz])
                            tt(p, zz[:], zz[:], tmc[:, hl * 128:(hl + 1) * 128], ALU.mult, [zz, tmc], [zz])
                            stt(p, o_[:], o_[:], cl[:, 10:11], dnnw[:], ALU.mult, ALU.mult, [o_, dnnw] + R_, [o_])
                            tt(p, o_[:], o_[:], zz[:], ALU.mult, [o_, zz], [o_])
                            out_toks.append(p.dma("sp", out_dn[t0:t0 + GC, hl * 128:(hl + 1) * 128], o_[:], reads=[o_]))
        p.end_phase()
    p.wait_final("sp", out_toks)
    p.finish()
    return nc


def mix0_inputs(k, x, ctx, mods, inp):
    b, hh = k // 2, k % 2
    w_in = inp["e_w_in"][0]
    H = [4 * hh + i for i in range(4)]
    cols = []
    for base in (0, 1024, 2048):
        for h in H:
            cols += list(range(base + h * 128, base + (h + 1) * 128))
    cols += list(range(4096 + hh * 512, 4096 + (hh + 1) * 512))
    cols += list(range(5120 + hh * 512, 5120 + (hh + 1) * 512))
    wfm = blk(np.ascontiguousarray(w_in[:, cols]))
    tcols = []
    for h in H:
        tcols += list(range(3072 + h * 128, 3072 + (h + 1) * 128))
    tcols += [6144 + h for h in H] + [6152 + h for h in H] + [6160 + h for h in H] + [6168 + h for h in H]
    wtm = np.ascontiguousarray(w_in[:, tcols].reshape(KC, 128, 528).transpose(1, 0, 2)).reshape(128, KC * 528)
    g0 = 32 * hh
    lre, lim, lst = inp["s5_lam_re"][0], inp["s5_lam_im"][0], inp["s5_log_step"][0]
    par = np.zeros((128, 3, 2, 16), np.float32)
    for d in range(2):
        for r in range(16):
            for gip in range(2):
                g = g0 + 2 * r + gip
                par[gip * 64:(gip + 1) * 64, 0, d, r] = lre[d, g]
                par[gip * 64:(gip + 1) * 64, 1, d, r] = lim[d, g]
                par[gip * 64:(gip + 1) * 64, 2, d, r] = lst[d, g]
    bre, bim, cre, cim = inp["s5_b_re"][0], inp["s5_b_im"][0], inp["s5_c_re"][0], inp["s5_c_im"][0]
    bw = np.zeros((4, 128, 2, 4, 2, 128), np.float32)
    cw = np.zeros((4, 128, 2, 4, 2, 128), np.float32)
    for bl in range(4):
        for gl in range(8):
            g = g0 + 8 * bl + gl
            r4, gip = gl // 2, gl % 2
            for d in range(2):
                bw[bl, gl * 16:(gl + 1) * 16, d, r4, 0, gip * 64:(gip + 1) * 64] = bre[d, g].T
                bw[bl, gl * 16:(gl + 1) * 16, d, r4, 1, gip * 64:(gip + 1) * 64] = bim[d, g].T
                cw[bl, gip * 64:(gip + 1) * 64, d, r4, 0, gl * 16:(gl + 1) * 16] = cre[d, g].T
                cw[bl, gip * 64:(gip + 1) * 64, d, r4, 1, gl * 16:(gl + 1) * 16] = cim[d, g].T
    s5d = np.ascontiguousarray(inp["s5_d"][0][hh * 512:(hh + 1) * 512].reshape(4, 128).T)
    mv = np.concatenate([colT(mods[4, 0, 0]), colT(mods[4, 0, 1]), colT(mods[b, 0, 0]), colT(mods[b, 0, 1])], axis=1)
    xT = np.ascontiguousarray(np.concatenate([ctx[b], x[b]], axis=0).T)
    dnpar = np.zeros((128, 16), np.float32)
    for d in range(2):
        for hl, h in enumerate(H):
            dnpar[:, d * 4 + hl] = inp["dn_a_log"][0][d, h]
            dnpar[:, 8 + d * 4 + hl] = inp["dn_dt_bias"][0][d, h]
    cwv = inp["dn_conv"][0]
    dncw = np.zeros((128, 12, 9), np.float32)
    for qi_, base in enumerate((0, 1024, 2048)):
        for hl, h in enumerate(H):
            dncw[:, qi_ * 4 + hl, :] = cwv[:, :, base + h * 128:base + (h + 1) * 128].reshape(9, 128).T
    dnnw = np.ascontiguousarray(np.broadcast_to(inp["dn_norm_w"][0], (128, 128)))
    pp = np.arange(128)[:, None]
    ff = np.arange(128)[None, :]
    dncst = np.ascontiguousarray(np.concatenate([(pp <= ff), (pp >= ff), (pp > ff), (pp < ff), (pp == ff), np.ones((128, 128), bool)], axis=1).astype(np.float32))
    return {"dnpar": dnpar, "dncw": dncw.reshape(128, 108), "dnnw": dnnw, "dncst": dncst,
            "xT": xT, "modv": np.ascontiguousarray(mv), "wfm": wfm, "wtm": wtm,
            "s5par": np.ascontiguousarray(par.reshape(128, 96)), "s5bw": bw.reshape(4, 128, 2048), "s5cw": cw.reshape(4, 128, 2048),
            "s5d": s5d, "iota": np.ascontiguousarray(np.broadcast_to(np.arange(T_LAT, dtype=np.float32), (128, T_LAT)))}


def run_mix0(x, ctx, mods, inp):
    nc = build_mix0(True, True)
    in_maps = [mix0_inputs(k, x, ctx, mods, inp) for k in range(NCORES)]
    res = run_bass_kernel_spmd(nc, in_maps, core_ids=list(range(NCORES)))
    mix_x = np.empty((4, T_LAT, D), np.float32)
    mix_c = np.empty((4, T_CTX, D), np.float32)
    for k in range(NCORES):
        b, hh = k // 2, k % 2
        s5 = res.results[k]["out_s5"]
        dn = res.results[k]["out_dn"]
        mix_c[b, :, hh * 512:(hh + 1) * 512] = s5[:, :T_CTX].T
        mix_x[b, :, hh * 512:(hh + 1) * 512] = s5[:, T_CTX:].T
        mix_c[b, :, 1024 + hh * 512:1024 + (hh + 1) * 512] = dn[:T_CTX]
        mix_x[b, :, 1024 + hh * 512:1024 + (hh + 1) * 512] = dn[T_CTX:]
    return mix_x, mix_c


def kernel(**inputs):
    inp = {k: np.asarray(v) for k, v in inputs.items()}
    x = np.ascontiguousarray(inp["x"], dtype=np.float32)
    ctx = np.ascontiguousarray(inp["ctx"], dtype=np.float32)
    mods = run_mods(inp)
    mix_x, mix_c = run_mix0(x, ctx, mods, inp)
    x1, c1 = run_ffn(0, mix_x, mix_c, x, ctx, mods, inp)
    mix1 = run_gla(x1, c1, mods, inp)
    x2, _ = run_ffn(1, mix1, None, x1, None, mods, inp)
    return x2.astype(np.float32)
```

```python
from contextlib import ExitStack
import numpy as np
import concourse.bass as bass
import concourse.mybir as mybir
from concourse.bass_utils import run_bass_kernel_spmd

F32 = mybir.dt.float32
F32R = mybir.dt.float32r
BF16 = mybir.dt.bfloat16
ALU = mybir.AluOpType
AF = mybir.ActivationFunctionType
AX = mybir.AxisListType

ENGS = ("pe", "dve", "act", "pool", "sp")
SEM_LIMIT = 20000
NDMA_SEM = 6
D = 2048
KC = 16
ALPHA = float(4 ** 0.25)
EPS = 1e-6
NCORES = 8


class Res:
    __slots__ = ("name", "w", "r")

    def __init__(self, name=""):
        self.name = name
        self.w = None
        self.r = []


class T:
    def __init__(self, t, name):
        self.t = t
        self.res = Res(name)
        self.name = name

    def __getitem__(self, idx):
        return self.t[idx]


class Rot:
    def __init__(self, tiles):
        self.tiles = tiles
        self.i = 0

    def next(self):
        t = self.tiles[self.i]
        self.i = (self.i + 1) % len(self.tiles)
        return t


def _res(x):
    return x.res if isinstance(x, T) else x


class Prog:
    def __init__(self, nc):
        self.nc = nc
        self.es = ExitStack()
        self.q = {e: [] for e in ENGS}
        self.sems = {}
        self.cur = {}
        self.nsem = 0
        for e in ENGS:
            self._new_epoch(e)
        self.seen = {e: {} for e in ENGS}
        self.dma_sems = {}
        self.dma_cnt = {}
        self.dma_rr = {}
        self.n_inst = 0
        self.final_waits = []
        self.phase = None
        self.pending = {e: [] for e in ENGS}

    def _alloc_sem(self, name):
        h = self.es.enter_context(self.nc.semaphore(name))
        self.sems[name] = h
        self.nsem += 1
        return name

    def _new_epoch(self, e):
        k = self._alloc_sem(f"s_{e}_{self.nsem}")
        self.cur[e] = [k, 0]

    def sb(self, name, shape, dt=F32):
        st = self.phase if self.phase is not None else self.es
        t = st.enter_context(self.nc.sbuf_tensor(name, list(shape), dt))
        return T(t, name)

    def begin_phase(self):
        self.phase = ExitStack()

    def end_phase(self):
        self.barrier()
        self.phase.close()
        self.phase = None

    def barrier(self):
        toks = []
        for e in ENGS:
            k, c = self.cur[e]
            if c > 0:
                toks.append((k, c))
        for e, sems in self.dma_sems.items():
            for i, k in enumerate(sems):
                if self.dma_cnt[e][i] > 0:
                    toks.append((k, self.dma_cnt[e][i]))
        for e in ENGS:
            for (k, v) in toks:
                if k == self.cur[e][0]:
                    continue
                if self.seen[e].get(k, 0) < v:
                    self.seen[e][k] = v
                    self.pending[e].append((k, v))

    def ps(self, name, shape, dt=F32):
        t = self.es.enter_context(self.nc.psum_tensor(name, list(shape), dt))
        return T(t, name)

    def rot_sb(self, name, n, shape, dt=F32):
        return Rot([self.sb(f"{name}{i}", shape, dt) for i in range(n)])

    def rot_ps(self, name, n, shape, dt=F32):
        return Rot([self.ps(f"{name}{i}", shape, dt) for i in range(n)])

    def _deps(self, eng, reads, writes):
        need = {}

        def add(tok):
            if tok is None:
                return
            k, v = tok
            if need.get(k, 0) < v:
                need[k] = v
        for r in reads:
            add(_res(r).w)
        for w in writes:
            w = _res(w)
            add(w.w)
            for t in w.r:
                add(t)
        waits = []
        seen = self.seen[eng]
        own = self.cur[eng][0]
        for k, v in need.items():
            if eng == "pe" and k == own:
                continue
            if seen.get(k, 0) < v:
                seen[k] = v
                waits.append((k, v))
        return waits

    def _commit(self, tok, reads, writes):
        for r in reads:
            r = _res(r)
            r.r.append(tok)
            if len(r.r) > 48:
                d = {}
                for k, v in r.r:
                    if d.get(k, 0) < v:
                        d[k] = v
                r.r = list(d.items())
        for w in writes:
            w = _res(w)
            w.w = tok
            w.r = []

    def op(self, eng, fn, reads=(), writes=(), inc=True):
        waits = self._deps(eng, reads, writes)
        if self.pending[eng]:
            waits = self.pending[eng] + waits
            self.pending[eng] = []
        cur = self.cur[eng]
        tok = (cur[0], cur[1] + 1)
        if inc:
            cur[1] += 1
        self.q[eng].append((waits, fn, (cur[0], 1) if inc else None))
        self._commit(tok, reads, writes)
        self.n_inst += 1
        if inc and cur[1] >= SEM_LIMIT:
            self._new_epoch(eng)
        return tok

    def dma(self, eng, out_ap, in_ap, reads=(), writes=(), **kw):
        waits = self._deps(eng, reads, writes)
        if self.pending[eng]:
            waits = self.pending[eng] + waits
            self.pending[eng] = []
        if eng not in self.dma_sems:
            self.dma_sems[eng] = [self._alloc_sem(f"d_{eng}_{i}") for i in range(NDMA_SEM)]
            self.dma_cnt[eng] = [0] * NDMA_SEM
            self.dma_rr[eng] = 0
        i = self.dma_rr[eng]
        self.dma_rr[eng] = (i + 1) % NDMA_SEM
        self.dma_cnt[eng][i] += 16
        k = self.dma_sems[eng][i]
        tok = (k, self.dma_cnt[eng][i])

        def fn(e, out_ap=out_ap, in_ap=in_ap, kw=kw):
            return e.dma_start(out=out_ap, in_=in_ap, **kw)
        self.q[eng].append((waits, fn, (k, 16)))
        self._commit(tok, reads, writes)
        self.n_inst += 1
        return tok

    def wait_final(self, eng, toks):
        d = {}
        for k, v in toks:
            if d.get(k, 0) < v:
                d[k] = v
        self.final_waits.append((eng, list(d.items())))

    def check(self):
        cnt = {}
        ptr = {e: 0 for e in ENGS}
        prog = True
        while prog:
            prog = False
            for e in ENGS:
                q = self.q[e]
                while ptr[e] < len(q):
                    waits, fn, inc = q[ptr[e]]
                    if all(cnt.get(k, 0) >= v for k, v in waits):
                        if inc is not None:
                            cnt[inc[0]] = cnt.get(inc[0], 0) + inc[1]
                        ptr[e] += 1
                        prog = True
                    else:
                        break
        stuck = {e: (ptr[e], len(self.q[e])) for e in ENGS if ptr[e] < len(self.q[e])}
        if stuck:
            for e in stuck:
                waits, fn, inc = self.q[e][ptr[e]]
                print("STUCK", e, stuck[e], [(k, v, cnt.get(k, 0)) for k, v in waits])
        return not stuck

    def finish(self):
        nc = self.nc
        engmap = {"pe": "tensor", "dve": "vector", "act": "scalar", "pool": "gpsimd", "sp": "sync"}
        fin = {e: [] for e in ENGS}
        for e, toks in self.final_waits:
            fin[e].extend(toks)
        with nc.Block() as block:
            for e in ENGS:
                items = self.q[e]
                fw = fin[e]
                if not items and not fw:
                    continue

                def body(engine, items=items, fw=fw):
                    for waits, fn, inc in items:
                        for k, v in waits:
                            engine.wait_ge(self.sems[k], v)
                        ins = fn(engine)
                        if inc is not None:
                            ins.then_inc(self.sems[inc[0]], inc[1])
                    for k, v in fw:
                        engine.wait_ge(self.sems[k], v)
                getattr(block, engmap[e])(body)
        self.es.close()


def mm(p, out, lhsT, rhs, start, stop, reads, writes):
    p.op("pe", lambda e: e.matmul(out, lhsT, rhs, start=start, stop=stop), reads=reads, writes=writes, inc=True)


def act(p, out, in_, func, reads, writes, **kw):
    p.op("act", lambda e: e.activation(out=out, in_=in_, func=func, **kw), reads=reads, writes=writes)


def tt(p, out, in0, in1, op, reads, writes, eng="dve"):
    p.op(eng, lambda e: e.tensor_tensor(out=out, in0=in0, in1=in1, op=op), reads=reads, writes=writes)


def ts(p, out, in0, s1, s2, op0, op1, reads, writes, eng="dve"):
    if op1 is None:
        p.op(eng, lambda e: e.tensor_scalar(out, in0, s1, None, op0=op0), reads=reads, writes=writes)
    else:
        p.op(eng, lambda e: e.tensor_scalar(out, in0, s1, s2, op0=op0, op1=op1), reads=reads, writes=writes)


def stt(p, out, in0, scalar, in1, op0, op1, reads, writes):
    p.op("dve", lambda e: e.scalar_tensor_tensor(out=out, in0=in0, scalar=scalar, in1=in1, op0=op0, op1=op1),
         reads=reads, writes=writes)


def dram_in(nc, name, shape, dt=F32):
    return nc.dram_tensor(name, list(shape), dt, kind="ExternalInput").ap()


def dram_out(nc, name, shape, dt=F32):
    return nc.dram_tensor(name, list(shape), dt, kind="ExternalOutput").ap()


def blk(W):
    Din, Dout = W.shape
    kc, m = Din // 128, Dout // 128
    return np.ascontiguousarray(W.reshape(kc, 128, m, 128).transpose(2, 1, 0, 3)).reshape(m, 128, kc * 128)


def colT(v):
    return np.ascontiguousarray(v.reshape(-1, 128).T)


NB_A = 2 * 6 * D // 128 // NCORES


def build_mods():
    nc = bass.Bass("TRN2", target_bir_lowering=False)
    condT = dram_in(nc, "condT", [128, KC * 6])
    wb = dram_in(nc, "wb", [NB_A, 128, D], F32R)
    bias = dram_in(nc, "bias", [128, NB_A])
    out = dram_out(nc, "out", [128, NB_A * 6])
    p = Prog(nc)
    c_sb = p.sb("c_sb", [128, KC * 6])
    c_r = p.sb("c_r", [128, KC * 6], F32R)
    b_sb = p.sb("b_sb", [128, NB_A])
    o_sb = p.sb("o_sb", [128, NB_A * 6])
    wt = p.rot_sb("wt", 3, [128, D], F32R)
    pst = p.rot_ps("ps", 4, [128, 512])
    p.dma("sp", c_sb[:], condT, writes=[c_sb])
    p.dma("sp", b_sb[:], bias, writes=[b_sb])
    act(p, c_r[:], c_sb[:], AF.Silu, [c_sb], [c_r])
    for m in range(NB_A):
        w = wt.next()
        p.dma("pool", w[:], wb[m], writes=[w])
        ps = pst.next()
        for kc in range(KC):
            mm(p, ps[:, 0:6], w[:, kc * 128:(kc + 1) * 128], c_r[:, kc * 6:(kc + 1) * 6], kc == 0, kc == KC - 1, [w, c_r], [ps])
        act(p, o_sb[:, m * 6:(m + 1) * 6], ps[:, 0:6], AF.Identity, [ps, b_sb], [o_sb], bias=b_sb[:, m:m + 1])
    t = p.dma("sp", out, o_sb[:], reads=[o_sb])
    p.wait_final("sp", [t])
    p.finish()
    return nc


def run_mods(inp):
    c, c_ctx = inp["c"], inp["c_ctx"]
    cond = np.zeros((6, D), np.float32)
    cond[:4] = c
    cond[4] = c_ctx
    condT = np.ascontiguousarray(cond.T.reshape(KC, 128, 6).transpose(1, 0, 2)).reshape(128, KC * 6)
    W = np.concatenate([inp["ada_w"][0], inp["ada_w"][1]], axis=1)
    B = np.concatenate([inp["ada_b"][0], inp["ada_b"][1]], axis=0)
    Wb = blk(W)
    Bc = colT(B)
    nc = build_mods()
    in_maps = []
    for k in range(NCORES):
        in_maps.append({"condT": condT, "wb": np.ascontiguousarray(Wb[k * NB_A:(k + 1) * NB_A]),
                        "bias": np.ascontiguousarray(Bc[:, k * NB_A:(k + 1) * NB_A])})
    res = run_bass_kernel_spmd(nc, in_maps, core_ids=list(range(NCORES)))
    o = np.stack([r["out"].reshape(128, NB_A, 6) for r in res.results], axis=0)
    full = o.transpose(3, 0, 2, 1).reshape(6, 2 * 6 * D)
    mods = full.reshape(6, 2, 6, D)
    return mods


def modcols(mods, layer, row):
    return np.ascontiguousarray(np.concatenate([colT(mods[row, layer, i]) for i in range(6)], axis=1))


NP_MAX = 512


def build_ffn(segs, E, HC, moe, mode="full"):
    nc = bass.Bass("TRN2", target_bir_lowering=False)
    NT = sum(segs)
    nseg = len(segs)
    do_pre = mode in ("full", "pre")
    do_ffn = mode in ("full", "exp")
    do_post = mode in ("full", "post")
    if do_pre:
        mixT = dram_in(nc, "mixT", [D, NT], F32R)
        xT = dram_in(nc, "xT", [D, NT])
        woutb = dram_in(nc, "woutb", [KC, 128, D], F32R)
    if mode != "exp":
        modv = dram_in(nc, "modv", [128, nseg * 96])
        lnp = dram_in(nc, "lnp", [128, 64])
    if do_ffn:
        w1b = dram_in(nc, "w1b", [E * HC, 128, D], F32R)
        w3b = dram_in(nc, "w3b", [E * HC, 128, D], F32R)
        w2 = dram_in(nc, "w2", [E * HC * 128, D], F32R)
    if mode == "pre":
        router = dram_in(nc, "router", [128, KC * 8])
        ident_d = dram_in(nc, "ident", [128, 128])
        xl_out = dram_out(nc, "xl_out", [D, NT])
        h_out = dram_out(nc, "h_out", [D, NT])
        g_out = dram_out(nc, "g_out", [8, NT])
    if mode == "exp":
        hT_in = dram_in(nc, "hT_in", [D, NT], F32R)
        grow = dram_in(nc, "grow", [128, NT])
        y_out = dram_out(nc, "y_out", [D, NT])
    if mode == "post":
        parts = dram_in(nc, "parts", [8 * D, NT])
        xl_in = dram_in(nc, "xl_in", [D, NT])
    if do_post:
        xoT = dram_out(nc, "xoT", [D, NT])
    p = Prog(nc)

    A = p.sb("A", [128, KC, NP_MAX]) if mode != "pre" else None
    C = p.sb("C", [128, KC, NP_MAX], F32R) if mode != "exp" else None
    Dl = p.sb("Dl", [128, KC, NP_MAX]) if mode != "exp" else None
    H = p.sb("H", [128, KC, NP_MAX], F32R)
    ones = p.sb("ones", [128, 128], F32R)
    wts = p.rot_sb("wts", {"full": 6, "exp": 6, "pre": 3, "post": 1}[mode], [128, D], F32R)
    sq = p.rot_sb("sq", 2, [128, NP_MAX], F32R)
    tmp = p.rot_sb("tmp", 3, [128, NP_MAX])
    a2 = p.rot_sb("a2", 2, [128, NP_MAX], F32R)
    xin = p.rot_sb("xin", 2, [128, NP_MAX])
    xout = p.rot_sb("xout", 2, [128, NP_MAX])
    st_m = p.sb("st_m", [128, NP_MAX])
    st_r = p.sb("st_r", [128, NP_MAX])
    st_n = p.sb("st_n", [128, NP_MAX])
    pst = p.rot_ps("ps", 8, [128, 512])
    ones_f = p.sb("ones_f", [128, 128])
    p.op("dve", lambda e: e.memset(ones_f[:], 1.0 / D), writes=[ones_f])
    act(p, ones[:], ones_f[:], AF.Copy, [ones_f], [ones])
    if mode != "exp":
        mod_sb = p.sb("mod_sb", [128, nseg * 96])
        ln_sb = p.sb("ln_sb", [128, 64])
        p.dma("sp", mod_sb[:], modv, writes=[mod_sb])
        p.dma("sp", ln_sb[:], lnp, writes=[ln_sb])
        for s in range(nseg):
            for mi in (1, 4):
                o = s * 96 + mi * 16
                ts(p, mod_sb[:, o:o + 16], mod_sb[:, o:o + 16], 1.0, None, ALU.add, None, [mod_sb], [mod_sb])
    if mode == "pre":
        r_sb = p.sb("r_sb", [128, KC * 8])
        ident = p.sb("ident_sb", [128, 128])
        p.dma("sp", r_sb[:], router, writes=[r_sb])
        p.dma("sp", ident[:], ident_d, writes=[ident])
        gT = p.sb("gT", [8, NP_MAX])
        sm = p.rot_sb("sm", 2, [128, 40])
    if mode == "exp":
        G = p.rot_sb("G", 2, [128, NP_MAX])

    def mcol(s, mi, f):
        o = s * 96 + mi * 16 + f
        return mod_sb[:, o:o + 1]

    def lcol(li, f):
        return ln_sb[:, li * 16 + f:li * 16 + f + 1]

    def ln_stats(src, n):
        mps = pst.next()
        eps_ = pst.next()
        for kc in range(KC):
            s_ = sq.next()
            act(p, s_[:, :n], src[:, kc, :n], AF.Square, [src], [s_])
            mm(p, mps[:, :n], ones[:], src[:, kc, :n], kc == 0, kc == KC - 1, [ones, src], [mps])
            mm(p, eps_[:, :n], ones[:], s_[:, :n], kc == 0, kc == KC - 1, [ones, s_], [eps_])
        act(p, st_m[:, :n], mps[:, :n], AF.Copy, [mps], [st_m])
        tt(p, st_n[:, :n], st_m[:, :n], st_m[:, :n], ALU.mult, [st_m], [st_n])
        tt(p, st_r[:, :n], eps_[:, :n], st_n[:, :n], ALU.subtract, [eps_, st_n], [st_r])
        ts(p, st_r[:, :n], st_r[:, :n], EPS, None, ALU.add, None, [st_r], [st_r])
        act(p, st_r[:, :n], st_r[:, :n], AF.Sqrt, [st_r], [st_r])
        p.op("dve", lambda e: e.reciprocal(st_r[:, :n], st_r[:, :n]), reads=[st_r], writes=[st_r])
        stt(p, st_n[:, :n], st_m[:, :n], -1.0, st_r[:, :n], ALU.mult, ALU.mult, [st_m, st_r], [st_n])

    C_f = C.t[:].bitcast(F32) if C is not None else None
    H_f = H.t[:].bitcast(F32)
    out_toks = []
    t_base = 0
    for s, ns in enumerate(segs):
        for t0 in range(0, ns, NP_MAX):
            n = min(NP_MAX, ns - t0)
            g0 = t_base + t0
            if do_pre:
                p.dma("pool", H[:, :, :n], mixT[:, g0:g0 + n].rearrange("(kc q) n -> q kc n", q=128), writes=[H])
                for f in range(KC):
                    w = wts.next()
                    p.dma("pool", w[:], woutb[f], writes=[w])
                    xi = xin.next()
                    p.dma("sp", xi[:, :n], xT[f * 128:(f + 1) * 128, g0:g0 + n], writes=[xi])
                    ps = pst.next()
                    for kc in range(KC):
                        mm(p, ps[:, :n], w[:, kc * 128:(kc + 1) * 128], H[:, kc, :n], kc == 0, kc == KC - 1, [w, H], [ps])
                    act(p, xi[:, :n], xi[:, :n], AF.Copy, [xi], [xi], scale=ALPHA)
                    stt(p, C[:, f, :n], ps[:, :n], mcol(s, 2, f), xi[:, :n], ALU.mult, ALU.add, [ps, xi, mod_sb], [C])
                ln_stats(C, n)
                for f in range(KC):
                    tm = tmp.next()
                    tt(p, tm[:, :n], C_f[:, f, :n], st_r[:, :n], ALU.mult, [C, st_r], [tm])
                    tt(p, tm[:, :n], tm[:, :n], st_n[:, :n], ALU.add, [tm, st_n], [tm])
                    ts(p, Dl[:, f, :n], tm[:, :n], lcol(0, f), lcol(1, f), ALU.mult, ALU.add, [tm, ln_sb], [Dl])
                    ts(p, H[:, f, :n], Dl[:, f, :n], mcol(s, 4, f), mcol(s, 3, f), ALU.mult, ALU.add, [Dl, mod_sb], [H])
            if mode == "pre":
                for st in range(n // 128):
                    lps = pst.next()
                    for kc in range(KC):
                        mm(p, lps[:, 0:8], H_f[:, kc, st * 128:(st + 1) * 128], r_sb[:, kc * 8:(kc + 1) * 8], kc == 0, kc == KC - 1, [H, r_sb], [lps])
                    m_ = sm.next()
                    lg, mx, ex, nm, sv = m_[:, 0:8], m_[:, 8:16], m_[:, 16:24], m_[:, 24:25], m_[:, 25:26]
                    mk = m_[:, 32:40]
                    act(p, lg, lps[:, 0:8], AF.Copy, [lps], [m_])
                    p.op("dve", lambda e, mx=mx, lg=lg: e.max(out=mx, in_=lg), reads=[m_], writes=[m_])
                    ts(p, nm, mx[:, 0:1], -1.0, None, ALU.mult, None, [m_], [m_])
                    act(p, ex, lg, AF.Exp, [m_], [m_], bias=nm)
                    ts(p, mk, lg, mx[:, 1:2], None, ALU.is_ge, None, [m_], [m_])
                    tt(p, ex, ex, mk, ALU.mult, [m_], [m_])
                    p.op("dve", lambda e, sv=sv, ex=ex: e.reduce_sum(out=sv, in_=ex, axis=AX.X), reads=[m_], writes=[m_])
                    p.op("dve", lambda e, sv=sv: e.reciprocal(sv, sv), reads=[m_], writes=[m_])
                    ts(p, ex, ex, sv, None, ALU.mult, None, [m_], [m_])
                    tps = pst.next()
                    mm(p, tps[0:8, 0:128], ex, ident[:], True, True, [m_, ident], [tps])
                    act(p, gT[:, st * 128:(st + 1) * 128], tps[0:8, 0:128], AF.Copy, [tps], [gT])
                out_toks.append(p.dma("sp", g_out[:, g0:g0 + n], gT[:, :n], reads=[gT]))
                out_toks.append(p.dma("sp", xl_out[:, g0:g0 + n].rearrange("(kc q) n -> q kc n", q=128), Dl[:, :, :n], reads=[Dl]))
                out_toks.append(p.dma("sp", h_out[:, g0:g0 + n].rearrange("(kc q) n -> q kc n", q=128), H_f[:, :, :n], reads=[H]))
            if mode == "exp":
                p.dma("pool", H[:, :, :n], hT_in[:, g0:g0 + n].rearrange("(kc q) n -> q kc n", q=128), writes=[H])
                Ge = G.next()
                p.dma("sp", Ge[:, :n], grow[:, g0:g0 + n], writes=[Ge])
            if do_ffn:
                first = True
                for e_ in range(E):
                    for j in range(HC):
                        w1t, w3t, w2t = wts.next(), wts.next(), wts.next()
                        p.dma("pool", w1t[:], w1b[e_ * HC + j], writes=[w1t])
                        p.dma("pool", w3t[:], w3b[e_ * HC + j], writes=[w3t])
                        p.dma("pool", w2t[:], w2[(e_ * HC + j) * 128:(e_ * HC + j + 1) * 128, :], writes=[w2t])
                        h1, h3 = pst.next(), pst.next()
                        for kc in range(KC):
                            mm(p, h1[:, :n], w1t[:, kc * 128:(kc + 1) * 128], H[:, kc, :n], kc == 0, kc == KC - 1, [w1t, H], [h1])
                        for kc in range(KC):
                            mm(p, h3[:, :n], w3t[:, kc * 128:(kc + 1) * 128], H[:, kc, :n], kc == 0, kc == KC - 1, [w3t, H], [h3])
                        s1 = tmp.next()
                        act(p, s1[:, :n], h1[:, :n], AF.Silu, [h1], [s1])
                        a_ = a2.next()
                        if mode == "exp":
                            tt(p, s1[:, :n], s1[:, :n], h3[:, :n], ALU.mult, [s1, h3], [s1])
                            tt(p, a_[:, :n], s1[:, :n], Ge[:, :n], ALU.mult, [s1, Ge], [a_])
                        else:
                            tt(p, a_[:, :n], s1[:, :n], h3[:, :n], ALU.mult, [s1, h3], [a_])
                        for f in range(KC):
                            yp = pst.next()
                            mm(p, yp[:, :n], w2t[:, f * 128:(f + 1) * 128], a_[:, :n], True, True, [w2t, a_], [yp])
                            if first:
                                p.op("dve", lambda e, f=f, yp=yp, n=n: e.tensor_copy(A[:, f, :n], yp[:, :n]), reads=[yp], writes=[A])
                            else:
                                tt(p, A[:, f, :n], A[:, f, :n], yp[:, :n], ALU.add, [A, yp], [A])
                        first = False
            if mode == "exp":
                out_toks.append(p.dma("sp", y_out[:, g0:g0 + n].rearrange("(kc q) n -> q kc n", q=128), A[:, :, :n], reads=[A]))
            if mode == "post":
                p.dma("sp", Dl[:, :, :n], xl_in[:, g0:g0 + n].rearrange("(kc q) n -> q kc n", q=128), writes=[Dl])
                for e_ in range(8):
                    if e_ == 0:
                        p.dma("sp", A[:, :, :n], parts[0:D, g0:g0 + n].rearrange("(kc q) n -> q kc n", q=128), writes=[A])
                    else:
                        for hf in range(2):
                            pt = xin.next()
                        for kc in range(KC):
                            pt = xin.next()
                            p.dma("sp", pt[:, :n], parts[e_ * D + kc * 128:e_ * D + (kc + 1) * 128, g0:g0 + n], writes=[pt])
                            tt(p, A[:, kc, :n], A[:, kc, :n], pt[:, :n], ALU.add, [A, pt], [A])
            if do_post:
                for f in range(KC):
                    tm = tmp.next()
                    act(p, tm[:, :n], Dl[:, f, :n], AF.Copy, [Dl], [tm], scale=ALPHA)
                    stt(p, C[:, f, :n], A[:, f, :n], mcol(s, 5, f), tm[:, :n], ALU.mult, ALU.add, [A, tm, mod_sb], [C])
                ln_stats(C, n)
                for f in range(KC):
                    tm = tmp.next()
                    tt(p, tm[:, :n], C_f[:, f, :n], st_r[:, :n], ALU.mult, [C, st_r], [tm])
                    tt(p, tm[:, :n], tm[:, :n], st_n[:, :n], ALU.add, [tm, st_n], [tm])
                    xo = xout.next()
                    ts(p, xo[:, :n], tm[:, :n], lcol(2, f), lcol(3, f), ALU.mult, ALU.add, [tm, ln_sb], [xo])
                    out_toks.append(p.dma("sp", xoT[f * 128:(f + 1) * 128, g0:g0 + n], xo[:, :n], reads=[xo]))
        t_base += ns
    p.wait_final("sp", out_toks)
    p.finish()
    return nc


NPX = 1024


def build_exp(NT, HC):
    nc = bass.Bass("TRN2", target_bir_lowering=False)
    hT_in = dram_in(nc, "hT_in", [D, NT])
    grow = dram_in(nc, "grow", [128, NT])
    w1b = dram_in(nc, "w1b", [HC, 128, D])
    w3b = dram_in(nc, "w3b", [HC, 128, D])
    w2 = dram_in(nc, "w2", [HC * 128, D])
    y_out = dram_out(nc, "y_out", [D, NT])
    p = Prog(nc)
    H = p.sb("H", [128, KC, NPX], BF16)
    A = p.sb("A", [128, KC, NPX])
    wts = p.rot_sb("wts", 6, [128, D], BF16)
    tmp = p.rot_sb("tmp", 3, [128, 512])
    a2 = p.rot_sb("a2", 3, [128, 512], BF16)
    G = p.rot_sb("G", 2, [128, NPX])
    pst = p.rot_ps("ps", 8, [128, 512])
    out_toks = []
    for t0 in range(0, NT, NPX):
        n = min(NPX, NT - t0)
        p.dma("pool", H[:, :, :n], hT_in[:, t0:t0 + n].rearrange("(kc q) n -> q kc n", q=128), writes=[H])
        Ge = G.next()
        p.dma("sp", Ge[:, :n], grow[:, t0:t0 + n], writes=[Ge])
        for j in range(HC):
            w1t, w3t, w2t = wts.next(), wts.next(), wts.next()
            p.dma("pool", w1t[:], w1b[j], writes=[w1t])
            p.dma("pool", w3t[:], w3b[j], writes=[w3t])
            p.dma("pool", w2t[:], w2[j * 128:(j + 1) * 128, :], writes=[w2t])
            for c0 in range(0, n, 512):
                cn = min(512, n - c0)
                h1, h3 = pst.next(), pst.next()
                for kc in range(KC):
                    mm(p, h1[:, :cn], w1t[:, kc * 128:(kc + 1) * 128], H[:, kc, c0:c0 + cn], kc == 0, kc == KC - 1, [w1t, H], [h1])
                for kc in range(KC):
                    mm(p, h3[:, :cn], w3t[:, kc * 128:(kc + 1) * 128], H[:, kc, c0:c0 + cn], kc == 0, kc == KC - 1, [w3t, H], [h3])
                s1 = tmp.next()
                act(p, s1[:, :cn], h1[:, :cn], AF.Silu, [h1], [s1])
                tt(p, s1[:, :cn], s1[:, :cn], h3[:, :cn], ALU.mult, [s1, h3], [s1])
                a_ = a2.next()
                tt(p, a_[:, :cn], s1[:, :cn], Ge[:, c0:c0 + cn], ALU.mult, [s1, Ge], [a_])
                for f in range(KC):
                    yp = pst.next()
                    mm(p, yp[:, :cn], w2t[:, f * 128:(f + 1) * 128], a_[:, :cn], True, True, [w2t, a_], [yp])
                    if j == 0:
                        p.op("dve", lambda e, f=f, yp=yp, c0=c0, cn=cn: e.tensor_copy(A[:, f, c0:c0 + cn], yp[:, :cn]), reads=[yp], writes=[A])
                    else:
                        tt(p, A[:, f, c0:c0 + cn], A[:, f, c0:c0 + cn], yp[:, :cn], ALU.add, [A, yp], [A])
        out_toks.append(p.dma("sp", y_out[:, t0:t0 + n].rearrange("(kc q) n -> q kc n", q=128), A[:, :, :n], reads=[A]))
    p.wait_final("sp", out_toks)
    p.finish()
    return nc


_IDENT = np.eye(128, dtype=np.float32)


def run_ffn(layer, mix_x, mix_c, x, ctx, mods, inp):
    lnp = np.ascontiguousarray(np.concatenate([colT(inp["ln1_g"][layer]), colT(inp["ln1_b"][layer]),
                                               colT(inp["ln2_g"][layer]), colT(inp["ln2_b"][layer])], axis=1))
    cores = list(range(NCORES))
    if layer == 0:
        segs = [128, 2048]
        E, HC = 1, 44
        w1b, w3b = blk(inp["ffn_w1"][0]), blk(inp["ffn_w3"][0])
        w2f = np.ascontiguousarray(inp["ffn_w2"][0])
        woutb = blk(inp["e_w_out"][0])
        nc = build_ffn(segs, E, HC, False, "full")
        in_maps = []
        for k in cores:
            b, hf = k // 2, k % 2
            in_maps.append({
                "mixT": np.ascontiguousarray(np.concatenate([mix_c[b, hf * 128:(hf + 1) * 128], mix_x[b, hf * 2048:(hf + 1) * 2048]], axis=0).T),
                "xT": np.ascontiguousarray(np.concatenate([ctx[b, hf * 128:(hf + 1) * 128], x[b, hf * 2048:(hf + 1) * 2048]], axis=0).T),
                "modv": np.ascontiguousarray(np.concatenate([modcols(mods, 0, 4), modcols(mods, 0, b)], axis=1)),
                "lnp": lnp, "woutb": woutb, "w1b": w1b, "w3b": w3b, "w2": w2f})
        res = run_bass_kernel_spmd(nc, in_maps, core_ids=cores)
        x_new = np.empty_like(x)
        c_new = np.empty_like(ctx)
        for k in cores:
            b, hf = k // 2, k % 2
            o = res.results[k]["xoT"]
            x_new[b, hf * 2048:(hf + 1) * 2048] = o[:, 128:].T
            c_new[b, hf * 128:(hf + 1) * 128] = o[:, :128].T
        return x_new, c_new
    woutb = blk(inp["o_w_out"][0])
    r = inp["moe_router"][0]
    router = np.ascontiguousarray(r.reshape(KC, 128, 8).transpose(1, 0, 2)).reshape(128, KC * 8)
    nc = build_ffn([2048], 0, 0, True, "pre")
    in_maps = []
    for k in cores:
        b, hf = k // 2, k % 2
        in_maps.append({"mixT": np.ascontiguousarray(mix_x[b, hf * 2048:(hf + 1) * 2048].T),
                        "xT": np.ascontiguousarray(x[b, hf * 2048:(hf + 1) * 2048].T),
                        "modv": modcols(mods, 1, b), "lnp": lnp, "woutb": woutb, "router": router, "ident": _IDENT})
    res = run_bass_kernel_spmd(nc, in_maps, core_ids=cores)
    xl = [res.results[k]["xl_out"] for k in cores]
    hT_all = np.ascontiguousarray(np.concatenate([res.results[k]["h_out"] for k in cores], axis=1))
    g_all = np.concatenate([res.results[k]["g_out"] for k in cores], axis=1)
    NTA = hT_all.shape[1]
    nc = build_exp(NTA, 56)
    in_maps = []
    for e in cores:
        in_maps.append({"hT_in": hT_all, "grow": np.ascontiguousarray(np.broadcast_to(g_all[e], (128, NTA))),
                        "w1b": blk(inp["moe_w1"][0][e]), "w3b": blk(inp["moe_w3"][0][e]),
                        "w2": np.ascontiguousarray(inp["moe_w2"][0][e])})
    res = run_bass_kernel_spmd(nc, in_maps, core_ids=cores)
    ys = [res.results[e]["y_out"] for e in cores]
    nc = build_ffn([2048], 0, 0, True, "post")
    in_maps = []
    for k in cores:
        b, hf = k // 2, k % 2
        in_maps.append({"parts": np.ascontiguousarray(np.concatenate([ys[e][:, k * 2048:(k + 1) * 2048] for e in cores], axis=0)),
                        "xl_in": xl[k], "modv": modcols(mods, 1, b), "lnp": lnp})
    res = run_bass_kernel_spmd(nc, in_maps, core_ids=cores)
    x_new = np.empty_like(x)
    for k in cores:
        b, hf = k // 2, k % 2
        x_new[b, hf * 2048:(hf + 1) * 2048] = res.results[k]["xoT"].T
    return x_new, None


T_CTX = 256
T_LAT = 4096
T_ALL = T_CTX + T_LAT
GC = 128
NCH = T_ALL // GC


def _tri_masks():
    j = np.arange(128)[:, None]
    i = np.arange(128)[None, :]
    return np.concatenate([(i >= j).astype(np.float32), (i <= j).astype(np.float32)], axis=1)


def build_gla():
    nc = bass.Bass("TRN2", target_bir_lowering=False)
    xT = dram_in(nc, "xT", [D, T_ALL])
    modv = dram_in(nc, "modv", [128, 2 * 32])
    wfm = dram_in(nc, "wfm", [16, 128, D], F32R)
    wv = dram_in(nc, "wv", [2, 128, KC * 512], F32R)
    wlr = dram_in(nc, "wlr", [128, 2 * KC * 16], F32R)
    gw2 = dram_in(nc, "gw2", [16, 2 * 512], F32R)
    gb = dram_in(nc, "gb", [128, 8])
    nw = dram_in(nc, "nw", [128, 4])
    cst = dram_in(nc, "cst", [128, 512])
    out = dram_out(nc, "out", [1024, T_LAT])
    q_s = nc.dram_tensor("q_s", [512, T_ALL], F32, kind="Internal").ap()
    k_s = nc.dram_tensor("k_s", [512, T_ALL], F32, kind="Internal").ap()
    r_s = nc.dram_tensor("r_s", [1024, T_ALL], F32, kind="Internal").ap()
    g_s = nc.dram_tensor("g_s", [2, 512, T_ALL], F32, kind="Internal").ap()
    v_s = nc.dram_tensor("v_s", [T_ALL, 1024], F32, kind="Internal").ap()
    o_s = nc.dram_tensor("o_s", [512, T_LAT], F32, kind="Internal").ap()
    RQ, RK, RR, RG, RV, RO = Res("q_s"), Res("k_s"), Res("r_s"), Res("g_s"), Res("v_s"), Res("o_s")
    p = Prog(nc)

    mod_sb = p.sb("mod_sb", [128, 64])
    gb_sb = p.sb("gb_sb", [128, 8])
    nw_sb = p.sb("nw_sb", [128, 4])
    cst_sb = p.sb("cst_sb", [128, 512])
    cst_r = p.sb("cst_r", [128, 512], F32R)
    wlr_sb = p.sb("wlr_sb", [128, 2 * KC * 16], F32R)
    gw2_sb = p.sb("gw2_sb", [16, 1024], F32R)
    pst = p.rot_ps("ps", 8, [128, 512])
    p.dma("sp", mod_sb[:], modv, writes=[mod_sb])
    p.dma("sp", gb_sb[:], gb, writes=[gb_sb])
    p.dma("sp", nw_sb[:], nw, writes=[nw_sb])
    p.dma("sp", cst_sb[:], cst, writes=[cst_sb])
    p.dma("pool", wlr_sb[:], wlr, writes=[wlr_sb])
    p.dma("pool", gw2_sb[:], gw2, writes=[gw2_sb])
    act(p, cst_r[:], cst_sb[:], AF.Copy, [cst_sb], [cst_r])
    ts(p, gb_sb[:], gb_sb[:], -1.0, None, ALU.mult, None, [gb_sb], [gb_sb])
    for s in range(2):
        ts(p, mod_sb[:, s * 32 + 16:s * 32 + 32], mod_sb[:, s * 32 + 16:s * 32 + 32], 1.0, None, ALU.add, None, [mod_sb], [mod_sb])
    maskf, maskr = cst_sb[:, 0:128], cst_sb[:, 128:256]
    ident_r = cst_r[:, 256:384]
    ones_f = cst_sb[:, 384:512]

    xt = p.sb("xt", [128, KC, 512])
    hT = p.sb("hT", [128, KC, 512], F32R)
    wblk = p.rot_sb("wblk", 3, [128, D], F32R)
    wv_sb = p.sb("wv_sb", [128, KC * 512], F32R)
    ev = p.rot_sb("ev", 3, [128, 512])
    lr_sb = p.rot_sb("lr", 2, [16, 512], F32R)
    tiles = [(0, 0, T_CTX)] + [(1, T_CTX + i * 512, 512) for i in range(T_LAT // 512)]
    for (s, t0, n) in tiles:
        p.dma("sp", xt[:, :, :n], xT[:, t0:t0 + n].rearrange("(kc q) n -> q kc n", q=128), writes=[xt])
        for kc in range(KC):
            ts(p, hT[:, kc, :n], xt[:, kc, :n], mod_sb[:, s * 32 + 16 + kc:s * 32 + 17 + kc], mod_sb[:, s * 32 + kc:s * 32 + kc + 1],
               ALU.mult, ALU.add, [xt, mod_sb], [hT])
        for m in range(16):
            w = wblk.next()
            p.dma("pool", w[:], wfm[m], writes=[w])
            ps = pst.next()
            for kc in range(KC):
                mm(p, ps[:, :n], w[:, kc * 128:(kc + 1) * 128], hT[:, kc, :n], kc == 0, kc == KC - 1, [w, hT], [ps])
            e_ = ev.next()
            act(p, e_[:, :n], ps[:, :n], AF.Copy, [ps], [e_])
            if m < 4:
                dst, R_ = q_s[m * 128:(m + 1) * 128, t0:t0 + n], RQ
            elif m < 8:
                dst, R_ = k_s[(m - 4) * 128:(m - 3) * 128, t0:t0 + n], RK
            else:
                dst, R_ = r_s[(m - 8) * 128:(m - 7) * 128, t0:t0 + n], RR
            p.dma("sp", dst, e_[:, :n], reads=[e_], writes=[R_])
        for d in range(2):
            ps = pst.next()
            for kc in range(KC):
                o_ = (d * KC + kc) * 16
                mm(p, ps[0:16, :n], wlr_sb[:, o_:o_ + 16], hT[:, kc, :n], kc == 0, kc == KC - 1, [wlr_sb, hT], [ps])
            l_ = lr_sb.next()
            act(p, l_[:, :n], ps[0:16, :n], AF.Copy, [ps], [l_])
            for blk_ in range(4):
                ps2 = pst.next()
                mm(p, ps2[:, :n], gw2_sb[:, d * 512 + blk_ * 128:d * 512 + (blk_ + 1) * 128], l_[:, :n], True, True, [gw2_sb, l_], [ps2])
                e_ = ev.next()
                act(p, e_[:, :n], ps2[:, :n], AF.Exp, [ps2, gb_sb], [e_], scale=-1.0, bias=gb_sb[:, d * 4 + blk_:d * 4 + blk_ + 1])
                act(p, e_[:, :n], e_[:, :n], AF.Ln, [e_], [e_], bias=1.0)
                ts(p, e_[:, :n], e_[:, :n], -1.0 / 16.0, None, ALU.mult, None, [e_], [e_])
                p.dma("sp", g_s[d, blk_ * 128:(blk_ + 1) * 128, t0:t0 + n], e_[:, :n], reads=[e_], writes=[RG])
        for h in range(2):
            p.dma("pool", wv_sb[:], wv[h], writes=[wv_sb])
            for st in range(n // 128):
                ps = pst.next()
                for kc in range(KC):
                    mm(p, ps[:, :], hT[:, kc, st * 128:(st + 1) * 128], wv_sb[:, kc * 512:(kc + 1) * 512], kc == 0, kc == KC - 1, [wv_sb, hT], [ps])
                e_ = ev.next()
                act(p, e_[:, :], ps[:, :], AF.Copy, [ps], [e_])
                p.dma("sp", v_s[t0 + st * 128:t0 + (st + 1) * 128, h * 512:(h + 1) * 512], e_[:, :], reads=[e_], writes=[RV])

    S32 = p.sb("S32", [128, 2, 512])
    Sr = p.sb("Sr", [128, 2, 512], F32R)
    qc_p = p.rot_sb("qc", 2, [128, 2, GC])
    kc_p = p.rot_sb("kc", 2, [128, 2, GC])
    gc_p = p.rot_sb("gc", 2, [128, 2, GC])
    vc_p = p.rot_sb("vc", 2, [128, 512], F32R)
    cum_p = p.rot_sb("cum", 2, [128, 2, GC])
    e_p = p.rot_sb("e", 2, [128, 2, GC])
    ei_p = p.rot_sb("ei", 2, [128, 2, GC])
    qi_p = p.rot_sb("qi", 2, [128, 2, GC], F32R)
    ki_p = p.rot_sb("ki", 2, [128, 2, GC], F32R)
    kd_p = p.rot_sb("kd", 2, [128, 2, GC], F32R)
    kdT_p = p.rot_sb("kdT", 2, [128, 256], F32R)
    aT_p = p.rot_sb("aT", 2, [128, GC], F32R)
    o_p = p.rot_sb("o", 2, [128, 4, GC])
    o0_p = p.rot_sb("o0", 2, [128, 4, GC])
    rc_p = p.rot_sb("rc", 2, [128, 4, GC])
    sq_p = p.rot_sb("sq", 2, [128, 4, GC], F32R)
    rs_p = p.rot_sb("rs", 2, [128, GC])
    out_toks = []
    for h in range(2):
        for d in range(2):
            p.op("dve", lambda e: e.memset(S32[:], 0.0), writes=[S32])
            act(p, Sr[:], S32[:], AF.Copy, [S32], [Sr])
            ctx_ch = [0, 1]
            lat_ch = list(range(2, NCH))
            order = (ctx_ch + lat_ch) if d == 0 else (ctx_ch[::-1] + lat_ch[::-1])
            for c in order:
                t0 = c * GC
                is_lat = c >= 2
                qc, kc_, gcn, vc = qc_p.next(), kc_p.next(), gc_p.next(), vc_p.next()
                p.dma("sp", kc_[:], k_s[h * 256:(h + 1) * 256, t0:t0 + GC].rearrange("(c q) n -> q c n", q=128), reads=[RK], writes=[kc_])
                p.dma("sp", gcn[:], g_s[d, h * 256:(h + 1) * 256, t0:t0 + GC].rearrange("(c q) n -> q c n", q=128), reads=[RG], writes=[gcn])
                p.dma("pool", vc[:], v_s[t0:t0 + GC, h * 512:(h + 1) * 512], reads=[RV], writes=[vc])
                cum, e_, ei = cum_p.next(), e_p.next(), ei_p.next()
                for k2 in range(2):
                    if d == 0:
                        p.op("dve", lambda e, cum=cum, gcn=gcn, k2=k2: e.tensor_tensor_scan(out=cum[:, k2, :], data0=ones_f, data1=gcn[:, k2, :], initial=0.0, op0=ALU.mult, op1=ALU.add),
                             reads=[gcn, cst_sb], writes=[cum])
                    else:
                        p.op("dve", lambda e, cum=cum, gcn=gcn, k2=k2: e.tensor_tensor_scan(out=cum[:, k2, ::-1], data0=ones_f, data1=gcn[:, k2, ::-1], initial=0.0, op0=ALU.mult, op1=ALU.add),
                             reads=[gcn, cst_sb], writes=[cum])
                act(p, ei[:], cum[:], AF.Exp, [cum], [ei], scale=-1.0)
                act(p, e_[:], cum[:], AF.Exp, [cum], [e_])
                ki, kd = ki_p.next(), kd_p.next()
                tt(p, ki[:], kc_[:], ei[:], ALU.mult, [kc_, ei], [ki])
                last = GC - 1 if d == 0 else 0
                for k2 in range(2):
                    ts(p, kd[:, k2, :], ki[:, k2, :], e_[:, k2, last:last + 1], None, ALU.mult, None, [ki, e_], [kd])
                tp = pst.next()
                for k2 in range(2):
                    mm(p, tp[:, k2 * 128:(k2 + 1) * 128], kd[:, k2, :], ident_r, True, True, [kd, cst_r], [tp])
                kdT = kdT_p.next()
                act(p, kdT[:], tp[:, 0:256], AF.Copy, [tp], [kdT])
                if is_lat:
                    qc = qc_p.next()
                    p.dma("sp", qc[:], q_s[h * 256:(h + 1) * 256, t0:t0 + GC].rearrange("(c q) n -> q c n", q=128), reads=[RQ], writes=[qc])
                    qi = qi_p.next()
                    stt(p, qi[:], qc[:], 1.0 / 16.0, e_[:], ALU.mult, ALU.mult, [qc, e_], [qi])
                    ap_ = pst.next()
                    for k2 in range(2):
                        mm(p, ap_[:, 0:GC], ki[:, k2, :], qi[:, k2, :], k2 == 0, k2 == 1, [ki, qi], [ap_])
                    aT = aT_p.next()
                    tt(p, aT[:], ap_[:, 0:GC], maskf if d == 0 else maskr, ALU.mult, [ap_, cst_sb], [aT])
                    op_ = pst.next()
                    for vch in range(4):
                        for k2 in range(2):
                            mm(p, op_[:, vch * GC:(vch + 1) * GC], Sr[:, k2, vch * 128:(vch + 1) * 128], qi[:, k2, :], k2 == 0, False, [Sr, qi], [op_])
                        mm(p, op_[:, vch * GC:(vch + 1) * GC], vc[:, vch * 128:(vch + 1) * 128], aT[:], False, True, [vc, aT], [op_])
                    tl = t0 - T_CTX
                    if d == 0:
                        o_ = o_p.next()
                        act(p, o_[:].rearrange("q a b -> q (a b)"), op_[:, :], AF.Copy, [op_], [o_])
                        p.dma("sp", o_s[:, tl:tl + GC].rearrange("(c q) n -> q c n", q=128), o_[:], reads=[o_], writes=[RO])
                    else:
                        o0, rc = o0_p.next(), rc_p.next()
                        p.dma("sp", o0[:], o_s[:, tl:tl + GC].rearrange("(c q) n -> q c n", q=128), reads=[RO], writes=[o0])
                        p.dma("sp", rc[:], r_s[h * 512:(h + 1) * 512, t0:t0 + GC].rearrange("(c q) n -> q c n", q=128), reads=[RR], writes=[rc])
                        o_ = o_p.next()
                        tt(p, o_[:].rearrange("q a b -> q (a b)"), op_[:, :], o0[:].rearrange("q a b -> q (a b)"), ALU.add, [op_, o0], [o_])
                        sq_ = sq_p.next()
                        act(p, sq_[:], o_[:], AF.Square, [o_], [sq_])
                        mp = pst.next()
                        for vch in range(4):
                            mm(p, mp[:, 0:GC], cst_r[:, 384:512], sq_[:, vch, :], vch == 0, vch == 3, [cst_r, sq_], [mp])
                        rs = rs_p.next()
                        ts(p, rs[:], mp[:, 0:GC], 1.0 / 512.0, EPS, ALU.mult, ALU.add, [mp], [rs])
                        act(p, rs[:], rs[:], AF.Sqrt, [rs], [rs])
                        p.op("dve", lambda e, rs=rs: e.reciprocal(rs[:], rs[:]), reads=[rs], writes=[rs])
                        act(p, rc[:], rc[:], AF.Silu, [rc], [rc])
                        for vch in range(4):
                            tt(p, o_[:, vch, :], o_[:, vch, :], rs[:], ALU.mult, [o_, rs], [o_])
                            stt(p, o_[:, vch, :], o_[:, vch, :], nw_sb[:, vch:vch + 1], rc[:, vch, :], ALU.mult, ALU.mult, [o_, nw_sb, rc], [o_])
                        out_toks.append(p.dma("sp", out[h * 512:(h + 1) * 512, tl:tl + GC].rearrange("(c q) n -> q c n", q=128), o_[:], reads=[o_]))
                for k2 in range(2):
                    sp_ = pst.next()
                    mm(p, sp_[:, :], kdT[:, k2 * 128:(k2 + 1) * 128], vc[:], True, True, [kdT, vc], [sp_])
                    stt(p, S32[:, k2, :], S32[:, k2, :], e_[:, k2, last:last + 1], sp_[:, :], ALU.mult, ALU.add, [S32, e_, sp_], [S32])
                act(p, Sr[:], S32[:], AF.Copy, [S32], [Sr])
    p.wait_final("sp", out_toks)
    p.finish()
    return nc


def run_gla(x, ctx, mods, inp):
    w_in = inp["o_w_in"][0]
    cst = np.ascontiguousarray(np.concatenate([_tri_masks(), np.eye(128, dtype=np.float32), np.ones((128, 128), np.float32)], axis=1))
    nc = build_gla()
    in_maps = []
    for k in range(NCORES):
        b, hh = k // 2, k % 2
        heads = [2 * hh, 2 * hh + 1]
        cols = []
        for h in heads:
            cols += list(range(h * 256, (h + 1) * 256))
        for h in heads:
            cols += list(range(1024 + h * 256, 1024 + (h + 1) * 256))
        for h in heads:
            cols += list(range(4096 + h * 512, 4096 + (h + 1) * 512))
        wfm = blk(np.ascontiguousarray(w_in[:, cols]))
        wv = np.stack([np.ascontiguousarray(w_in[:, 2048 + h * 512:2048 + (h + 1) * 512].reshape(KC, 128, 512).transpose(1, 0, 2)).reshape(128, KC * 512) for h in heads])
        wlr = np.ascontiguousarray(w_in[:, 6144:6176].reshape(KC, 128, 2, 16).transpose(1, 2, 0, 3)).reshape(128, 2 * KC * 16)
        dk0 = heads[0] * 256
        gw2 = np.ascontiguousarray(inp["gla_w2"][0][:, :, dk0:dk0 + 512].transpose(1, 0, 2)).reshape(16, 1024)
        gbv = inp["gla_b"][0][:, dk0:dk0 + 512]
        gb = np.ascontiguousarray(gbv.reshape(2, 4, 128).transpose(2, 0, 1)).reshape(128, 8)
        nw = colT(inp["gla_norm_w"][0])
        mv = np.concatenate([colT(mods[4, 1, 0]), colT(mods[4, 1, 1]), colT(mods[b, 1, 0]), colT(mods[b, 1, 1])], axis=1)
        xT = np.ascontiguousarray(np.concatenate([ctx[b], x[b]], axis=0).T)
        in_maps.append({"xT": xT, "modv": np.ascontiguousarray(mv), "wfm": wfm, "wv": wv, "wlr": wlr, "gw2": gw2, "gb": gb,
                        "nw": np.ascontiguousarray(nw), "cst": cst})
    res = run_bass_kernel_spmd(nc, in_maps, core_ids=list(range(NCORES)))
    mix = np.empty((4, T_LAT, D), np.float32)
    for k in range(NCORES):
        b, hh = k // 2, k % 2
        mix[b, :, hh * 1024:(hh + 1) * 1024] = res.results[k]["out"].T
    return mix


MAGIC = 12582912.0
TWO_PI = float(2 * np.pi)
CW1 = 6.28125
CW2 = float(2 * np.pi - 6.28125)
HALF_PI = float(np.pi / 2)
SCH = 1024


def emit_sin(p, out, x, k_t, reads, n_slice):
    rd = list(reads)
    ts(p, k_t, x, 1.0 / TWO_PI, MAGIC, ALU.mult, ALU.add, rd, [n_slice[0]])
    ts(p, k_t, k_t, -MAGIC, None, ALU.add, None, [n_slice[0]], [n_slice[0]])
    stt(p, x, k_t, -CW1, x, ALU.mult, ALU.add, [n_slice[0]] + rd, rd)
    stt(p, x, k_t, -CW2, x, ALU.mult, ALU.add, [n_slice[0]] + rd, rd)
    ts(p, x, x, 3.14159, -3.14159, ALU.min, ALU.max, rd, rd)
    act(p, out, x, AF.Sin, rd, [n_slice[1]])


def build_mix0(do_s5=True, do_dn=True):
    nc = bass.Bass("TRN2", target_bir_lowering=False)
    xT = dram_in(nc, "xT", [D, T_ALL])
    modv = dram_in(nc, "modv", [128, 64])
    wfm = dram_in(nc, "wfm", [20, 128, D], F32R)
    wtm = dram_in(nc, "wtm", [128, KC * 528], F32R)
    s5par = dram_in(nc, "s5par", [128, 3 * 32])
    s5bw = dram_in(nc, "s5bw", [4, 128, 2048], F32R)
    s5cw = dram_in(nc, "s5cw", [4, 128, 2048], F32R)
    s5d = dram_in(nc, "s5d", [128, 4])
    iota_d = dram_in(nc, "iota", [128, T_LAT])
    out_s5 = dram_out(nc, "out_s5", [512, T_ALL])
    dnpar_d = dram_in(nc, "dnpar", [128, 16])
    dncw_d = dram_in(nc, "dncw", [128, 12 * 9])
    dnnw_d = dram_in(nc, "dnnw", [128, 128])
    dncst_d = dram_in(nc, "dncst", [128, 6 * 128])
    out_dn = dram_out(nc, "out_dn", [T_ALL, 512])
    o0_s = nc.dram_tensor("o0_s", [T_ALL, 256], F32, kind="Internal").ap()
    RO0 = Res("o0_s")
    fm_s = nc.dram_tensor("fm_s", [20 * 128, T_ALL], F32, kind="Internal").ap()
    tm_s = nc.dram_tensor("tm_s", [T_ALL, 528], F32, kind="Internal").ap()
    RFM, RTM = Res("fm_s"), Res("tm_s")
    p = Prog(nc)
    pst = p.rot_ps("ps", 8, [128, 512])
    mod_sb = p.sb("mod_sb", [128, 64])
    p.dma("sp", mod_sb[:], modv, writes=[mod_sb])
    for s in range(2):
        ts(p, mod_sb[:, s * 32 + 16:s * 32 + 32], mod_sb[:, s * 32 + 16:s * 32 + 32], 1.0, None, ALU.add, None, [mod_sb], [mod_sb])

    p.begin_phase()
    xt = p.sb("xt", [128, KC, 512])
    hT = p.sb("hT", [128, KC, 512], F32R)
    wblk = p.rot_sb("wblk", 3, [128, D], F32R)
    wtm_sb = p.sb("wtm_sb", [128, KC * 528], F32R)
    ev = p.rot_sb("ev", 3, [128, 528])
    p.dma("pool", wtm_sb[:], wtm, writes=[wtm_sb])
    tiles = [(0, 0, T_CTX)] + [(1, T_CTX + i * 512, 512) for i in range(T_LAT // 512)]
    for (s, t0, n) in tiles:
        p.dma("sp", xt[:, :, :n], xT[:, t0:t0 + n].rearrange("(kc q) n -> q kc n", q=128), writes=[xt])
        for kc in range(KC):
            ts(p, hT[:, kc, :n], xt[:, kc, :n], mod_sb[:, s * 32 + 16 + kc:s * 32 + 17 + kc], mod_sb[:, s * 32 + kc:s * 32 + kc + 1],
               ALU.mult, ALU.add, [xt, mod_sb], [hT])
        for m in range(20):
            w = wblk.next()
            p.dma("pool", w[:], wfm[m], writes=[w])
            ps = pst.next()
            for kc in range(KC):
                mm(p, ps[:, :n], w[:, kc * 128:(kc + 1) * 128], hT[:, kc, :n], kc == 0, kc == KC - 1, [w, hT], [ps])
            e_ = ev.next()
            act(p, e_[:, :n], ps[:, :n], AF.Copy, [ps], [e_])
            p.dma("sp", fm_s[m * 128:(m + 1) * 128, t0:t0 + n], e_[:, :n], reads=[e_], writes=[RFM])
        for st in range(n // 128):
            ps = pst.next()
            ps2 = pst.next()
            for kc in range(KC):
                mm(p, ps[:, :], hT[:, kc, st * 128:(st + 1) * 128], wtm_sb[:, kc * 528:kc * 528 + 512], kc == 0, kc == KC - 1, [wtm_sb, hT], [ps])
            for kc in range(KC):
                mm(p, ps2[:, 0:16], hT[:, kc, st * 128:(st + 1) * 128], wtm_sb[:, kc * 528 + 512:kc * 528 + 528], kc == 0, kc == KC - 1, [wtm_sb, hT], [ps2])
            e_ = ev.next()
            act(p, e_[:, 0:512], ps[:, :], AF.Copy, [ps], [e_])
            act(p, e_[:, 512:528], ps2[:, 0:16], AF.Copy, [ps2], [e_])
            p.dma("sp", tm_s[t0 + st * 128:t0 + (st + 1) * 128, :], e_[:, :], reads=[e_], writes=[RTM])

    p.end_phase()
    out_toks = []
    if do_s5:
        p.begin_phase()
        par = p.sb("par", [128, 96])
        iota = p.sb("iota_sb", [128, T_LAT])
        d_sb = p.sb("d_sb", [128, 4])
        p.dma("sp", par[:], s5par, writes=[par])
        p.dma("sp", iota[:], iota_d, writes=[iota])
        p.dma("sp", d_sb[:], s5d, writes=[d_sb])
        cs = p.sb("cs", [128, 12, 32])
        LRE, LIM, LS = par[:, 0:32], par[:, 32:64], par[:, 64:96]
        (DEL, MAG, ANG, SINA, COSA, ARE, AIM, RDEN, FRE, FIM, T1, T2) = [cs[:, i, :] for i in range(12)]
        RC, RP = [cs], [par]
        act(p, DEL, LS, AF.Exp, RP, RC)
        tt(p, MAG, LRE, DEL, ALU.mult, RP + RC, RC)
        act(p, MAG, MAG, AF.Exp, RC, RC)
        tt(p, ANG, LIM, DEL, ALU.mult, RP + RC, RC)
        ts(p, T1, ANG, 1.0, None, ALU.mult, None, RC, RC)
        emit_sin(p, SINA, T1, T2, RC, (cs, cs))
        ts(p, T1, ANG, HALF_PI, None, ALU.add, None, RC, RC)
        emit_sin(p, COSA, T1, T2, RC, (cs, cs))
        tt(p, ARE, MAG, COSA, ALU.mult, RC, RC)
        tt(p, AIM, MAG, SINA, ALU.mult, RC, RC)
        tt(p, T1, LRE, LRE, ALU.mult, RP, RC)
        tt(p, T2, LIM, LIM, ALU.mult, RP, RC)
        tt(p, RDEN, T1, T2, ALU.add, RC, RC)
        p.op("dve", lambda e: e.reciprocal(RDEN, RDEN), reads=RC, writes=RC)
        ts(p, T1, ARE, -1.0, None, ALU.add, None, RC, RC)
        tt(p, FRE, T1, LRE, ALU.mult, RC + RP, RC)
        tt(p, T2, AIM, LIM, ALU.mult, RC + RP, RC)
        tt(p, FRE, FRE, T2, ALU.add, RC, RC)
        tt(p, FRE, FRE, RDEN, ALU.mult, RC, RC)
        tt(p, FIM, AIM, LRE, ALU.mult, RC + RP, RC)
        tt(p, T2, T1, LIM, ALU.mult, RC + RP, RC)
        tt(p, FIM, FIM, T2, ALU.subtract, RC, RC)
        tt(p, FIM, FIM, RDEN, ALU.mult, RC, RC)

        U = p.sb("U", [128, T_ALL], F32R)
        Y = p.sb("Y", [128, T_ALL])
        bw = p.sb("bw", [128, 2048], F32R)
        cw = p.sb("cw", [128, 2048], F32R)
        tabs = p.sb("tabs", [128, 4, SCH])
        kt = p.sb("kt", [128, SCH])
        bu = p.rot_sb("bu", 2, [128, 2, SCH])
        vv = p.rot_sb("vv", 2, [128, 2, SCH])
        gg = p.rot_sb("gg", 2, [128, 2, SCH])
        hh_ = p.rot_sb("hh", 2, [128, 2, SCH], F32R)
        tq = p.rot_sb("tq", 2, [128, SCH])
        ini = p.rot_sb("ini", 2, [128, 4])
        S_t, C_t, Ec, Es = tabs[:, 0, :], tabs[:, 1, :], tabs[:, 2, :], tabs[:, 3, :]
        for blk_ in range(4):
            p.dma("pool", U[:], fm_s[(12 + blk_) * 128:(13 + blk_) * 128, :], reads=[RFM], writes=[U])
            p.dma("pool", bw[:], s5bw[blk_], writes=[bw])
            p.dma("pool", cw[:], s5cw[blk_], writes=[cw])
            first_y = True
            for r4 in range(4):
                r = blk_ * 4 + r4
                half = r4 // 2
                for d in range(2):
                    col = d * 16 + r
                    th = ANG[:, col:col + 1]
                    rr = MAG[:, col:col + 1]
                    carry = None
                    chunks = [(0, 0, T_CTX)] + [(1, i * SCH, SCH) for i in range(T_LAT // SCH)]
                    for (seg, tau0, n) in chunks:
                        seg_off, seg_len = (0, T_CTX) if seg == 0 else (T_CTX, T_LAT)
                        if d == 0:
                            n0 = seg_off + tau0
                        else:
                            n0 = seg_off + seg_len - tau0 - n
                        rev = (d == 1)

                        def nat(ap):
                            return ap[:, ::-1] if rev else ap
                        ts(p, S_t[:, :n], iota[:, tau0:tau0 + n], th, None, ALU.mult, None, [iota, cs], [tabs])
                        ts(p, C_t[:, :n], S_t[:, :n], HALF_PI, None, ALU.add, None, [tabs], [tabs])
                        emit_sin(p, S_t[:, :n], S_t[:, :n], kt[:, :n], [tabs], (kt, tabs))
                        emit_sin(p, C_t[:, :n], C_t[:, :n], kt[:, :n], [tabs], (kt, tabs))
                        fre, fim = FRE[:, col:col + 1], FIM[:, col:col + 1]
                        ts(p, Ec[:, :n], C_t[:, :n], fre, None, ALU.mult, None, [tabs, cs], [tabs])
                        stt(p, Ec[:, :n], S_t[:, :n], fim, Ec[:, :n], ALU.mult, ALU.add, [tabs, cs], [tabs])
                        ts(p, Es[:, :n], S_t[:, :n], fre, None, ALU.mult, None, [tabs, cs], [tabs])
                        stt(p, Es[:, :n], C_t[:, :n], fim, Es[:, :n], ALU.mult, ALU.subtract, [tabs, cs], [tabs])
                        b_ = bu.next()
                        for reim in range(2):
                            wcol = ((d * 4 + r4) * 2 + reim) * 128
                            for c0 in range(0, n, 512):
                                cn = min(512, n - c0)
                                ps = pst.next()
                                mm(p, ps[:, :cn], bw[64 * half:64 * half + 64, wcol:wcol + 128], U[64 * half:64 * half + 64, n0 + c0:n0 + c0 + cn],
                                   True, True, [bw, U], [ps])
                                act(p, b_[:, reim, c0:c0 + cn], ps[:, :cn], AF.Copy, [ps], [b_])
                        bre, bim = nat(b_[:, 0, :n]), nat(b_[:, 1, :n])
                        v_ = vv.next()
                        t1, t2 = tq.next(), tq.next()
                        tt(p, t1[:, :n], Ec[:, :n], bre, ALU.mult, [tabs, b_], [t1])
                        tt(p, t2[:, :n], Es[:, :n], bim, ALU.mult, [tabs, b_], [t2])
                        tt(p, v_[:, 0, :n], t1[:, :n], t2[:, :n], ALU.subtract, [t1, t2], [v_])
                        tt(p, t1[:, :n], Ec[:, :n], bim, ALU.mult, [tabs, b_], [t1])
                        tt(p, t2[:, :n], Es[:, :n], bre, ALU.mult, [tabs, b_], [t2])
                        tt(p, v_[:, 1, :n], t1[:, :n], t2[:, :n], ALU.add, [t1, t2], [v_])
                        g_ = gg.next()
                        if seg == 1 and tau0 == 0:
                            i_ = ini.next()
                            pg = carry
                            c256, s256 = C_t[:, 256:257], S_t[:, 256:257]
                            tt(p, i_[:, 0:1], pg[:, 0, T_CTX - 1:T_CTX], c256, ALU.mult, [pg, tabs], [i_])
                            tt(p, i_[:, 1:2], pg[:, 1, T_CTX - 1:T_CTX], s256, ALU.mult, [pg, tabs], [i_])
                            tt(p, i_[:, 2:3], i_[:, 0:1], i_[:, 1:2], ALU.subtract, [i_], [i_])
                            tt(p, i_[:, 0:1], pg[:, 0, T_CTX - 1:T_CTX], s256, ALU.mult, [pg, tabs], [i_])
                            tt(p, i_[:, 1:2], pg[:, 1, T_CTX - 1:T_CTX], c256, ALU.mult, [pg, tabs], [i_])
                            tt(p, i_[:, 3:4], i_[:, 0:1], i_[:, 1:2], ALU.add, [i_], [i_])
                            inits = (i_[:, 2:3], i_[:, 3:4], i_)
                        elif carry is None:
                            inits = (0.0, 0.0, None)
                        else:
                            pn = carry_n
                            inits = (carry[:, 0, pn - 1:pn], carry[:, 1, pn - 1:pn], carry)
                        for reim in range(2):
                            rd = [v_, cs] + ([inits[2]] if inits[2] is not None else [])
                            p.op("dve", lambda e, g_=g_, v_=v_, reim=reim, n=n, ini_=inits[reim], rr=rr: e.tensor_tensor_scan(
                                out=g_[:, reim, :n], data0=rr.to_broadcast([128, n]), data1=v_[:, reim, :n], initial=ini_, op0=ALU.mult, op1=ALU.add),
                                reads=rd, writes=[g_])
                        carry, carry_n = g_, n
                        h_ = hh_.next()
                        t1, t2 = tq.next(), tq.next()
                        tt(p, t1[:, :n], C_t[:, :n], g_[:, 0, :n], ALU.mult, [tabs, g_], [t1])
                        tt(p, t2[:, :n], S_t[:, :n], g_[:, 1, :n], ALU.mult, [tabs, g_], [t2])
                        tt(p, nat(h_[:, 0, :n]), t1[:, :n], t2[:, :n], ALU.subtract, [t1, t2], [h_])
                        tt(p, t1[:, :n], S_t[:, :n], g_[:, 0, :n], ALU.mult, [tabs, g_], [t1])
                        tt(p, t2[:, :n], C_t[:, :n], g_[:, 1, :n], ALU.mult, [tabs, g_], [t2])
                        stt(p, nat(h_[:, 1, :n]), t1[:, :n], -1.0, t2[:, :n], ALU.mult, ALU.subtract, [t1, t2], [h_])
                        for c0 in range(0, n, 512):
                            cn = min(512, n - c0)
                            ps = pst.next()
                            for reim in range(2):
                                wcol = ((d * 4 + r4) * 2 + reim) * 128
                                mm(p, ps[:, :cn], cw[:, wcol:wcol + 128], h_[:, reim, c0:c0 + cn], reim == 0, reim == 1, [cw, h_], [ps])
                            ysl = Y[:, n0 + c0:n0 + c0 + cn]
                            if first_y:
                                p.op("dve", lambda e, ysl=ysl, ps=ps, cn=cn: e.tensor_copy(ysl, ps[:, :cn]), reads=[ps], writes=[Y])
                            else:
                                tt(p, ysl, ysl, ps[:, :cn], ALU.add, [Y, ps], [Y])
                    first_y = False
            for c0 in range(0, T_ALL, SCH):
                cn = min(SCH, T_ALL - c0)
                g1, g2 = tq.next(), tq.next()
                U_f = U.t[:].bitcast(F32)
                stt(p, Y[:, c0:c0 + cn], U_f[:, c0:c0 + cn], d_sb[:, blk_:blk_ + 1], Y[:, c0:c0 + cn], ALU.mult, ALU.add, [U, d_sb, Y], [Y])
                tt(p, g1[:, :cn], Y[:, c0:c0 + cn], Y[:, c0:c0 + cn], ALU.mult, [Y], [g1])
                ts(p, g1[:, :cn], g1[:, :cn], 0.044715, 1.0, ALU.mult, ALU.add, [g1], [g1])
                tt(p, g1[:, :cn], g1[:, :cn], Y[:, c0:c0 + cn], ALU.mult, [g1, Y], [g1])
                act(p, g1[:, :cn], g1[:, :cn], AF.Sigmoid, [g1], [g1], scale=1.5957691216057308)
                tt(p, g1[:, :cn], g1[:, :cn], Y[:, c0:c0 + cn], ALU.mult, [g1, Y], [g1])
                p.dma("sp", g2[:, :cn], fm_s[(16 + blk_) * 128:(17 + blk_) * 128, c0:c0 + cn], reads=[RFM], writes=[g2])
                act(p, g2[:, :cn], g2[:, :cn], AF.Sigmoid, [g2], [g2])
                tt(p, g1[:, :cn], g1[:, :cn], g2[:, :cn], ALU.mult, [g1, g2], [g1])
                out_toks.append(p.dma("sp", out_s5[blk_ * 128:(blk_ + 1) * 128, c0:c0 + cn], g1[:, :cn], reads=[g1]))
        p.end_phase()

    if do_dn:
        p.begin_phase()
        dnpar = p.sb("dnpar_sb", [128, 16])
        dncw = p.sb("dncw_sb", [128, 108])
        dnnw = p.sb("dnnw_sb", [128, 128])
        cst = p.sb("dncst_sb", [128, 768])
        for t_, d_ in ((dnpar, dnpar_d), (dncw, dncw_d), (dnnw, dnnw_d), (cst, dncst_d)):
            p.dma("sp", t_[:], d_, writes=[t_])
        LE, GE, GT, LT, IDN, ONE = [cst[:, i * 128:(i + 1) * 128] for i in range(6)]
        nexpa = p.sb("nexpa", [128, 8])
        act(p, nexpa[:], dnpar[:, 0:8], AF.Exp, [dnpar], [nexpa])
        ts(p, nexpa[:], nexpa[:], -1.0, None, ALU.mult, None, [nexpa], [nexpa])
        raw = p.sb("raw", [128, T_ALL])
        qkvs = [[p.sb(f"qkv{a}_{i}", [128, T_ALL]) for i in range(3)] for a in range(2)]
        sqb = p.rot_sb("sqb", 2, [128, 512])
        rsb = p.rot_sb("rsb", 2, [128, 512])
        Ss = [p.sb(f"S_dn{a}", [128, 128]) for a in range(2)]
        tmc_p = p.rot_sb("tmc", 2, [128, 528])
        col_p = p.rot_sb("col", 4, [128, 16])
        names = ["gcb", "DT", "Dm", "X", "XT", "IX", "MT", "attnT", "ktok", "vtok", "kbe", "vb", "WT", "U", "vnew", "o2", "kg", "o", "o0", "zz"]
        sq_ = {nm: p.rot_sb("dn_" + nm, 4, [128, 128]) for nm in names}

        _nh, _nc, _it = 4, 999, 6
        for hp in range(2):
            for hi in range(2):
                hl = hp * 2 + hi
                qkv = qkvs[hi]
                for qi_ in range(3):
                    blk_i = qi_ * 4 + hl
                    dst = qkv[qi_]
                    p.dma("sp", raw[:], fm_s[blk_i * 128:(blk_i + 1) * 128, :], reads=[RFM], writes=[raw])
                    wc = lambda i, j: dncw[:, blk_i * 9 + i * 3 + j:blk_i * 9 + i * 3 + j + 1]
                    rl = raw[:, T_CTX:T_ALL].rearrange("q (r c) -> q r c", c=64)
                    dl = dst[:, T_CTX:T_ALL].rearrange("q (r c) -> q r c", c=64)
                    ts(p, dst[:, T_CTX:T_ALL], raw[:, T_CTX:T_ALL], wc(1, 1), None, ALU.mult, None, [raw, dncw], [dst])
                    for i in range(3):
                        for j in range(3):
                            if i == 1 and j == 1:
                                continue
                            di, dj = i - 1, j - 1
                            r0, r1 = max(0, -di), 64 - max(0, di)
                            c0, c1 = max(0, -dj), 64 - max(0, dj)
                            stt(p, dl[:, r0:r1, c0:c1], rl[:, r0 + di:r1 + di, c0 + dj:c1 + dj], wc(i, j), dl[:, r0:r1, c0:c1], ALU.mult, ALU.add, [raw, dncw, dst], [dst])
                    ts(p, dst[:, 0:T_CTX], raw[:, 0:T_CTX], wc(1, 1), None, ALU.mult, None, [raw, dncw], [dst])
                    stt(p, dst[:, 1:T_CTX], raw[:, 0:T_CTX - 1], wc(1, 0), dst[:, 1:T_CTX], ALU.mult, ALU.add, [raw, dncw, dst], [dst])
                    stt(p, dst[:, 0:T_CTX - 1], raw[:, 1:T_CTX], wc(1, 2), dst[:, 0:T_CTX - 1], ALU.mult, ALU.add, [raw, dncw, dst], [dst])
                    act(p, dst[:], dst[:], AF.Silu, [dst], [dst])
                    if qi_ < 2:
                        for c0 in range(0, T_ALL, 512):
                            cn = min(512, T_ALL - c0)
                            sb_, rb_ = sqb.next(), rsb.next()
                            act(p, sb_[:, :cn], dst[:, c0:c0 + cn], AF.Square, [dst], [sb_])
                            ps = pst.next()
                            mm(p, ps[:, :cn], ONE, sb_[:, :cn], True, True, [cst, sb_], [ps])
                            ts(p, rb_[:, :cn], ps[:, :cn], EPS, None, ALU.add, None, [ps], [rb_])
                            act(p, rb_[:, :cn], rb_[:, :cn], AF.Sqrt, [rb_], [rb_])
                            p.op("dve", lambda e, rb_=rb_, cn=cn: e.reciprocal(rb_[:, :cn], rb_[:, :cn]), reads=[rb_], writes=[rb_])
                            if qi_ == 0:
                                stt(p, dst[:, c0:c0 + cn], dst[:, c0:c0 + cn], float(128 ** -0.5), rb_[:, :cn], ALU.mult, ALU.mult, [dst, rb_], [dst])
                            else:
                                tt(p, dst[:, c0:c0 + cn], dst[:, c0:c0 + cn], rb_[:, :cn], ALU.mult, [dst, rb_], [dst])
            for d in range(2):
                TRI, MI, MS = (LE, LE, GT) if d == 0 else (GE, GE, LT)
                last = 127 if d == 0 else 0
                for S in Ss:
                    p.op("dve", lambda e, S=S: e.memset(S[:], 0.0), writes=[S])
                order = [0, 1] + list(range(2, NCH)) if d == 0 else [1, 0] + list(range(NCH - 1, 1, -1))
                for c in order[:_nc]:
                    t0 = c * GC
                    tmc = tmc_p.next()
                    p.dma("sp", tmc[:], tm_s[t0:t0 + GC, :], reads=[RTM], writes=[tmc])
                    for hi in range(2):
                        hl = hp * 2 + hi
                        pi = d * 4 + hl
                        qn, kn, vs = qkvs[hi]
                        S = Ss[hi]
                        cl = col_p.next()
                        beta, nbeta, g, gc, ngc, egc, sc, gl, kgc, egl = [cl[:, i:i + 1] for i in range(10)]
                        R_ = [cl]
                        act(p, beta, tmc[:, 512 + pi:513 + pi], AF.Exp, [tmc], R_, scale=-1.0)
                        ts(p, beta, beta, 1.0, None, ALU.add, None, R_, R_)
                        p.op("dve", lambda e, beta=beta: e.reciprocal(beta, beta), reads=R_, writes=R_)
                        ts(p, nbeta, beta, -1.0, None, ALU.mult, None, R_, R_)
                        act(p, g, tmc[:, 520 + pi:521 + pi], AF.Exp, [tmc, dnpar], R_, bias=dnpar[:, 8 + pi:9 + pi])
                        act(p, g, g, AF.Ln, R_, R_, bias=1.0)
                        ts(p, g, g, nexpa[:, pi:pi + 1], None, ALU.mult, None, R_ + [nexpa], R_)
                        ps = pst.next()
                        mm(p, ps[:, 0:1], TRI, g, True, True, [cst, cl], [ps])
                        act(p, gc, ps[:, 0:1], AF.Copy, [ps], R_)
                        ts(p, ngc, gc, -1.0, None, ALU.mult, None, R_, R_)
                        act(p, egc, gc, AF.Exp, R_, R_)
                        tt(p, sc, beta, egc, ALU.mult, R_, R_)
                        gcb = sq_["gcb"].next()
                        ts(p, gcb[:], ONE, gc, None, ALU.mult, None, [cst] + R_, [gcb])
                        bc = pst.next()
                        mm(p, bc[:, 0:128], gcb[:], IDN, True, True, [gcb, cst], [bc])
                        DT, Dm = sq_["DT"].next(), sq_["Dm"].next()
                        act(p, DT[:], bc[:, 0:128], AF.Exp, [bc] + R_, [DT], bias=ngc)
                        stt(p, DT[:], DT[:], 1.0, MI, ALU.min, ALU.mult, [DT, cst], [DT])
                        act(p, Dm[:], bc[:, 0:128], AF.Exp, [bc] + R_, [Dm], scale=-1.0, bias=gc)
                        stt(p, Dm[:], Dm[:], 1.0, MS, ALU.min, ALU.mult, [Dm, cst], [Dm])
                        act(p, gl, bc[:, last:last + 1], AF.Copy, [bc], R_)
                        act(p, kgc, gc, AF.Exp, R_, R_, scale=-1.0, bias=gl)
                        act(p, egl, gl, AF.Exp, R_, R_)
                        kT, qT, vT = kn[:, t0:t0 + GC], qn[:, t0:t0 + GC], vs[:, t0:t0 + GC]
                        kk = pst.next()
                        mm(p, kk[:, 0:128], kT, kT, True, True, [kn], [kk])
                        X, XT = sq_["X"].next(), sq_["XT"].next()
                        stt(p, X[:], kk[:, 0:128], nbeta, Dm[:], ALU.mult, ALU.mult, [kk, Dm] + R_, [X])
                        tp = pst.next()
                        mm(p, tp[:, 0:128], X[:], IDN, True, True, [X, cst], [tp])
                        act(p, XT[:], tp[:, 0:128], AF.Copy, [tp], [XT])
                        qk = pst.next()
                        mm(p, qk[:, 0:128], kT, qT, True, True, [kn, qn], [qk])
                        attnT = sq_["attnT"].next()
                        tt(p, attnT[:], qk[:, 0:128], DT[:], ALU.mult, [qk, DT], [attnT])
                        ktok, vtok = sq_["ktok"].next(), sq_["vtok"].next()
                        tp = pst.next()
                        mm(p, tp[:, 0:128], kT, IDN, True, True, [kn, cst], [tp])
                        act(p, ktok[:], tp[:, 0:128], AF.Copy, [tp], [ktok])
                        tp = pst.next()
                        mm(p, tp[:, 0:128], vT, IDN, True, True, [vs, cst], [tp])
                        act(p, vtok[:], tp[:, 0:128], AF.Copy, [tp], [vtok])
                        kbe, vb, kg = sq_["kbe"].next(), sq_["vb"].next(), sq_["kg"].next()
                        ts(p, kbe[:], ktok[:], sc, None, ALU.mult, None, [ktok] + R_, [kbe])
                        ts(p, vb[:], vtok[:], beta, None, ALU.mult, None, [vtok] + R_, [vb])
                        ts(p, kg[:], ktok[:], kgc, None, ALU.mult, None, [ktok] + R_, [kg])
                        MT = sq_["MT"].next()
                        tt(p, MT[:], XT[:], IDN, ALU.add, [XT, cst], [MT])
                        _stg = 4
                        for it in range(_it):
                            x2 = pst.next()
                            mm(p, x2[:, 0:128], XT[:], X[:], True, True, [XT, X], [x2])
                            Xn = sq_["X"].next()
                            XTn = sq_["XT"].next()
                            if it < 5 and _stg >= 2:
                                xt2 = pst.next()
                                mm(p, xt2[:, 0:128], X[:], XT[:], True, True, [X, XT], [xt2])
                                act(p, XTn[:], xt2[:, 0:128], AF.Copy, [xt2], [XTn])
                            p.op("dve", lambda e, Xn=Xn, x2=x2: e.tensor_copy(Xn[:], x2[:, 0:128]), reads=[x2], writes=[Xn])
                            IX = sq_["IX"].next()
                            tt(p, IX[:], Xn[:], IDN, ALU.add, [Xn, cst], [IX])
                            if it < 5:
                                X, XT = Xn, XTn
                            if _stg >= 4:
                                mp = pst.next()
                                mm(p, mp[:, 0:128], IX[:], MT[:], True, True, [IX, MT], [mp])
                                MT = sq_["MT"].next()
                                p.op("dve", lambda e, MT=MT, mp=mp: e.tensor_copy(MT[:], mp[:, 0:128]), reads=[mp], writes=[MT])
                        WT, U_ = sq_["WT"].next(), sq_["U"].next()
                        wp = pst.next()
                        mm(p, wp[:, 0:128], kbe[:], MT[:], True, True, [kbe, MT], [wp])
                        act(p, WT[:], wp[:, 0:128], AF.Copy, [wp], [WT])
                        up = pst.next()
                        mm(p, up[:, 0:128], MT[:], vb[:], True, True, [MT, vb], [up])
                        act(p, U_[:], up[:, 0:128], AF.Copy, [up], [U_])
                        wsp = pst.next()
                        mm(p, wsp[:, 0:128], WT[:], S[:], True, True, [WT, S], [wsp])
                        vnew = sq_["vnew"].next()
                        tt(p, vnew[:], U_[:], wsp[:, 0:128], ALU.subtract, [U_, wsp], [vnew])
                        o1 = pst.next()
                        mm(p, o1[:, 0:128], qT, S[:], True, True, [qn, S], [o1])
                        o2p = pst.next()
                        mm(p, o2p[:, 0:128], attnT[:], vnew[:], True, True, [attnT, vnew], [o2p])
                        o2 = sq_["o2"].next()
                        act(p, o2[:], o2p[:, 0:128], AF.Copy, [o2p], [o2])
                        o_ = sq_["o"].next()
                        stt(p, o_[:], o1[:, 0:128], egc, o2[:], ALU.mult, ALU.add, [o1, o2] + R_, [o_])
                        sp_ = pst.next()
                        mm(p, sp_[:, 0:128], kg[:], vnew[:], True, True, [kg, vnew], [sp_])
                        stt(p, S[:], S[:], egl, sp_[:, 0:128], ALU.mult, ALU.add, [S, sp_] + R_, [S])
                        if d == 0:
                            p.dma("sp", o0_s[t0:t0 + GC, hi * 128:(hi + 1) * 128], o_[:], reads=[o_], writes=[RO0])
                        else:
                            o0 = sq_["o0"].next()
                            p.dma("sp", o0[:], o0_s[t0:t0 + GC, hi * 128:(hi + 1) * 128], reads=[RO0], writes=[o0])
                            tt(p, o_[:], o_[:], o0[:], ALU.add, [o_, o0], [o_])
                            zz = sq_["zz"].next()
                            tt(p, zz[:], o_[:], o_[:], ALU.mult, [o_], [zz])
                            p.op("dve", lambda e, cl=cl, zz=zz: e.reduce_sum(out=cl[:, 10:11], in_=zz[:], axis=AX.X), reads=[zz], writes=R_)
                            ts(p, cl[:, 10:11], cl[:, 10:11], 1.0 / 128.0, EPS, ALU.mult, ALU.add, R_, R_)
                            act(p, cl[:, 10:11], cl[:, 10:11], AF.Ln, R_, R_)
                            act(p, cl[:, 10:11], cl[:, 10:11], AF.Exp, R_, R_, scale=-0.5)
                            act(p, zz[:], tmc[:, hl * 128:(hl + 1) * 128], AF.Exp, [tmc], [zz], scale=-1.0)
                            ts(p, zz[:], zz[:], 1.0, None, ALU.add, None, [zz], [zz])
                            p.op("dve", lambda e, zz=zz: e.reciprocal(zz[:], zz[:]), reads=[zz], writes=[zz])
                            tt(p, zz[:], zz[:], tmc[:, hl * 128:(hl + 1) * 128], ALU.mult, [zz, tmc], [zz])
                            stt(p, o_[:], o_[:], cl[:, 10:11], dnnw[:], ALU.mult, ALU.mult, [o_, dnnw] + R_, [o_])
                            tt(p, o_[:], o_[:], zz[:], ALU.mult, [o_, zz], [o_])
                            out_toks.append(p.dma("sp", out_dn[t0:t0 + GC, hl * 128:(hl + 1) * 128], o_[:], reads=[o_]))
        p.end_phase()
    p.wait_final("sp", out_toks)
    p.finish()
    return nc


def mix0_inputs(k, x, ctx, mods, inp):
    b, hh = k // 2, k % 2
    w_in = inp["e_w_in"][0]
    H = [4 * hh + i for i in range(4)]
    cols = []
    for base in (0, 1024, 2048):
        for h in H:
            cols += list(range(base + h * 128, base + (h + 1) * 128))
    cols += list(range(4096 + hh * 512, 4096 + (hh + 1) * 512))
    cols += list(range(5120 + hh * 512, 5120 + (hh + 1) * 512))
    wfm = blk(np.ascontiguousarray(w_in[:, cols]))
    tcols = []
    for h in H:
        tcols += list(range(3072 + h * 128, 3072 + (h + 1) * 128))
    tcols += [6144 + h for h in H] + [6152 + h for h in H] + [6160 + h for h in H] + [6168 + h for h in H]
    wtm = np.ascontiguousarray(w_in[:, tcols].reshape(KC, 128, 528).transpose(1, 0, 2)).reshape(128, KC * 528)
    g0 = 32 * hh
    lre, lim, lst = inp["s5_lam_re"][0], inp["s5_lam_im"][0], inp["s5_log_step"][0]
    par = np.zeros((128, 3, 2, 16), np.float32)
    for d in range(2):
        for r in range(16):
            for gip in range(2):
                g = g0 + 2 * r + gip
                par[gip * 64:(gip + 1) * 64, 0, d, r] = lre[d, g]
                par[gip * 64:(gip + 1) * 64, 1, d, r] = lim[d, g]
                par[gip * 64:(gip + 1) * 64, 2, d, r] = lst[d, g]
    bre, bim, cre, cim = inp["s5_b_re"][0], inp["s5_b_im"][0], inp["s5_c_re"][0], inp["s5_c_im"][0]
    bw = np.zeros((4, 128, 2, 4, 2, 128), np.float32)
    cw = np.zeros((4, 128, 2, 4, 2, 128), np.float32)
    for bl in range(4):
        for gl in range(8):
            g = g0 + 8 * bl + gl
            r4, gip = gl // 2, gl % 2
            for d in range(2):
                bw[bl, gl * 16:(gl + 1) * 16, d, r4, 0, gip * 64:(gip + 1) * 64] = bre[d, g].T
                bw[bl, gl * 16:(gl + 1) * 16, d, r4, 1, gip * 64:(gip + 1) * 64] = bim[d, g].T
                cw[bl, gip * 64:(gip + 1) * 64, d, r4, 0, gl * 16:(gl + 1) * 16] = cre[d, g].T
                cw[bl, gip * 64:(gip + 1) * 64, d, r4, 1, gl * 16:(gl + 1) * 16] = cim[d, g].T
    s5d = np.ascontiguousarray(inp["s5_d"][0][hh * 512:(hh + 1) * 512].reshape(4, 128).T)
    mv = np.concatenate([colT(mods[4, 0, 0]), colT(mods[4, 0, 1]), colT(mods[b, 0, 0]), colT(mods[b, 0, 1])], axis=1)
    xT = np.ascontiguousarray(np.concatenate([ctx[b], x[b]], axis=0).T)
    dnpar = np.zeros((128, 16), np.float32)
    for d in range(2):
        for hl, h in enumerate(H):
            dnpar[:, d * 4 + hl] = inp["dn_a_log"][0][d, h]
            dnpar[:, 8 + d * 4 + hl] = inp["dn_dt_bias"][0][d, h]
    cwv = inp["dn_conv"][0]
    dncw = np.zeros((128, 12, 9), np.float32)
    for qi_, base in enumerate((0, 1024, 2048)):
        for hl, h in enumerate(H):
            dncw[:, qi_ * 4 + hl, :] = cwv[:, :, base + h * 128:base + (h + 1) * 128].reshape(9, 128).T
    dnnw = np.ascontiguousarray(np.broadcast_to(inp["dn_norm_w"][0], (128, 128)))
    pp = np.arange(128)[:, None]
    ff = np.arange(128)[None, :]
    dncst = np.ascontiguousarray(np.concatenate([(pp <= ff), (pp >= ff), (pp > ff), (pp < ff), (pp == ff), np.ones((128, 128), bool)], axis=1).astype(np.float32))
    return {"dnpar": dnpar, "dncw": dncw.reshape(128, 108), "dnnw": dnnw, "dncst": dncst,
            "xT": xT, "modv": np.ascontiguousarray(mv), "wfm": wfm, "wtm": wtm,
            "s5par": np.ascontiguousarray(par.reshape(128, 96)), "s5bw": bw.reshape(4, 128, 2048), "s5cw": cw.reshape(4, 128, 2048),
            "s5d": s5d, "iota": np.ascontiguousarray(np.broadcast_to(np.arange(T_LAT, dtype=np.float32), (128, T_LAT)))}


def run_mix0(x, ctx, mods, inp):
    nc = build_mix0(True, True)
    in_maps = [mix0_inputs(k, x, ctx, mods, inp) for k in range(NCORES)]
    res = run_bass_kernel_spmd(nc, in_maps, core_ids=list(range(NCORES)))
    mix_x = np.empty((4, T_LAT, D), np.float32)
    mix_c = np.empty((4, T_CTX, D), np.float32)
    for k in range(NCORES):
        b, hh = k // 2, k % 2
        s5 = res.results[k]["out_s5"]
        dn = res.results[k]["out_dn"]
        mix_c[b, :, hh * 512:(hh + 1) * 512] = s5[:, :T_CTX].T
        mix_x[b, :, hh * 512:(hh + 1) * 512] = s5[:, T_CTX:].T
        mix_c[b, :, 1024 + hh * 512:1024 + (hh + 1) * 512] = dn[:T_CTX]
        mix_x[b, :, 1024 + hh * 512:1024 + (hh + 1) * 512] = dn[T_CTX:]
    return mix_x, mix_c


def kernel(**inputs):
    inp = {k: np.asarray(v) for k, v in inputs.items()}
    x = np.ascontiguousarray(inp["x"], dtype=np.float32)
    ctx = np.ascontiguousarray(inp["ctx"], dtype=np.float32)
    mods = run_mods(inp)
    mix_x, mix_c = run_mix0(x, ctx, mods, inp)
    x1, c1 = run_ffn(0, mix_x, mix_c, x, ctx, mods, inp)
    mix1 = run_gla(x1, c1, mods, inp)
    x2, _ = run_ffn(1, mix1, None, x1, None, mods, inp)
    return x2.astype(np.float32)
```
